# Optimizing a Trainium2 kernel written in Bass

```python
import jax, jax.numpy as jnp
from jax import lax
import numpy as np

D_MODEL = 1024
BATCH = 16
SEQ = 256
DEPTH = 1
DEC_BATCH = 4
DEC_SEQ = 2048
PAST_LEN = 256

GRID_W = 64
CHUNK = GRID_W
HG_H = 8
HG_DK = D_MODEL // HG_H
HG_DV = D_MODEL // HG_H
HG_KW = HG_H * HG_DK
HG_VW = HG_H * HG_DV
GLA_H = 4
GLA_DK = D_MODEL // 2 // GLA_H
GLA_DV = D_MODEL // GLA_H
GLA_KW = GLA_H * GLA_DK
GLA_VW = GLA_H * GLA_DV
GLA_RANK = 16
GLA_GATE_NORM = 16.0
N_GROUPS = 4
EXPERTS_PER_GROUP = 8
N_EXPERTS = N_GROUPS * EXPERTS_PER_GROUP
TOP_K_IN_GROUP = 2
D_EXPERT = D_MODEL // 4
EPS = 1e-6
IN_SIZES = (HG_KW, HG_KW, HG_KW, HG_VW, HG_VW,
            GLA_KW, GLA_KW, GLA_VW, GLA_VW, GLA_RANK, GLA_RANK,
            D_MODEL, D_MODEL)
IN_WIDTH = sum(IN_SIZES)
IN_SPLIT_POINTS = tuple(int(v) for v in np.cumsum(IN_SIZES)[:-1])

kernel_name = "hybrid_hgrn2_gla_hmoe_diffusion_step"


def rmsnorm(x, g):
    xf = x.astype(jnp.float32)
    y = xf * lax.rsqrt(jnp.mean(xf * xf, axis=-1, keepdims=True) + EPS)
    return (y * g.astype(jnp.float32)).astype(x.dtype)


def gated_chunk_scan(q, k, v, log_a, s0, n_chunks):
    b, L, h, _ = q.shape
    dv = v.shape[-1]
    c = L // n_chunks

    def to_chunks(t):
        return t.astype(jnp.float32).reshape(b, n_chunks, c, h, t.shape[-1]).transpose(1, 0, 3, 2, 4)

    causal = jnp.tril(jnp.ones((c, c), dtype=bool))[:, :, None]

    def step(s, inp):
        qc, kc, vc, gc = inp
        cum = jnp.cumsum(gc, axis=2)
        last = cum[:, :, -1:, :]
        o_inter = jnp.einsum('bhtk,bhkv->bhtv', qc * jnp.exp(cum), s)
        diff = cum[:, :, :, None, :] - cum[:, :, None, :, :]
        decay = jnp.where(causal, jnp.exp(jnp.where(causal, diff, 0.0)), 0.0)
        scores = jnp.einsum('bhtk,bhsk,bhtsk->bhts', qc, kc, decay)
        o_intra = jnp.einsum('bhts,bhsv->bhtv', scores, vc)
        s_new = jnp.exp(last[:, :, 0, :, None]) * s + jnp.einsum('bhsk,bhsv->bhkv', kc * jnp.exp(last - cum), vc)
        return s_new, o_inter + o_intra

    s_fin, o = lax.scan(step, s0.astype(jnp.float32),
                        (to_chunks(q), to_chunks(k), to_chunks(v), to_chunks(log_a)))
    o = o.transpose(1, 0, 3, 2, 4).reshape(b, L, h, dv)
    return o, s_fin


def bidirectional_scan(q, k_f, k_b, v, g_f, g_b, s0, n_chunks):
    flip = lambda t: jnp.flip(t, axis=1)
    o_f, s_f = gated_chunk_scan(q, k_f, v, g_f, s0[:, 0], n_chunks)
    o_b, s_b = gated_chunk_scan(flip(q), flip(k_b), flip(v), flip(g_b), s0[:, 1], n_chunks)
    return o_f + flip(o_b), jnp.stack([s_f, s_b], axis=1)


def head_norm_gate(o, gate, g, dtype):
    y = o * lax.rsqrt(jnp.mean(o * o, axis=-1, keepdims=True) + EPS) * g.astype(jnp.float32)
    b, L = o.shape[:2]
    return (y.reshape(b, L, -1) * jax.nn.silu(gate.astype(jnp.float32))).astype(dtype)


def token_mixer(h, s0_hg, s0_gla, n_chunks, lb, w_in, hg_norm_g, gla_wa_up, gla_ba, gla_norm_g,
                w_br_a, w_br_b, w_out):
    b, L, _ = h.shape
    proj = h @ w_in
    (hq, hf_f, hf_b, hi, hgate, gq, gk, gv, gr, ga_f, ga_b, m_a, m_b) = jnp.split(proj, IN_SPLIT_POINTS, axis=-1)
    heads = lambda t, n: t.reshape(b, L, n, -1)
    f_f = lb + (1.0 - lb) * jax.nn.sigmoid(hf_f.astype(jnp.float32))
    f_b = lb + (1.0 - lb) * jax.nn.sigmoid(hf_b.astype(jnp.float32))
    o_hg, st_hg = bidirectional_scan(heads(hq, HG_H) * HG_DK ** -0.5,
                                     heads(1.0 - f_f, HG_H), heads(1.0 - f_b, HG_H), heads(hi, HG_H),
                                     heads(jnp.log(f_f), HG_H), heads(jnp.log(f_b), HG_H), s0_hg, n_chunks)
    log_a_f = jax.nn.log_sigmoid((ga_f @ gla_wa_up[0] + gla_ba[0]).astype(jnp.float32)) / GLA_GATE_NORM
    log_a_b = jax.nn.log_sigmoid((ga_b @ gla_wa_up[1] + gla_ba[1]).astype(jnp.float32)) / GLA_GATE_NORM
    kg = heads(gk, GLA_H)
    o_gla, st_gla = bidirectional_scan(heads(gq, GLA_H) * GLA_DK ** -0.5, kg, kg, heads(gv, GLA_H),
                                       heads(log_a_f, GLA_H), heads(log_a_b, GLA_H), s0_gla, n_chunks)
    y_hg = head_norm_gate(o_hg, hgate, hg_norm_g, h.dtype)
    y_gla = head_norm_gate(o_gla, gr, gla_norm_g, h.dtype)
    merged = jax.nn.sigmoid(m_a) * (y_hg @ w_br_a) + jax.nn.sigmoid(m_b) * (y_gla @ w_br_b)
    return merged @ w_out, st_hg, st_gla


def hierarchical_moe(h, w_router_group, w_router_expert, w_exp_gate, w_exp_up, w_exp_down):
    b, L, d = h.shape
    t = h.reshape(-1, d)
    p_group = jax.nn.softmax((t @ w_router_group).astype(jnp.float32), axis=-1)
    g_idx = jnp.argmax(p_group, axis=-1)
    p_top = jnp.take_along_axis(p_group, g_idx[:, None], axis=-1)
    exp_logits = jnp.einsum('nd,gde->nge', t, w_router_expert).astype(jnp.float32)
    sel = jnp.take_along_axis(exp_logits, g_idx[:, None, None], axis=1)[:, 0]
    w_top, e_idx = lax.top_k(jax.nn.softmax(sel, axis=-1), TOP_K_IN_GROUP)
    w_top = w_top / jnp.sum(w_top, axis=-1, keepdims=True) * p_top
    expert_id = g_idx[:, None] * EXPERTS_PER_GROUP + e_idx
    combine = jnp.sum(jax.nn.one_hot(expert_id, N_EXPERTS, dtype=jnp.float32) * w_top[..., None], axis=1)
    combine = combine.astype(t.dtype)
    out = jnp.zeros_like(t)
    for g in range(N_GROUPS):
        sl = slice(g * EXPERTS_PER_GROUP, (g + 1) * EXPERTS_PER_GROUP)
        a = jnp.einsum('nd,edf->nef', t, w_exp_gate[sl])
        u = jnp.einsum('nd,edf->nef', t, w_exp_up[sl])
        hid = jax.nn.silu(a) * u * combine[:, sl, None]
        out = out + jnp.einsum('nef,efd->nd', hid, w_exp_down[sl])
    return out.reshape(b, L, d)


def trunk_layer(x, cond, s0_hg, s0_gla, n_chunks, lb, ada_w, ada_b, norm1_g, norm2_g, w_in, hg_norm_g,
                gla_wa_up, gla_ba, gla_norm_g, w_br_a, w_br_b, w_out,
                w_router_group, w_router_expert, w_exp_gate, w_exp_up, w_exp_down):
    mod = (jax.nn.silu(cond) @ ada_w + ada_b).reshape(-1, 1, 6 * D_MODEL)
    shift1, scale1, gate1, shift2, scale2, gate2 = jnp.split(mod, 6, axis=-1)
    h = rmsnorm(x, norm1_g) * (1.0 + scale1) + shift1
    mix, st_hg, st_gla = token_mixer(h, s0_hg, s0_gla, n_chunks, lb, w_in, hg_norm_g, gla_wa_up, gla_ba,
                                     gla_norm_g, w_br_a, w_br_b, w_out)
    x = x + gate1 * mix
    h2 = rmsnorm(x, norm2_g) * (1.0 + scale2) + shift2
    x = x + gate2 * hierarchical_moe(h2, w_router_group, w_router_expert, w_exp_gate, w_exp_up, w_exp_down)
    return x, st_hg, st_gla


def setup_inputs(seed: int = 0) -> dict:
    key = jax.random.key(seed)
    ks = jax.random.split(key, 26)
    nrm = lambda k, shape, scale: jax.random.normal(k, shape, jnp.float32) * scale
    D = D_MODEL
    return {
        "x_prompt": nrm(ks[0], (BATCH, SEQ, D), 1.0),
        "x_sample": nrm(ks[1], (DEC_BATCH, DEC_SEQ, D), 1.0),
        "state_hgrn": nrm(ks[2], (DEC_BATCH, DEPTH, 2, HG_H, HG_DK, HG_DV), 0.1),
        "state_gla": nrm(ks[3], (DEC_BATCH, DEPTH, 2, GLA_H, GLA_DK, GLA_DV), 0.1),
        "c": nrm(ks[4], (DEC_BATCH, D), 1.0),
        "c_ctx": nrm(ks[5], (D,), 1.0),
        "ada_w": nrm(ks[6], (DEPTH, D, 6 * D), D ** -0.5),
        "ada_b": nrm(ks[7], (DEPTH, 6 * D), 0.01),
        "norm1_g": 1.0 + nrm(ks[8], (DEPTH, D), 0.1),
        "norm2_g": 1.0 + nrm(ks[9], (DEPTH, D), 0.1),
        "w_in": nrm(ks[10], (DEPTH, D, IN_WIDTH), D ** -0.5),
        "hg_lb_param": nrm(ks[11], (DEPTH + 1, HG_KW), 1.0),
        "hg_norm_g": 1.0 + nrm(ks[12], (DEPTH, HG_DV), 0.1),
        "gla_wa_up": nrm(ks[13], (DEPTH, 2, GLA_RANK, GLA_KW), GLA_RANK ** -0.5),
        "gla_ba": 1.0 + nrm(ks[14], (DEPTH, 2, GLA_KW), 0.5),
        "gla_norm_g": 1.0 + nrm(ks[15], (DEPTH, GLA_DV), 0.1),
        "w_br_a": nrm(ks[16], (DEPTH, HG_VW, D), HG_VW ** -0.5),
        "w_br_b": nrm(ks[17], (DEPTH, GLA_VW, D), GLA_VW ** -0.5),
        "w_out": nrm(ks[18], (DEPTH, D, D), D ** -0.5),
        "w_router_group": nrm(ks[19], (DEPTH, D, N_GROUPS), D ** -0.5),
        "w_router_expert": nrm(ks[20], (DEPTH, N_GROUPS, D, EXPERTS_PER_GROUP), D ** -0.5),
        "w_exp_gate": nrm(ks[21], (DEPTH, N_EXPERTS, D, D_EXPERT), D ** -0.5),
        "w_exp_up": nrm(ks[22], (DEPTH, N_EXPERTS, D, D_EXPERT), D ** -0.5),
        "w_exp_down": nrm(ks[23], (DEPTH, N_EXPERTS, D_EXPERT, D), D_EXPERT ** -0.5),
        "final_norm_g": 1.0 + nrm(ks[24], (D,), 0.1),
    }


def reference(x_prompt, x_sample, state_hgrn, state_gla, c, c_ctx, ada_w, ada_b, norm1_g, norm2_g, w_in,
              hg_lb_param, hg_norm_g, gla_wa_up, gla_ba, gla_norm_g, w_br_a, w_br_b, w_out,
              w_router_group, w_router_expert, w_exp_gate, w_exp_up, w_exp_down, final_norm_g):
    bp, lp, _ = x_prompt.shape
    ls = x_sample.shape[1]
    ctx_chunks = lp // CHUNK
    rows = ls // GRID_W
    lb_all = jnp.cumsum(jax.nn.softmax(hg_lb_param.astype(jnp.float32), axis=0), axis=0)
    zero_hg = jnp.zeros((bp, 2, HG_H, HG_DK, HG_DV), jnp.float32)
    zero_gla = jnp.zeros((bp, 2, GLA_H, GLA_DK, GLA_DV), jnp.float32)
    hp, hs = x_prompt, x_sample
    new_hg, new_gla = [], []
    for l in range(DEPTH):
        lp_args = (lb_all[l], ada_w[l], ada_b[l], norm1_g[l], norm2_g[l], w_in[l], hg_norm_g[l], gla_wa_up[l],
                   gla_ba[l], gla_norm_g[l], w_br_a[l], w_br_b[l], w_out[l], w_router_group[l],
                   w_router_expert[l], w_exp_gate[l], w_exp_up[l], w_exp_down[l])
        hp, st_hg, st_gla = trunk_layer(hp, c_ctx, zero_hg, zero_gla, ctx_chunks, *lp_args)
        new_hg.append(st_hg)
        new_gla.append(st_gla)
        hs, _, _ = trunk_layer(hs, c, state_hgrn[:, l], state_gla[:, l], rows, *lp_args)
    y_prompt = rmsnorm(hp, final_norm_g)
    y_sample = rmsnorm(hs, final_norm_g)
    new_state_hgrn = jnp.stack(new_hg, axis=1)
    new_state_gla = jnp.stack(new_gla, axis=1)
    return (y_prompt, y_sample, new_state_hgrn, new_state_gla)
```

```python
import os
from contextlib import ExitStack
import numpy as np
import concourse.bass as bass
import concourse.mybir as mybir
from concourse.bass_utils import run_bass_kernel_spmd

F32 = mybir.dt.float32
BF16 = mybir.dt.bfloat16
AF = mybir.ActivationFunctionType
ALU = mybir.AluOpType
AX = mybir.AxisListType

D = 1024
TO = 1536
TOTH = 1024
TA = TO + TOTH
NT_O = TO // 128
NT_A = TA // 128
EPS = 1e-6
NEXP = 32


class Sched:
    def __init__(self, nc):
        self.nc = nc
        self.ops = []
        self.lw = {}
        self.rd = {}
        self.bar = set()

    def barrier(self):
        last = {}
        dmas = {}
        for i, op in enumerate(self.ops):
            last[op["eng"]] = i
            if op["dma"]:
                dmas.setdefault(op["eng"], []).append(i)
        b = set(last.values())
        for e, l in dmas.items():
            b.update(l[-8:])
        self.bar = b

    def add(self, eng, fn, r=(), w=(), dma=False):
        deps = set(self.bar)
        for k in r:
            if k in self.lw:
                deps.add(self.lw[k])
        for k in w:
            if k in self.lw:
                deps.add(self.lw[k])
            deps.update(self.rd.get(k, ()))
        i = len(self.ops)
        self.ops.append(dict(eng=eng, fn=fn, deps=deps, dma=dma, need=dma, sig=None, pre=None))
        for k in r:
            self.rd.setdefault(k, []).append(i)
        for k in w:
            self.lw[k] = i
            self.rd[k] = []
        return i

    def emit(self, stack):
        nc = self.nc
        ops = self.ops
        for op in ops:
            for d in op["deps"]:
                if ops[d]["dma"] or not (ops[d]["eng"] == "pe" and op["eng"] == "pe"):
                    ops[d]["need"] = True
        engs = ("pe", "act", "dve", "pool", "sp")
        esem = {e: stack.enter_context(nc.semaphore("c_" + e)) for e in engs}
        KD = 8
        dsem = {e: [stack.enter_context(nc.semaphore("d_%s%d" % (e, i))) for i in range(KD)]
                for e in ("sp", "pool", "act")}
        ecnt = {e: 0 for e in engs}
        dcnt = {e: 0 for e in dsem}
        dfinal = {}
        for op in ops:
            e = op["eng"]
            if op["dma"]:
                j = dcnt[e]
                dcnt[e] += 1
                s = dsem[e][j % KD]
                u = j // KD
                if u > 0:
                    op["pre"] = (s, 16 * u)
                op["sig"] = (s, 16 * (u + 1), 16)
                dfinal[id(s)] = (s, 16 * (u + 1))
            elif op["need"]:
                ecnt[e] += 1
                op["sig"] = (esem[e], ecnt[e], 1)

        def run(name, e):
            waited = {}

            def wait(s, v):
                if waited.get(id(s), 0) < v:
                    e.wait_ge(s, v)
                    waited[id(s)] = v

            for op in ops:
                if op["eng"] != name:
                    continue
                if op["pre"] is not None:
                    wait(*op["pre"])
                for d in sorted(op["deps"]):
                    dop = ops[d]
                    if dop["dma"] or not (dop["eng"] == "pe" and name == "pe"):
                        wait(dop["sig"][0], dop["sig"][1])
                ins = op["fn"](e)
                if op["sig"] is not None:
                    ins.then_inc(op["sig"][0], op["sig"][2])
            if name == "sp":
                for s, v in dfinal.values():
                    wait(s, v)

        with nc.Block() as block:
            @block.sync
            def _(e):
                run("sp", e)

            @block.tensor
            def _(e):
                run("pe", e)

            @block.scalar
            def _(e):
                run("act", e)

            @block.vector
            def _(e):
                run("dve", e)

            @block.gpsimd
            def _(e):
                run("pool", e)


def build(dbg=None):
    nc = bass.Bass("TRN2", target_bir_lowering=False)
    S = Sched(nc)
    dbg = dbg or {}

    def din(name, shape):
        return nc.dram_tensor(name, list(shape), F32, kind="ExternalInput").ap()

    def dout(name, shape, dt=F32):
        return nc.dram_tensor(name, list(shape), dt, kind="ExternalOutput").ap()

    x_all = din("x_all", [TA, D])
    condT_d = din("condT", [128, 16])
    ada_w_d = din("ada_w", [D, 6 * D])
    ada_bT_d = din("ada_bT", [128, 48])
    g1T_d = din("g1T", [128, 8])
    g2T_d = din("g2T", [128, 8])
    w_hg_d = din("w_hg", [8, D, 640])
    w_gla_d = din("w_gla", [4, D, 768])
    w_ga_d = din("w_ga", [D, 32])
    w_m_d = din("w_m", [16, D, 128])
    lbp_d = din("lbp", [128, 16])
    hgn_d = din("hgn", [128, 1])
    glan_d = din("glan", [128, 2])
    wa_up_d = din("wa_up", [2, 16, 512])
    nbaT_d = din("baT", [128, 8])
    w_bra_d = din("w_bra", [8, D, 128])
    w_brb_d = din("w_brb", [8, D, 128])
    w_out_d = din("w_out", [D, D])
    w_rt_d = din("w_rt", [D, 36])
    w_eg_d = din("w_eg", [NEXP, D, 256])
    w_eu_d = din("w_eu", [NEXP, D, 256])
    w_ed_d = din("w_ed", [NEXP, 256, D])
    fng_d = din("fng", [1, D])
    s0_hg_d = din("s0_hg", [2, 8, 128, 128])
    s0_gla_d = din("s0_gla", [2, 4, 128, 256])

    y_out = dout("y_out", [TO, D])
    st_hg = dout("st_hg", [2, 2, 8, 128, 128])
    st_gla = dout("st_gla", [2, 2, 4, 128, 256])
    dbg_out = {k: dout("dbg_" + k, shp, BF16 if k in ("yT",) else F32) for k, shp in dbg.items() if not k.startswith("_")}

    with ExitStack() as st:
        def sb(name, shape, dt=F32):
            return st.enter_context(nc.sbuf_tensor("s_" + name, list(shape), dt))

        psb = [st.enter_context(nc.psum_tensor("ps%d" % i, [128, 512], F32)) for i in range(8)]

        def PSK(b):
            return ("ps", b)

        ident_f = sb("ident_f", [128, 128])
        ident_b = sb("ident_b", [128, 128], BF16)
        ones_f = sb("ones_f", [128, 512], BF16)
        one_c = sb("one_c", [128, 1])
        mask32 = sb("mask32", [128, 1024], BF16)
        trimask = sb("trimask", [128, 512], BF16)
        S.add("pool", lambda e: e.memset(ident_f[:], 0.0), w=["ident_f"])
        S.add("pool", lambda e: e.affine_select(out=ident_f[:], in_=ident_f[:], pattern=[[-1, 128]],
                                                compare_op=ALU.not_equal, fill=1.0, base=0,
                                                channel_multiplier=1), r=["ident_f"], w=["ident_f"])
        S.add("dve", lambda e: e.tensor_copy(out=ident_b[:], in_=ident_f[:]), r=["ident_f"], w=["ident_b"])
        S.add("pool", lambda e: e.memset(ones_f[:], 1.0), w=["ones_f"])
        S.add("pool", lambda e: e.memset(one_c[:], 1.0), w=["one_c"])
        S.add("pool", lambda e: e.memset(mask32[:], 1.0), w=["mask32"])
        S.add("pool", lambda e: e.memset(mask32[:].rearrange("p (c j) -> p c j", j=32)[:, :, 0:1], 0.0),
              r=["mask32"], w=["mask32"])
        tmv = trimask[:].rearrange("p (a d t) -> p a d t", a=4, d=2)
        onv = ones_f[:].rearrange("p (a d t) -> p a d t", a=4, d=2)
        for half in range(2):
            pr = slice(half * 64, half * 64 + 64)
            S.add("pool", lambda e, pr=pr: e.affine_select(
                out=tmv[pr, :, 0, :], in_=onv[pr, :, 0, :], pattern=[[0, 4], [1, 64]],
                compare_op=ALU.is_ge, fill=0.0, base=0, channel_multiplier=-1),
                r=["ones_f"], w=["trimask"])
            S.add("pool", lambda e, pr=pr: e.affine_select(
                out=tmv[pr, :, 1, :], in_=onv[pr, :, 1, :], pattern=[[0, 4], [-1, 64]],
                compare_op=ALU.is_ge, fill=0.0, base=0, channel_multiplier=1),
                r=["ones_f"], w=["trimask"])

        condT = sb("condT", [128, 16])
        scT = sb("scT", [128, 16])
        ada_bT = sb("ada_bT", [128, 48])
        g1T = sb("g1T", [128, 8])
        g2T = sb("g2T", [128, 8])
        lbp = sb("lbp", [128, 16])
        hgn = sb("hgn", [128, 1])
        glan = sb("glan", [128, 2])
        baT = sb("baT_s", [128, 8])
        nbaT = sb("nbaT", [128, 8])
        modT = sb("modT", [128, 96])
        A1 = sb("A1", [128, 16])
        A2 = sb("A2", [128, 16])
        lb = sb("lb", [128, 8])
        oml = sb("oml", [128, 8])
        noml = sb("noml", [128, 8])
        epsb = sb("epsb", [128, 1])
        for t_, d_, nm in ((condT, condT_d, "condT"), (ada_bT, ada_bT_d, "ada_bT"), (g1T, g1T_d, "g1T"),
                           (g2T, g2T_d, "g2T"), (lbp, lbp_d, "lbp"), (hgn, hgn_d, "hgn"),
                           (glan, glan_d, "glan"), (baT, nbaT_d, "baT")):
            S.add("sp", lambda e, t_=t_, d_=d_: e.dma_start(out=t_[:], in_=d_), w=[nm], dma=True)
        S.add("pool", lambda e: e.memset(epsb[:], EPS), w=["epsb"])
        S.add("act", lambda e: e.activation(out=scT[:], in_=condT[:], func=AF.Silu), r=["condT"], w=["scT"])
        S.add("dve", lambda e: e.tensor_sub(out=lb[:], in0=lbp[:, 0:8], in1=lbp[:, 8:16]), r=["lbp"], w=["lb"])
        S.add("act", lambda e: e.activation(out=lb[:], in_=lb[:], func=AF.Sigmoid), r=["lb"], w=["lb"])
        S.add("dve", lambda e: e.tensor_scalar(out=oml[:], in0=lb[:], scalar1=-1.0, scalar2=1.0,
                                               op0=ALU.mult, op1=ALU.add), r=["lb"], w=["oml"])
        S.add("dve", lambda e: e.tensor_single_scalar(out=noml[:], in_=oml[:], scalar=-1.0, op=ALU.mult),
              r=["oml"], w=["noml"])
        S.add("dve", lambda e: e.tensor_single_scalar(out=nbaT[:], in_=baT[:], scalar=-1.0, op=ALU.mult),
              r=["baT"], w=["nbaT"])

        def pipeline(stages, n, skews):
            for i in range(n + max(skews)):
                for stg, sk in zip(stages, skews):
                    if 0 <= i - sk < n:
                        stg(i - sk)

        hT = sb("hT", [128, 8, TA], BF16)
        ydram = nc.dram_tensor("yscr", [TO, 2 * D], BF16).ap()
        ssq = sb("ssq", [128, NT_A])
        rstd = sb("rstd", [128, NT_A])
        mv = modT[:].rearrange("p (j c) -> p j c", c=2)

        def modkey(j):
            return "modT0" if j < 16 else "modT1"

        def modcol(j0, dc, c):
            return modT[:, (j0 + dc) * 2 + c:(j0 + dc) * 2 + c + 1]

        def cond_of_tile(ti):
            return 1 if 8 <= ti < 12 else 0

        with ExitStack() as st01:
            adaw = [st01.enter_context(nc.sbuf_tensor("adaw%d" % i, [128, 8, 512], F32)) for i in range(3)]
            scTb = st01.enter_context(nc.sbuf_tensor("scTb", [128, 16], BF16))
            modrow = st01.enter_context(nc.sbuf_tensor("modrow", [2, 6 * D], F32))
            xts = [st01.enter_context(nc.sbuf_tensor("xt%d" % i, [128, D], F32)) for i in range(3)]
            xns = [st01.enter_context(nc.sbuf_tensor("xn%d" % i, [128, D], BF16)) for i in range(3)]
            adv = ada_w_d.rearrange("(dc p) n -> p dc n", p=128)
            S.add("dve", lambda e: e.tensor_copy(out=scTb[:], in_=scT[:]), r=["scT"], w=["scTb"])

            def mod_block(blk):
                a = adaw[blk % 3]
                S.add("sp", lambda e: e.dma_start(out=a[:], in_=adv[:, :, blk * 512:(blk + 1) * 512]),
                      w=[("adaw", blk % 3)], dma=True)

                def f(e):
                    ins = None
                    for dc in range(8):
                        ins = e.matmul(psb[0][0:2, 0:512], lhsT=scT[:, dc * 2:dc * 2 + 2], rhs=a[:, dc, :],
                                       start=(dc == 0), stop=(dc == 7))
                    return ins
                S.add("pe", f, r=[("adaw", blk % 3), "scT"], w=[PSK(0)])
                S.add("dve", lambda e: e.tensor_copy(out=modrow[0:2, blk * 512:(blk + 1) * 512], in_=psb[0][0:2, 0:512]),
                      r=[PSK(0)], w=[("modrow", blk)])

                def ft(e):
                    ins = None
                    for j4 in range(4):
                        jc = blk * 4 + j4
                        ins = e.transpose(psb[7][:, jc * 2:jc * 2 + 2], modrow[0:2, jc * 128:(jc + 1) * 128], ident_f[0:2, 0:2])
                    return ins
                S.add("pe", ft, r=[("modrow", blk), "ident_f"], w=[PSK(7)])

            def mod_evac(j_lo, j_hi, key):
                for c in range(2):
                    S.add("dve", lambda e, c=c: e.tensor_tensor(
                        out=mv[:, j_lo:j_hi, c], in0=psb[7][:, 0:96].rearrange("p (j c) -> p j c", c=2)[:, j_lo:j_hi, c],
                        in1=ada_bT[:, j_lo:j_hi], op=ALU.add), r=[PSK(7), "ada_bT"], w=[key])

            for blk in range(4):
                mod_block(blk)
            mod_evac(0, 16, "modT0")
            Av = A1[:].rearrange("p (j c) -> p j c", c=2)
            for c in range(2):
                S.add("dve", lambda e, c=c: e.scalar_tensor_tensor(
                    out=Av[:, :, c], in0=mv[:, 8:16, c], scalar=1.0, in1=g1T[:],
                    op0=ALU.add, op1=ALU.mult), r=["modT0", "g1T"], w=["A1"])

            def p1_stage1(ti):
                xt = xts[ti % 3]
                xn = xns[ti % 3]
                S.add("sp", lambda e: e.dma_start(out=xt[:], in_=x_all[ti * 128:(ti + 1) * 128, :]),
                      w=[("xt", ti % 3)], dma=True)
                S.add("act", lambda e: e.activation(out=xn[:], in_=xt[:], func=AF.Square, accum_out=ssq[:, ti:ti + 1]),
                      r=[("xt", ti % 3)], w=[("xn", ti % 3), ("ssq", ti)])
                S.add("act", lambda e: e.activation(out=rstd[:, ti:ti + 1], in_=ssq[:, ti:ti + 1], func=AF.Sqrt,
                                                    bias=epsb[:], scale=1.0 / D),
                      r=[("ssq", ti), "epsb"], w=[("rstd", ti)])
                S.add("dve", lambda e: e.reciprocal(out=rstd[:, ti:ti + 1], in_=rstd[:, ti:ti + 1]),
                      r=[("rstd", ti)], w=[("rstd", ti)])
                S.add("dve", lambda e: e.tensor_scalar(out=xn[:], in0=xt[:], scalar1=rstd[:, ti:ti + 1], scalar2=None, op0=ALU.mult),
                      r=[("xt", ti % 3), ("rstd", ti)], w=[("xn", ti % 3)])

            def p1_stage2(ti):
                xn = xns[ti % 3]
                bank = 1 + (ti % 2)
                c = cond_of_tile(ti)
                pbv = psb[bank][:].bitcast(BF16)

                def ftr(e):
                    ins = None
                    for dc in range(8):
                        ins = e.transpose(pbv[:, dc * 128:(dc + 1) * 128], xn[:, dc * 128:(dc + 1) * 128], ident_b[:])
                    return ins
                S.add("pe", ftr, r=[("xn", ti % 3), "ident_b"], w=[PSK(bank)])
                for dc in range(8):
                    a_ap = A1[:, dc * 2 + c:dc * 2 + c + 1]
                    s_ap = modcol(0, dc, c)
                    o_ap = hT[:, dc, ti * 128:(ti + 1) * 128]
                    i_ap = pbv[:, dc * 128:(dc + 1) * 128]
                    if dc % 8 < 5:
                        S.add("dve", lambda e, o_ap=o_ap, i_ap=i_ap, a_ap=a_ap, s_ap=s_ap: e.tensor_scalar(
                            out=o_ap, in0=i_ap, scalar1=a_ap, scalar2=s_ap, op0=ALU.mult, op1=ALU.add),
                            r=[PSK(bank), "A1", "modT0"], w=[("hT", ti)])
                    else:
                        S.add("act", lambda e, o_ap=o_ap, i_ap=i_ap, a_ap=a_ap, s_ap=s_ap: e.activation(
                            out=o_ap, in_=i_ap, func=AF.Identity, bias=s_ap, scale=a_ap),
                            r=[PSK(bank), "A1", "modT0"], w=[("hT", ti)])

            def p1_mod(ti):
                if ti < 8:
                    mod_block(4 + ti)
                if ti == 8:
                    mod_evac(16, 48, "modT1")
                    Av2 = A2[:].rearrange("p (j c) -> p j c", c=2)
                    for c in range(2):
                        S.add("dve", lambda e, c=c: e.scalar_tensor_tensor(
                            out=Av2[:, :, c], in0=mv[:, 32:40, c], scalar=1.0, in1=g2T[:],
                            op0=ALU.add, op1=ALU.mult), r=["modT1", "g2T"], w=["A2"])
            pipeline([p1_stage1, p1_mod, p1_stage2], NT_A, [0, 0, 2])

        st2 = st.enter_context(ExitStack())
        S.barrier()

        def sb2(name, shape, dt=F32):
            return st2.enter_context(nc.sbuf_tensor("s_" + name, list(shape), dt))

        W = sb2("W", [128, 8, 768], BF16)
        NSET = 4
        B1 = sb2("B1", [128, NSET, 512])
        B2 = sb2("B2", [128, NSET, 512])
        B3 = sb2("B3", [128, NSET, 512])
        q_bf = sb2("q_bf", [128, TO], BF16)
        k_bf = sb2("k_bf", [128, 2048], BF16)
        k_o = sb2("k_o", [128, TOTH], BF16)
        qI = [sb2("qI%d" % d, [128, TO], BF16) for d in range(2)]
        qX2 = [sb2("qX2%d" % d, [128, TO], BF16) for d in range(2)]
        kX1 = [sb2("kX1%d" % d, [128, TO], BF16) for d in range(2)]
        kX2 = [sb2("kX2%d" % d, [128, TO], BF16) for d in range(2)]
        kSb = sb2("kSb", [128, NSET, 512], BF16)
        kS_tm = [sb2("kStm0", [128, NT_O, 128], BF16), sb2("kStm1", [128, NT_A, 128], BF16)]
        Dd = [sb2("Dd%d" % d, [128, 40]) for d in range(2)]
        Eo = sb2("Eo", [128, 8 * NSET, 2])
        v_tm = sb2("v_tm", [128, NT_A, 256], BF16)
        G_tm = sb2("G_tm", [128, NT_O, 256], BF16)
        S32 = sb2("S32", [128, 6, 256])
        S_bf = [sb2("S_bf0", [128, 4, 256], BF16), sb2("S_bf1", [128, 24, 256], BF16)]
        scS = sb2("scS", [128, 512], BF16)
        y_tm = [sb2("y_tm%d" % i, [128, 256], BF16) for i in range(4)]
        ssq2 = sb2("ssq2", [128, 160])
        rs2 = sb2("rs2", [128, 160])
        gaT = sb2("gaT", [64, TA], BF16)
        wa_bf = sb2("wa_bf", [64, 512], BF16)
        junk2 = sb2("junk2", [128, 256], BF16)

        for d in range(2):
            for i_, (arr, nm) in enumerate(((qX2[d], "qX2"), (kX1[d], "kX1"), (kX2[d], "kX2"))):
                S.add("dve" if (i_ + d) % 2 == 0 else "pool", lambda e, arr=arr: e.memset(arr[:], 0.0), w=[(nm, d)])
        S.add("pool", lambda e: e.memset(ssq2[:], 0.0), w=["ssq2"])
        for d in range(2):
            S.add("pool", lambda e, d=d: e.dma_start(out=wa_bf[d * 32:d * 32 + 16, :], in_=wa_up_d[d]),
                  w=[("wa_bf", d)], dma=True)

        GEO = [dict(fh=slice(0, 32), sh=slice(32, 64), pf=31, pse=63),
               dict(fh=slice(32, 64), sh=slice(0, 32), pf=32, pse=0)]
        cm_ctr = [0]
        tm_ctr = [0]
        st_ctr = [0]

        def c64(ap):
            return ap.rearrange("p (c j) -> p c j", j=64)

        def cmaj(col0, tok0, ntok, consume, M=128, wkey="Wc", wsrc=None, pbase=0):
            bank = cm_ctr[0] % 2
            cm_ctr[0] += 1
            src = W if wsrc is None else wsrc

            def f(e):
                ins = None
                for dc in range(8):
                    ins = e.matmul(psb[bank][pbase:pbase + M, 0:ntok], lhsT=src[:, dc, col0:col0 + M],
                                   rhs=hT[:, dc, tok0:tok0 + ntok], start=(dc == 0), stop=(dc == 7))
                return ins
            tiles = range(tok0 // 128, (tok0 + ntok) // 128)
            S.add("pe", f, r=[wkey] + [("hT", t) for t in tiles], w=[PSK(bank)])
            consume(psb[bank][pbase:pbase + M, 0:ntok], PSK(bank))

        def gating(d, n, tokbase, gbase, k_ap, kkey, se, own, par):
            g = GEO[d]
            fh, sh, pf, pse = g["fh"], g["sh"], g["pf"], g["pse"]
            ncnk = n // 64
            b1, b2, b3 = B1[:, par, 0:n], B2[:, par, 0:n], B3[:, par, 0:n]
            k1, k2, k3 = ("B1", par), ("B2", par), ("B3", par)
            c32v = c64(b3)
            e32v = c64(b1)
            iev = c64(b2)
            kv = c64(k_ap)
            Dv = Dd[d][:, gbase:gbase + ncnk]
            Dv3 = Dv.rearrange("p (c o) -> p c o", o=1)
            bc = lambda ap: ap.to_broadcast([128, ncnk, 32])
            S.add("act", lambda e: e.activation(out=b2, in_=b3, func=AF.Exp, scale=-se), r=[k3], w=[k2])
            if own:
                S.add("act", lambda e: e.activation(out=b1, in_=b3, func=AF.Exp, scale=se), r=[k3], w=[k1])
                EF = e32v[:, :, pf:pf + 1]
                ES = e32v[:, :, pse:pse + 1]
                ekey = k1
            else:
                eo = Eo[:, par * 8:par * 8 + ncnk, :]
                S.add("act", lambda e: e.activation(out=eo[:, :, 0:1], in_=c32v[:, :, pf:pf + 1], func=AF.Exp, scale=se),
                      r=[k3], w=[("Eo", par)])
                S.add("act", lambda e: e.activation(out=eo[:, :, 1:2], in_=c32v[:, :, pse:pse + 1], func=AF.Exp, scale=se),
                      r=[k3], w=[("Eo", par)])
                EF = eo[:, :, 0:1]
                ES = eo[:, :, 1:2]
                ekey = ("Eo", par)
            S.add("dve", lambda e: e.tensor_tensor(out=Dv3, in0=EF, in1=ES, op=ALU.mult), r=[ekey], w=[("Dd", d)])
            if own:
                qv = c64(q_bf[:, tokbase:tokbase + n])
                for (eng, dst, nm, a_, b_, hs) in (
                        ("pool", kX1[d], "kX1", kv, iev, fh), ("pool", kX2[d], "kX2", kv, iev, sh),
                        ("dve", qX2[d], "qX2", qv, e32v, sh), ("dve", qI[d], "qI", qv, e32v, fh)):
                    dv_ = c64(dst[:, tokbase:tokbase + n])
                    S.add(eng, lambda e, dv_=dv_, a_=a_, b_=b_, hs=hs: e.tensor_tensor(
                        out=dv_[:, :, hs], in0=a_[:, :, hs], in1=b_[:, :, hs], op=ALU.mult),
                        r=[kkey, "q_bf", k1, k2], w=[(nm, d)])
            S.add("pool", lambda e: e.tensor_tensor(out=iev[:, :, fh], in0=iev[:, :, fh], in1=bc(Dv3), op=ALU.mult),
                  r=[k2, ("Dd", d)], w=[k2])
            S.add("pool", lambda e: e.tensor_tensor(out=iev[:, :, sh], in0=iev[:, :, sh], in1=bc(ES), op=ALU.mult),
                  r=[k2, ekey], w=[k2])
            if own:
                S.add("dve", lambda e: e.tensor_tensor(out=e32v[:, :, sh], in0=e32v[:, :, sh], in1=bc(EF), op=ALU.mult),
                      r=[k1], w=[k1])
                dv_ = c64(qI[d][:, tokbase:tokbase + n])
                S.add("dve", lambda e: e.tensor_tensor(out=dv_[:, :, sh], in0=qv[:, :, sh], in1=e32v[:, :, sh], op=ALU.mult),
                      r=["q_bf", k1], w=[("qI", d)])
            ksb = kSb[:, par, 0:n]
            S.add("dve", lambda e: e.tensor_tensor(out=ksb, in0=k_ap, in1=b2, op=ALU.mult), r=[kkey, k2], w=[("kSb", par)])

        def ks_transpose(d, n, tokbase, par):
            ksb = kSb[:, par, 0:n]
            nt = n // 128
            tile0 = tokbase // 128
            pbv = psb[4][:].bitcast(BF16)

            def ftr(e):
                ins = None
                for j in range(nt):
                    ins = e.transpose(pbv[:, j * 128:(j + 1) * 128], ksb[:, j * 128:(j + 1) * 128], ident_b[:])
                return ins
            S.add("pe", ftr, r=[("kSb", par), "ident_b"], w=[PSK(4)])
            S.add("act", lambda e: e.copy(out=kS_tm[d][:, tile0:tile0 + nt, :].rearrange("p a b -> p (a b)"),
                                          in_=pbv[:, 0:nt * 128]), r=[PSK(4)], w=[("kStm", d)])

        def seg_scan(d, n, par):
            b2, b3 = B2[:, par, 0:n], B3[:, par, 0:n]
            if d == 0:
                S.add("dve", lambda e: e.tensor_tensor_scan(out=b3, data0=mask32[:, 0:n], data1=b2,
                                                            initial=0.0, op0=ALU.mult, op1=ALU.add),
                      r=[("B2", par), "mask32"], w=[("B3", par)])
            else:
                S.add("dve", lambda e: e.tensor_tensor_scan(out=b3[:, ::-1], data0=mask32[:, 0:n], data1=b2[:, ::-1],
                                                            initial=0.0, op0=ALU.mult, op1=ALU.add),
                      r=[("B2", par), "mask32"], w=[("B3", par)])

        def tok_of_chunk(g):
            return g * 64

        zero_state = set()

        def state_step(dv, ci, d, g, first_zero, copy_to=None):
            ti, hf = g // 2, g % 2
            pr = slice(hf * 64, hf * 64 + 64)
            bank = 5 + st_ctr[0] % 2
            st_ctr[0] += 1
            pS = psb[bank][:, 0:dv]
            pkey = PSK(bank)
            S.add("pe", lambda e: e.matmul(pS, lhsT=kS_tm[d][pr, ti, :], rhs=v_tm[pr, ti, 0:dv], start=True, stop=True),
                  r=[("kStm", d), ("v_tm", ti)], w=[pkey])
            if copy_to is not None:
                if first_zero:
                    zero_state.add((d, g))
                else:
                    S.add("act", lambda e: e.copy(out=copy_to[0], in_=S32[:, ci, 0:dv]), r=[("S32", ci)], w=[copy_to[1]])
            if first_zero:
                S.add("dve", lambda e: e.tensor_copy(out=S32[:, ci, 0:dv], in_=pS), r=[pkey], w=[("S32", ci)])
            else:
                S.add("dve", lambda e: e.scalar_tensor_tensor(
                    out=S32[:, ci, 0:dv], in0=S32[:, ci, 0:dv], scalar=Dd[d][:, g:g + 1], in1=pS,
                    op0=ALU.mult, op1=ALU.add), r=[pkey, ("S32", ci), ("Dd", d)], w=[("S32", ci)])

        def sweep_dir1(kind, h, dv):
            s0d = s0_hg_d if kind == "hg" else s0_gla_d
            std = st_hg if kind == "hg" else st_gla
            zero_state.clear()
            ch1 = [dict(ci=3, seq=list(range(39, 23, -1)) + list(range(15, -1, -1)), init=1, prompt=None),
                   dict(ci=4, seq=list(range(19, 15, -1)), init=None, prompt=0),
                   dict(ci=5, seq=list(range(23, 19, -1)), init=None, prompt=1)]
            S.add("sp", lambda e: e.dma_start(out=S32[:, 3, 0:dv], in_=s0d[1, h]), w=[("S32", 3)], dma=True)
            S.add("sp", lambda e: e.dma_start(out=S32[:, 0, 0:dv], in_=s0d[0, h]), w=[("S32", 0)], dma=True)
            for step in range(32):
                for ch in ch1:
                    if step >= len(ch["seq"]):
                        continue
                    g = ch["seq"][step]
                    ci = ch["ci"]
                    fz = ch["init"] is None and step == 0
                    cp = (S_bf[1][:, g, 0:dv], ("S_bf", 1, g)) if g < 24 else None
                    state_step(dv, ci, 1, g, fz, cp)
                    if ch["prompt"] is not None and step == len(ch["seq"]) - 1:
                        S.add("sp", lambda e, ci=ci, ch=ch: e.dma_start(out=std[ch["prompt"], 1, h], in_=S32[:, ci, 0:dv]),
                              r=[("S32", ci)], dma=True)
                if step % 4 == 3:
                    yield

        def dir0_and_output(kind, h, dv, ychunk0):
            std = st_hg if kind == "hg" else st_gla
            pscb = psb[7]
            pbv = psb[4][:].bitcast(BF16)
            nvc = dv // 128

            def stageA(ti):
                ci = 0 if ti < 8 else (1 if ti < 10 else 2)
                for hf in range(2):
                    g = 2 * ti + hf
                    fz = ci > 0 and g in (16, 20)
                    rs = g % 4
                    state_step(dv, ci, 0, g, fz, (S_bf[0][:, rs, 0:dv], ("S_bf", 0, rs)))
                if ti in (9, 11):
                    S.add("sp", lambda e, ci=ci: e.dma_start(out=std[ci - 1, 0, h], in_=S32[:, ci, 0:dv]),
                          r=[("S32", ci)], dma=True)
                slot = ti % 4

                def fsc(e):
                    ins = None
                    for hf in range(2):
                        g = 2 * ti + hf
                        tk = slice(g * 64, g * 64 + 64)
                        pr = slice(hf * 64, hf * 64 + 64)
                        for d in range(2):
                            o_ap = pscb[pr, slot * 128 + d * 64:slot * 128 + d * 64 + 64]
                            e.matmul(o_ap, lhsT=kX1[d][:, tk], rhs=qI[d][:, tk], start=True, stop=False)
                            ins = e.matmul(o_ap, lhsT=kX2[d][:, tk], rhs=qX2[d][:, tk], start=False, stop=True)
                    return ins
                S.add("pe", fsc, r=[("kX1", 0), ("kX1", 1), ("kX2", 0), ("kX2", 1), ("qI", 0), ("qI", 1),
                                    ("qX2", 0), ("qX2", 1)], w=[PSK(7)])
                S.add("dve", lambda e: e.tensor_tensor(
                    out=scS[:, slot * 128:(slot + 1) * 128], in0=pscb[:, slot * 128:(slot + 1) * 128],
                    in1=trimask[:, slot * 128:(slot + 1) * 128], op=ALU.mult),
                    r=[PSK(7), "trimask"], w=[("scS", slot)])

            def stageB(ti):
                slot = ti % 4
                ob = 2 + (ti % 2)
                po = psb[ob][:, 0:dv]

                def fo(e):
                    ins = None
                    for hf in range(2):
                        g = 2 * ti + hf
                        tk = slice(g * 64, g * 64 + 64)
                        pr = slice(hf * 64, hf * 64 + 64)
                        mms = []
                        if (0, g) not in zero_state:
                            mms.append((qI[0][:, tk], S_bf[0][:, g % 4, 0:dv]))
                        if (1, g) not in zero_state:
                            mms.append((qI[1][:, tk], S_bf[1][:, g, 0:dv]))
                        for d in range(2):
                            mms.append((scS[pr, slot * 128 + d * 64:slot * 128 + d * 64 + 64], v_tm[pr, ti, 0:dv]))
                        for i, (l, r_) in enumerate(mms):
                            ins = e.matmul(psb[ob][pr, 0:dv], lhsT=l, rhs=r_, start=(i == 0), stop=(i == len(mms) - 1))
                    return ins
                S.add("pe", fo, r=[("qI", 0), ("qI", 1), ("scS", slot), ("v_tm", ti)] +
                      [("S_bf", 0, (2 * ti + hf) % 4) for hf in range(2)] +
                      [("S_bf", 1, 2 * ti + hf) for hf in range(2)], w=[PSK(ob)])
                si = (ychunk0 if kind == "hg" else 8 + h) * 12 + ti
                sc_ = ssq2[:, si:si + 1]
                rc_ = rs2[:, si:si + 1]
                S.add("act", lambda e: e.activation(out=junk2[:, 0:dv], in_=po, func=AF.Square, accum_out=sc_),
                      r=[PSK(ob)], w=["junk2", ("ssq2", ti % 2)])
                S.add("act", lambda e: e.activation(out=rc_, in_=sc_, func=AF.Sqrt, bias=epsb[:], scale=1.0 / dv),
                      r=[("ssq2", ti % 2), "epsb"], w=[("rs2", ti % 2)])
                S.add("dve", lambda e: e.reciprocal(out=rc_, in_=rc_), r=[("rs2", ti % 2)], w=[("rs2", ti % 2)])
                yt = y_tm[ti % 4]
                S.add("dve", lambda e: e.scalar_tensor_tensor(
                    out=yt[:, 0:dv], in0=po, scalar=rc_, in1=G_tm[:, ti, 0:dv], op0=ALU.mult, op1=ALU.mult),
                    r=[PSK(ob), ("rs2", ti % 2), ("G_tm", ti)], w=[("y_tm", ti % 4)])

            def stageC(ti):
                yt = y_tm[ti % 4]
                S.add("sp", lambda e: e.dma_start(out=ydram[ti * 128:(ti + 1) * 128, ychunk0 * 128:ychunk0 * 128 + dv], in_=yt[:, 0:dv]),
                      r=[("y_tm", ti % 4)], dma=True)

            for i in range(NT_O + 2):
                if i < NT_O:
                    stageA(i)
                if 0 <= i - 1 < NT_O:
                    stageB(i - 1)
                if 0 <= i - 2 < NT_O:
                    stageC(i - 2)

        def tmaj_pass(col_v, ncol_v, col_g, ncol_g):
            for ti in range(NT_A):
                own = ti < NT_O
                bank = 2 + tm_ctr[0] % 2
                tm_ctr[0] += 1
                ncol = ncol_v + (ncol_g if own else 0)

                def f(e, ti=ti, bank=bank, ncol=ncol):
                    ins = None
                    for dc in range(8):
                        ins = e.matmul(psb[bank][:, 0:ncol], lhsT=hT[:, dc, ti * 128:(ti + 1) * 128],
                                       rhs=W[:, dc, col_v:col_v + ncol], start=(dc == 0), stop=(dc == 7))
                    return ins
                S.add("pe", f, r=["Wt", ("hT", ti)], w=[PSK(bank)])
                S.add("act", lambda e, ti=ti, bank=bank: e.copy(out=v_tm[:, ti, 0:ncol_v], in_=psb[bank][:, 0:ncol_v]),
                      r=[PSK(bank)], w=[("v_tm", ti)])
                if own:
                    S.add("act", lambda e, ti=ti, bank=bank: e.activation(
                        out=G_tm[:, ti, 0:ncol_g], in_=psb[bank][:, ncol_v:ncol_v + ncol_g], func=AF.Silu),
                        r=[PSK(bank)], w=[("G_tm", ti)])

        PASSES = ([(1, TO + i * 512, 512, 24 + i * 8, False) for i in range(2)] +
                  [(1, i * 512, 512, i * 8, True) for i in range(3)] +
                  [(0, i * 512, 512, i * 8, True) for i in range(3)])
        N_D1 = 5

        def run_passes(p0, p1, p2_, sweep):
            n = len(PASSES)
            p0(0)
            sw = None
            for i in range(n + 2):
                if i + 1 < n:
                    p0(i + 1)
                if i < n:
                    p1(i)
                if 0 <= i - 1 < n:
                    p2_(i - 1)
                if 0 <= i - 2 < n:
                    d, tok0, nn, gb, own = PASSES[i - 2]
                    ks_transpose(d, nn, tok0, (i - 2) % NSET)
                    if i - 2 == N_D1 - 1:
                        sw = sweep()
                if sw is not None:
                    for _ in range(2):
                        next(sw, None)
            if sw is not None:
                for _ in sw:
                    pass

        def hg_head(h):
            wv = w_hg_d[h].rearrange("(dc p) n -> p dc n", p=128)
            S.add("pool", lambda e: e.dma_start(out=W[:, :, 0:384], in_=wv[:, :, 0:384]), w=["Wc"], dma=True)
            S.add("pool", lambda e: e.dma_start(out=W[:, :, 384:640], in_=wv[:, :, 384:640]), w=["Wt"], dma=True)
            for gi in range(3):
                cmaj(0, gi * 512, 512, lambda ps, pk, gi=gi: S.add(
                    "act", lambda e: e.activation(out=q_bf[:, gi * 512:(gi + 1) * 512], in_=ps, func=AF.Copy, scale=128 ** -0.5),
                    r=[pk], w=["q_bf"]))
            tmaj_pass(384, 128, 512, 128)

            pbank = {}

            def p0(pi):
                d, tok0, n, gb, own = PASSES[pi]
                col = 128 if d == 0 else 256
                cmaj(col, tok0, 512, lambda ps, pk: pbank.__setitem__(pi, (ps, pk)))

            def p1(pi):
                d, tok0, n, gb, own = PASSES[pi]
                par = pi % NSET
                ps, pk = pbank[pi]
                b1, b2, b3 = B1[:, par, :], B2[:, par, :], B3[:, par, :]
                k1, k2, k3 = ("B1", par), ("B2", par), ("B3", par)
                S.add("act", lambda e: e.activation(out=b1, in_=ps, func=AF.Exp, scale=-1.0), r=[pk], w=[k1])
                S.add("act", lambda e: e.activation(out=b3, in_=b1, func=AF.Ln, bias=one_c[:], scale=1.0), r=[k1, "one_c"], w=[k3])
                S.add("act", lambda e: e.activation(out=b2, in_=b1, func=AF.Ln, bias=one_c[:], scale=lb[:, h:h + 1]),
                      r=[k1, "one_c", "lb"], w=[k2])
                S.add("dve", lambda e: e.tensor_sub(out=b2, in0=b2, in1=b3), r=[k2, k3], w=[k2])
                S.add("act", lambda e: e.activation(out=b1, in_=b3, func=AF.Exp, scale=-1.0), r=[k3], w=[k1])
                k_ap = k_bf[:, par * 512:(par + 1) * 512]
                S.add("pool", lambda e: e.tensor_scalar(out=k_ap, in0=b1, scalar1=-1.0,
                                                        scalar2=noml[:, h:h + 1], op0=ALU.add, op1=ALU.mult),
                      r=[k1, "noml"], w=[("k_src", par)])
                seg_scan(d, n, par)

            def p2_(pi):
                d, tok0, n, gb, own = PASSES[pi]
                par = pi % NSET
                gating(d, n, tok0, gb, k_bf[:, par * 512:(par + 1) * 512], ("k_src", par), 1.0, own, par)
            run_passes(p0, p1, p2_, lambda: sweep_dir1("hg", h, 128))
            dir0_and_output("hg", h, 128, h)

        def gla_prep():
            wga = sb2("wga", [128, 8, 32], BF16)
            S.add("pool", lambda e: e.dma_start(out=wga[:], in_=w_ga_d.rearrange("(dc p) n -> p dc n", p=128)),
                  w=["wga"], dma=True)
            for d in range(2):
                ntok = TO if d == 0 else TA
                for gi in range(ntok // 512):
                    cmaj(d * 16, gi * 512, 512, lambda ps, pk, gi=gi, d=d: S.add(
                        "act", lambda e: e.copy(out=gaT[d * 32:d * 32 + 16, gi * 512:(gi + 1) * 512], in_=ps),
                        r=[pk], w=[("gaT", d)]), M=16, wkey="wga", wsrc=wga, pbase=d * 32)

        def gla_head(h):
            wv = w_gla_d[h].rearrange("(dc p) n -> p dc n", p=128)
            S.add("pool", lambda e: e.dma_start(out=W[:, :, 0:256], in_=wv[:, :, 0:256]), w=["Wc"], dma=True)
            S.add("pool", lambda e: e.dma_start(out=W[:, :, 256:768], in_=wv[:, :, 256:768]), w=["Wt"], dma=True)
            for gi in range(3):
                cmaj(0, gi * 512, 512, lambda ps, pk, gi=gi: S.add(
                    "act", lambda e: e.activation(out=q_bf[:, gi * 512:(gi + 1) * 512], in_=ps, func=AF.Copy, scale=128 ** -0.5),
                    r=[pk], w=["q_bf"]))
            for gi in range(5):
                dst = k_bf[:, gi * 512:(gi + 1) * 512] if gi < 3 else k_o[:, (gi - 3) * 512:(gi - 2) * 512]
                cmaj(128, gi * 512, 512, lambda ps, pk, dst=dst: S.add(
                    "act", lambda e: e.copy(out=dst, in_=ps), r=[pk], w=["k_gla"]))
            tmaj_pass(256, 256, 512, 256)

            pbank = {}

            def p0(pi):
                d, tok0, n, gb, own = PASSES[pi]
                bank = cm_ctr[0] % 2
                cm_ctr[0] += 1
                S.add("pe", lambda e: e.matmul(
                    psb[bank][:, 0:512], lhsT=wa_bf[d * 32:d * 32 + 16, h * 128:(h + 1) * 128],
                    rhs=gaT[d * 32:d * 32 + 16, tok0:tok0 + 512], start=True, stop=True),
                    r=[("wa_bf", d), ("gaT", d)], w=[PSK(bank)])
                pbank[pi] = bank

            def p1(pi):
                d, tok0, n, gb, own = PASSES[pi]
                par = pi % NSET
                bank = pbank[pi]
                S.add("act", lambda e: e.activation(
                    out=B1[:, par, :], in_=psb[bank][:, 0:512], func=AF.Exp,
                    bias=nbaT[:, d * 4 + h:d * 4 + h + 1], scale=-1.0),
                    r=[PSK(bank), "nbaT"], w=[("B1", par)])
                S.add("act", lambda e: e.activation(out=B2[:, par, :], in_=B1[:, par, :], func=AF.Ln, bias=one_c[:], scale=1.0),
                      r=[("B1", par), "one_c"], w=[("B2", par)])
                seg_scan(d, n, par)

            def p2_(pi):
                d, tok0, n, gb, own = PASSES[pi]
                par = pi % NSET
                k_ap = k_bf[:, tok0:tok0 + n] if own else k_o[:, tok0 - TO:tok0 - TO + n]
                gating(d, n, tok0, gb, k_ap, "k_gla", -1.0 / 16.0, own, par)
            run_passes(p0, p1, p2_, lambda: sweep_dir1("gla", h, 256))
            dir0_and_output("gla", h, 256, 8 + 2 * h)

        NH_HG = dbg.get("_nh_hg", 8)
        NH_GLA = dbg.get("_nh_gla", 4)
        for h in range(NH_HG):
            hg_head(h)
        if NH_GLA:
            gla_prep()
        for h in range(NH_GLA):
            gla_head(h)

        if dbg.get("_p2only"):
            if "modT" in dbg:
                S.add("sp", lambda e: e.dma_start(out=dbg_out["modT"], in_=modT[:]), r=["modT0", "modT1"], dma=True)
            S.emit(st)
            return nc
        st2.close()
        S.barrier()
        st3 = st.enter_context(ExitStack())

        def sb3(name, shape, dt=F32):
            return st3.enter_context(nc.sbuf_tensor("s_" + name, list(shape), dt))

        mergedT = sb3("mergedT", [128, 8, TO], BF16)
        yT = sb3("yT", [128, 16, TO], BF16)
        with ExitStack() as st3a:
            def sb3a(name, shape, dt=F32):
                return st3a.enter_context(nc.sbuf_tensor("s_" + name, list(shape), dt))
            stgA = [sb3a("stgA%d" % i, [128, 8, 128]) for i in range(2)]
            stgB = [sb3a("stgB%d" % i, [128, 8, 128]) for i in range(2)]
            wbA = [sb3a("wbA%d" % i, [128, 8, 128], BF16) for i in range(2)]
            wbB = [sb3a("wbB%d" % i, [128, 8, 128], BF16) for i in range(2)]
            wmA = [sb3a("wmA%d" % i, [128, 8, 128], BF16) for i in range(2)]
            wmB = [sb3a("wmB%d" % i, [128, 8, 128], BF16) for i in range(2)]
            sgA = [sb3a("sgA%d" % i, [128, 512]) for i in range(2)]
            sgB = [sb3a("sgB%d" % i, [128, 512]) for i in range(2)]
            tpA = [sb3a("tpA%d" % i, [128, 512]) for i in range(2)]
            tpB = [sb3a("tpB%d" % i, [128, 512]) for i in range(2)]
            S.barrier()
            ytok = [sb3a("ytok%d" % i, [128, 2 * D], BF16) for i in range(2)]
            for ti in range(NT_O):
                sl = ti % 2
                S.add("sp", lambda e, ti=ti, sl=sl: e.dma_start(out=ytok[sl][:], in_=ydram[ti * 128:(ti + 1) * 128, :]),
                      w=[("ytok", sl)], dma=True)
                for hb in range(2):
                    bank = (2 * ti + hb) % 4
                    pbv = psb[bank][:].bitcast(BF16)

                    def fyt(e, sl=sl, hb=hb, pbv=pbv):
                        ins = None
                        for c in range(8):
                            cc = hb * 8 + c
                            ins = e.transpose(pbv[:, c * 128:(c + 1) * 128], ytok[sl][:, cc * 128:(cc + 1) * 128], ident_b[:])
                        return ins
                    S.add("pe", fyt, r=[("ytok", sl), "ident_b"], w=[PSK(bank)])
                    S.add("act" if hb == 0 else "dve",
                          (lambda e, ti=ti, hb=hb, pbv=pbv: e.copy(out=yT[:, hb * 8:hb * 8 + 8, ti * 128:(ti + 1) * 128],
                                                                 in_=pbv[:, 0:1024].rearrange("p (c t) -> p c t", t=128)))
                          if hb == 0 else
                          (lambda e, ti=ti, hb=hb, pbv=pbv: e.tensor_copy(out=yT[:, hb * 8:hb * 8 + 8, ti * 128:(ti + 1) * 128],
                                                                        in_=pbv[:, 0:1024].rearrange("p (c t) -> p c t", t=128))),
                          r=[PSK(bank)], w=[("yT", c, ti) for c in range(hb * 8, hb * 8 + 8)])
            it = 0
            for ncn in range(8):
                p = ncn % 2
                S.add("sp", lambda e, p=p, ncn=ncn: e.dma_start(out=stgA[p][:], in_=w_bra_d[ncn].rearrange("(vc q) n -> q vc n", q=128)),
                      w=[("stgA", p)], dma=True)
                S.add("sp", lambda e, p=p, ncn=ncn: e.dma_start(out=stgB[p][:], in_=w_brb_d[ncn].rearrange("(vc q) n -> q vc n", q=128)),
                      w=[("stgB", p)], dma=True)
                S.add("pool", lambda e, p=p, ncn=ncn: e.dma_start(out=wmA[p][:], in_=w_m_d[ncn].rearrange("(dc q) n -> q dc n", q=128)),
                      w=[("wmA", p)], dma=True)
                S.add("pool", lambda e, p=p, ncn=ncn: e.dma_start(out=wmB[p][:], in_=w_m_d[8 + ncn].rearrange("(dc q) n -> q dc n", q=128)),
                      w=[("wmB", p)], dma=True)
                S.add("act", lambda e, p=p: e.activation(out=wbA[p][:], in_=stgA[p][:], func=AF.Copy, scale=hgn[:, 0:1]),
                      r=[("stgA", p), "hgn"], w=[("wbA", p)])
                for v2 in range(2):
                    S.add("act", lambda e, p=p, v2=v2: e.activation(
                        out=wbB[p][:].rearrange("q (h v) n -> q h v n", v=2)[:, :, v2, :],
                        in_=stgB[p][:].rearrange("q (h v) n -> q h v n", v=2)[:, :, v2, :],
                        func=AF.Copy, scale=glan[:, v2:v2 + 1]),
                        r=[("stgB", p), "glan"], w=[("wbB", p)])
                for tg in range(3):
                    b0 = 4 * (it % 2)
                    q = it % 2
                    it += 1
                    tk = slice(tg * 512, (tg + 1) * 512)
                    tiles = [("hT", t) for t in range(tg * 4, tg * 4 + 4)]
                    ytl = lambda c0: [("yT", c, t) for c in range(c0, c0 + 8) for t in range(tg * 4, tg * 4 + 4)]

                    def mm8(e, bank, wt, src, c0, tk=tk):
                        ins = None
                        for c in range(8):
                            ins = e.matmul(psb[bank][:, 0:512], lhsT=wt[:, c, :], rhs=src[:, c0 + c, tk],
                                           start=(c == 0), stop=(c == 7))
                        return ins
                    S.add("pe", lambda e, b0=b0, p=p, mm8=mm8: mm8(e, b0 + 2, wmA[p], hT, 0), r=[("wmA", p)] + tiles, w=[PSK(b0 + 2)])
                    S.add("pe", lambda e, b0=b0, p=p, mm8=mm8: mm8(e, b0 + 3, wmB[p], hT, 0), r=[("wmB", p)] + tiles, w=[PSK(b0 + 3)])
                    S.add("pe", lambda e, b0=b0, p=p, mm8=mm8: mm8(e, b0 + 0, wbA[p], yT, 0), r=[("wbA", p)] + ytl(0), w=[PSK(b0 + 0)])
                    S.add("pe", lambda e, b0=b0, p=p, mm8=mm8: mm8(e, b0 + 1, wbB[p], yT, 8), r=[("wbB", p)] + ytl(8), w=[PSK(b0 + 1)])
                    S.add("act", lambda e, b0=b0, q=q: e.activation(out=sgA[q][:], in_=psb[b0 + 2][:, 0:512], func=AF.Sigmoid),
                          r=[PSK(b0 + 2)], w=[("sgA", q)])
                    S.add("act", lambda e, b0=b0, q=q: e.activation(out=sgB[q][:], in_=psb[b0 + 3][:, 0:512], func=AF.Sigmoid),
                          r=[PSK(b0 + 3)], w=[("sgB", q)])
                    S.add("dve", lambda e, b0=b0, q=q: e.tensor_tensor(out=tpA[q][:], in0=psb[b0 + 0][:, 0:512], in1=sgA[q][:], op=ALU.mult),
                          r=[PSK(b0 + 0), ("sgA", q)], w=[("tpA", q)])
                    S.add("dve", lambda e, b0=b0, q=q: e.tensor_tensor(out=tpB[q][:], in0=psb[b0 + 1][:, 0:512], in1=sgB[q][:], op=ALU.mult),
                          r=[PSK(b0 + 1), ("sgB", q)], w=[("tpB", q)])
                    S.add("pool", lambda e, q=q, ncn=ncn, tk=tk: e.tensor_tensor(out=mergedT[:, ncn, tk], in0=tpA[q][:], in1=tpB[q][:], op=ALU.add),
                          r=[("tpA", q), ("tpB", q)], w=[("mergedT", ncn, tg)])
        if "mergedT" in dbg:
            S.add("sp", lambda e: e.dma_start(out=dbg_out["mergedT"], in_=mergedT[:]),
                  r=[("mergedT", n_, t_) for n_ in range(8) for t_ in range(3)], dma=True)
        S.barrier()
        x1s = yT[:].bitcast(F32).rearrange("p a b -> p (a b)").rearrange("p (t d) -> p t d", d=D)
        hflat = hT[:].rearrange("p a b -> p (a b)")
        h2T = hflat[:, 0:8 * TO].rearrange("p (a b) -> p a b", b=TO)
        cwT = sb3("cwT", [32, TO])
        ssq3 = sb3("ssq3", [128, NT_O])
        rs3 = sb3("rs3", [128, NT_O])
        with ExitStack() as st3b:
            def sb3b(name, shape, dt=F32):
                return st3b.enter_context(nc.sbuf_tensor("s_" + name, list(shape), dt))
            wout = sb3b("wout", [128, 8, D], BF16)
            wrt = sb3b("wrt", [128, 8, 36])
            g1bc = [sb3b("g1bc%d" % c, [128, D]) for c in range(2)]
            ones32 = sb3b("ones32", [128, 128])
            dg = [sb3b("dg%d" % i, [128, 128]) for i in range(2)]
            xr = [sb3b("xr%d" % i, [128, D]) for i in range(2)]
            tmpx = [sb3b("tmpx%d" % i, [128, D]) for i in range(2)]
            xn2 = [sb3b("xn2_%d" % i, [128, D]) for i in range(2)]
            h2f = [sb3b("h2f%d" % i, [128, 8, 128]) for i in range(2)]
            junk3 = sb3b("junk3", [128, D], BF16)
            NT = NT_O
            lga = sb3b("lga", [128, NT, 36])
            cwa = sb3b("cwa", [128, NT, 32])
            rA = sb3b("rA", [128, NT, 4])
            rB = sb3b("rB", [128, NT, 4])
            rC = sb3b("rC", [128, NT, 32])
            rsel = sb3b("rsel", [128, NT, 8])
            rsel2 = sb3b("rsel2", [128, NT, 8])
            rmk1 = sb3b("rmk1", [128, NT, 8])
            rmk2 = sb3b("rmk2", [128, NT, 8])
            rcw8 = sb3b("rcw8", [128, NT, 8])
            rs = sb3b("rs", [128, 10, NT])
            S.add("pool", lambda e: e.dma_start(out=wout[:], in_=w_out_d.rearrange("(c q) j -> q c j", q=128)), w=["wout"], dma=True)
            S.add("sp", lambda e: e.dma_start(out=wrt[:], in_=w_rt_d.rearrange("(c q) j -> q c j", q=128)), w=["wrt"], dma=True)
            S.add("pool", lambda e: e.memset(ones32[:], 1.0), w=["ones32"])

            def bcast_tile(dst, j0, c, nm):
                for jc in range(8):
                    d_ = dg[jc % 2]
                    S.add("dve", lambda e, d_=d_, jc=jc: e.tensor_scalar(out=d_[:], in0=ident_f[:], scalar1=modcol(j0, jc, c),
                                                                         scalar2=None, op0=ALU.mult),
                          r=["ident_f", "modT1"], w=[("dg", jc % 2)])
                    bank = jc % 2
                    S.add("pe", lambda e, d_=d_, bank=bank: e.matmul(psb[bank][:, 0:128], lhsT=ones32[:], rhs=d_[:], start=True, stop=True),
                          r=["ones32", ("dg", jc % 2)], w=[PSK(bank)])
                    S.add("act", lambda e, bank=bank, jc=jc: e.copy(out=dst[:, jc * 128:(jc + 1) * 128], in_=psb[bank][:, 0:128]),
                          r=[PSK(bank)], w=[nm])
            for c in range(2):
                bcast_tile(g1bc[c], 16, c, ("g1bc", c))

            def s3b_1(ti):
                c = cond_of_tile(ti)
                sl = ti % 2
                S.add("sp", lambda e, ti=ti, sl=sl: e.dma_start(out=xr[sl][:], in_=x_all[ti * 128:(ti + 1) * 128, :]),
                      w=[("xr", sl)], dma=True)
                for jh in range(2):
                    bank = 2 + jh

                    def fm(e, ti=ti, jh=jh, bank=bank):
                        ins = None
                        for n_ in range(8):
                            ins = e.matmul(psb[bank][:, 0:512], lhsT=mergedT[:, n_, ti * 128:(ti + 1) * 128],
                                           rhs=wout[:, n_, jh * 512:(jh + 1) * 512], start=(n_ == 0), stop=(n_ == 7))
                        return ins
                    S.add("pe", fm, r=["wout"] + [("mergedT", n_, ti // 4) for n_ in range(8)], w=[PSK(bank)])
                    js = slice(jh * 512, (jh + 1) * 512)
                    S.add("dve", lambda e, sl=sl, bank=bank, js=js, c=c: e.tensor_tensor(
                        out=tmpx[sl][:, js], in0=psb[bank][:, 0:512], in1=g1bc[c][:, js], op=ALU.mult),
                        r=[PSK(bank), ("g1bc", c)], w=[("tmpx", sl)])
                    S.add("pool", lambda e, sl=sl, js=js, ti=ti: e.tensor_tensor(
                        out=x1s[:, ti, js], in0=tmpx[sl][:, js], in1=xr[sl][:, js], op=ALU.add),
                        r=[("tmpx", sl), ("xr", sl)], w=[("x1s", ti)])

            def s3b_2(ti):
                c = cond_of_tile(ti)
                sl = ti % 2
                S.add("act", lambda e, ti=ti: e.activation(out=junk3[:], in_=x1s[:, ti, :], func=AF.Square, accum_out=ssq3[:, ti:ti + 1]),
                      r=[("x1s", ti)], w=["junk3", ("ssq3", ti)])
                S.add("act", lambda e, ti=ti: e.activation(out=rs3[:, ti:ti + 1], in_=ssq3[:, ti:ti + 1], func=AF.Ln, bias=epsb[:], scale=1.0 / D),
                      r=[("ssq3", ti), "epsb"], w=[("rs3", ti)])
                S.add("act", lambda e, ti=ti: e.activation(out=rs3[:, ti:ti + 1], in_=rs3[:, ti:ti + 1], func=AF.Exp, scale=-0.5),
                      r=[("rs3", ti)], w=[("rs3", ti)])
                S.add("dve", lambda e, ti=ti, sl=sl: e.tensor_scalar(out=xn2[sl][:], in0=x1s[:, ti, :], scalar1=rs3[:, ti:ti + 1],
                                                                     scalar2=None, op0=ALU.mult),
                      r=[("x1s", ti), ("rs3", ti)], w=[("xn2", sl)])
                for half in range(2):
                    bank = 4 + half

                    def ft(e, sl=sl, half=half, bank=bank):
                        ins = None
                        for j in range(4):
                            dc = half * 4 + j
                            ins = e.transpose(psb[bank][:, j * 128:(j + 1) * 128], xn2[sl][:, dc * 128:(dc + 1) * 128], ident_f[:])
                        return ins
                    S.add("pe", ft, r=[("xn2", sl), "ident_f"], w=[PSK(bank)])
                    for j in range(4):
                        dc = half * 4 + j
                        S.add("dve" if j % 2 == 0 else "act",
                              (lambda e, sl=sl, bank=bank, j=j, dc=dc, c=c: e.tensor_scalar(
                                  out=h2f[sl][:, dc, :], in0=psb[bank][:, j * 128:(j + 1) * 128],
                                  scalar1=A2[:, dc * 2 + c:dc * 2 + c + 1], scalar2=modcol(24, dc, c), op0=ALU.mult, op1=ALU.add))
                              if j % 2 == 0 else
                              (lambda e, sl=sl, bank=bank, j=j, dc=dc, c=c: e.activation(
                                  out=h2f[sl][:, dc, :], in_=psb[bank][:, j * 128:(j + 1) * 128], func=AF.Identity,
                                  bias=modcol(24, dc, c), scale=A2[:, dc * 2 + c:dc * 2 + c + 1])),
                              r=[PSK(bank), "A2", "modT1"], w=[("h2f", sl)])
                S.add("act", lambda e, sl=sl, ti=ti: e.copy(out=h2T[:, :, ti * 128:(ti + 1) * 128], in_=h2f[sl][:]),
                      r=[("h2f", sl)], w=[("h2T", ti)])
                def frt(e, sl=sl):
                    ins = None
                    for dc in range(8):
                        ins = e.matmul(psb[6][:, 0:36], lhsT=h2f[sl][:, dc, :], rhs=wrt[:, dc, :], start=(dc == 0), stop=(dc == 7))
                    return ins
                S.add("pe", frt, r=[("h2f", sl), "wrt"], w=[PSK(6)])
                S.add("act", lambda e, ti=ti: e.copy(out=lga[:, ti, :], in_=psb[6][:, 0:36]), r=[PSK(6)], w=[("lga", ti)])

            pipeline([s3b_1, s3b_2], NT_O, [0, 1])
            lgk = [("lga", t) for t in range(NT)]
            gm, gs, ptop, m1, m2, e2, den, w1, w2 = (rs[:, i, :] for i in range(9))
            lg = lga[:, :, 0:4]
            le = lga[:, :, 4:36].rearrange("p t (g e) -> p t g e", e=8)
            b3 = lambda ap, k: ap.rearrange("p (t o) -> p t o", o=1).to_broadcast([128, NT, k])
            V = lambda fn, r, w: S.add("dve", fn, r=r, w=w)
            V(lambda e: e.tensor_reduce(out=gm, in_=lg, axis=AX.X, op=ALU.max), lgk, ["r_gm"])
            V(lambda e: e.tensor_tensor(out=rA[:], in0=lg, in1=b3(gm, 4), op=ALU.is_ge), lgk + ["r_gm"], ["rA"])
            V(lambda e: e.tensor_tensor(out=rB[:], in0=lg, in1=b3(gm, 4), op=ALU.subtract), lgk + ["r_gm"], ["rB"])
            S.add("act", lambda e: e.activation(out=rB[:], in_=rB[:], func=AF.Exp), r=["rB"], w=["rB"])
            V(lambda e: e.tensor_reduce(out=gs, in_=rB[:], axis=AX.X, op=ALU.add), ["rB"], ["r_gs"])
            V(lambda e: e.reciprocal(out=ptop, in_=gs), ["r_gs"], ["r_ptop"])
            rC4 = rC[:].rearrange("p t (g e) -> p t g e", e=8)
            V(lambda e: e.tensor_tensor(out=rC4, in0=le, in1=rA[:].rearrange("p t (g o) -> p t g o", o=1).to_broadcast([128, NT, 4, 8]),
                                        op=ALU.mult), lgk + ["rA"], ["rC"])
            V(lambda e: e.tensor_reduce(out=rsel[:], in_=rC[:].rearrange("p t (g e) -> p t e g", e=8), axis=AX.X, op=ALU.add),
              ["rC"], ["rsel"])
            V(lambda e: e.tensor_reduce(out=m1, in_=rsel[:], axis=AX.X, op=ALU.max), ["rsel"], ["r_m1"])
            V(lambda e: e.tensor_tensor(out=rmk1[:], in0=rsel[:], in1=b3(m1, 8), op=ALU.is_ge), ["rsel", "r_m1"], ["rmk1"])
            V(lambda e: e.scalar_tensor_tensor(out=rsel2[:], in0=rmk1[:], scalar=-1e30, in1=rsel[:], op0=ALU.mult, op1=ALU.add),
              ["rmk1", "rsel"], ["rsel2"])
            V(lambda e: e.tensor_reduce(out=m2, in_=rsel2[:], axis=AX.X, op=ALU.max), ["rsel2"], ["r_m2"])
            V(lambda e: e.tensor_tensor(out=rmk2[:], in0=rsel2[:], in1=b3(m2, 8), op=ALU.is_ge), ["rsel2", "r_m2"], ["rmk2"])
            V(lambda e: e.tensor_sub(out=e2, in0=m2, in1=m1), ["r_m1", "r_m2"], ["r_e2"])
            S.add("act", lambda e: e.activation(out=e2, in_=e2, func=AF.Exp), r=["r_e2"], w=["r_e2"])
            V(lambda e: e.tensor_scalar(out=den, in0=e2, scalar1=1.0, scalar2=None, op0=ALU.add), ["r_e2"], ["r_den"])
            V(lambda e: e.reciprocal(out=den, in_=den), ["r_den"], ["r_den"])
            V(lambda e: e.tensor_tensor(out=w1, in0=den, in1=ptop, op=ALU.mult), ["r_den", "r_ptop"], ["r_w1"])
            V(lambda e: e.tensor_tensor(out=w2, in0=w1, in1=e2, op=ALU.mult), ["r_w1", "r_e2"], ["r_w2"])
            V(lambda e: e.tensor_tensor(out=rcw8[:], in0=rmk1[:], in1=b3(w1, 8), op=ALU.mult), ["rmk1", "r_w1"], ["rcw8"])
            V(lambda e: e.tensor_tensor(out=rmk2[:], in0=rmk2[:], in1=b3(w2, 8), op=ALU.mult), ["rmk2", "r_w2"], ["rmk2"])
            V(lambda e: e.tensor_tensor(out=rcw8[:], in0=rcw8[:], in1=rmk2[:], op=ALU.add), ["rcw8", "rmk2"], ["rcw8"])
            cwa4 = cwa[:].rearrange("p t (g e) -> p t g e", e=8)
            for g_ in range(4):
                V(lambda e, g_=g_: e.tensor_tensor(out=cwa4[:, :, g_, :], in0=rcw8[:], in1=rA[:, :, g_:g_ + 1].to_broadcast([128, NT, 8]),
                                                   op=ALU.mult), ["rcw8", "rA"], ["cwa"])
            for t3 in range(3):
                def ftc(e, t3=t3):
                    ins = None
                    for j in range(4):
                        ti = t3 * 4 + j
                        ins = e.transpose(psb[5 + t3][0:32, j * 128:(j + 1) * 128], cwa[:, ti, :], ident_f[:])
                    return ins
                S.add("pe", ftc, r=["cwa", "ident_f"], w=[PSK(5 + t3)])
                S.add("act", lambda e, t3=t3: e.copy(out=cwT[:, t3 * 512:(t3 + 1) * 512], in_=psb[5 + t3][0:32, 0:512]),
                      r=[PSK(5 + t3)], w=[("cwT", t3 * 4 + j) for j in range(4)])
        if "x1" in dbg:
            S.add("sp", lambda e: e.dma_start(out=dbg_out["x1"], in_=x1s[:]), r=[("x1s", t) for t in range(NT_O)], dma=True)
        if "cwT" in dbg:
            S.add("sp", lambda e: e.dma_start(out=dbg_out["cwT"], in_=cwT[:]), r=[("cwT", t) for t in range(NT_O)], dma=True)
        S.barrier()

        with ExitStack() as st4:
            def sb4(name, shape, dt=F32):
                return st4.enter_context(nc.sbuf_tensor("s_" + name, list(shape), dt))
            accA = mergedT[:].bitcast(F32).rearrange("p a b -> p (a b)").rearrange("p (t d) -> p t d", d=D)
            accB = sb4("accB", [128, 6, D])
            acc = lambda ti: accA[:, ti, :] if ti < 6 else accB[:, ti - 6, :]
            spare = hflat[:, 8 * TO:8 * TA]
            wsl = lambda i: spare[:, i * 2048:(i + 1) * 2048].rearrange("p (c f) -> p c f", f=256)
            wg = [wsl(0), wsl(1)]
            wu = [wsl(2), wsl(3)]
            wd = [sb4("wd%d" % i, [128, 2, D], BF16) for i in range(2)]
            selE = [sb4("selE%d" % i, [32, 128]) for i in range(2)]
            sA = [sb4("sA%d" % i, [128, 512]) for i in range(2)]
            tA = [sb4("tA%d" % i, [128, 512]) for i in range(2)]
            cbs = [sb4("cbs%d" % i, [128, 512]) for i in range(2)]
            hid = [[sb4("hid%d_%d" % (i, j), [128, 512], BF16) for j in range(2)] for i in range(2)]
            g2bc = [sb4("g2bc%d" % c, [128, D]) for c in range(2)]
            fngbc = sb4("fngbc", [128, D])
            ones32b = sb4("ones32b", [128, 128])
            dgb = [sb4("dgb%d" % i, [128, 128]) for i in range(2)]
            junk4 = sb4("junk4", [128, D], BF16)
            ssq4 = sb4("ssq4", [128, NT_O])
            rs4 = sb4("rs4", [128, NT_O])
            S.add("pool", lambda e: e.memset(ones32b[:], 1.0), w=["ones32b"])
            S.add("sp", lambda e: e.dma_start(out=fngbc[:], in_=fng_d.to_broadcast([128, D])), w=["fngbc"], dma=True)
            for c in range(2):
                for jc in range(8):
                    d_ = dgb[jc % 2]
                    bank = jc % 2
                    S.add("dve", lambda e, d_=d_, jc=jc, c=c: e.tensor_scalar(out=d_[:], in0=ident_f[:], scalar1=modcol(40, jc, c),
                                                                              scalar2=None, op0=ALU.mult),
                          r=["ident_f", "modT1"], w=[("dgb", jc % 2)])
                    S.add("pe", lambda e, d_=d_, bank=bank: e.matmul(psb[bank][:, 0:128], lhsT=ones32b[:], rhs=d_[:], start=True, stop=True),
                          r=["ones32b", ("dgb", jc % 2)], w=[PSK(bank)])
                    S.add("act", lambda e, bank=bank, jc=jc, c=c: e.copy(out=g2bc[c][:, jc * 128:(jc + 1) * 128], in_=psb[bank][:, 0:128]),
                          r=[PSK(bank)], w=[("g2bc", c)])

            NE = dbg.get("_nexp", NEXP)
            itc = 0
            pending = [None]
            for e_ in range(NE):
                pp = e_ % 2
                S.add("pool", lambda e, pp=pp, e_=e_: e.dma_start(out=wg[pp], in_=w_eg_d[e_].rearrange("(c q) f -> q c f", q=128)),
                      w=[("wg", pp)], dma=True)
                S.add("pool", lambda e, pp=pp, e_=e_: e.dma_start(out=wu[pp], in_=w_eu_d[e_].rearrange("(c q) f -> q c f", q=128)),
                      w=[("wu", pp)], dma=True)
                S.add("pool", lambda e, pp=pp, e_=e_: e.dma_start(out=wd[pp][:], in_=w_ed_d[e_].rearrange("(c q) d -> q c d", q=128)),
                      w=[("wd", pp)], dma=True)
                S.add("pool", lambda e, pp=pp: e.memset(selE[pp][:], 0.0), w=[("selE", pp)])
                S.add("pool", lambda e, pp=pp, e_=e_: e.affine_select(out=selE[pp][:], in_=selE[pp][:], pattern=[[0, 128]],
                                                                     compare_op=ALU.not_equal, fill=1.0, base=-e_, channel_multiplier=1),
                      r=[("selE", pp)], w=[("selE", pp)])
                for tg in range(3):
                    tk = slice(tg * 512, (tg + 1) * 512)
                    hs = itc % 2
                    itc += 1
                    S.add("pe", lambda e, pp=pp, tk=tk: e.matmul(psb[4][:, 0:512], lhsT=selE[pp][:], rhs=cwT[:, tk], start=True, stop=True),
                          r=[("selE", pp)] + [("cwT", t) for t in range(tg * 4, tg * 4 + 4)], w=[PSK(4)])
                    S.add("act", lambda e, hs=hs: e.copy(out=cbs[hs][:], in_=psb[4][:, 0:512]), r=[PSK(4)], w=[("cbs", hs)])
                    for f in range(2):
                        def fg(e, wt, bank, f=f, tk=tk):
                            ins = None
                            for dc in range(8):
                                ins = e.matmul(psb[bank][:, 0:512], lhsT=wt[:, dc, f * 128:(f + 1) * 128], rhs=h2T[:, dc, tk],
                                               start=(dc == 0), stop=(dc == 7))
                            return ins
                        ba, bu = 2 * f, 2 * f + 1
                        h2k = [("h2T", t) for t in range(tg * 4, tg * 4 + 4)]
                        S.add("pe", lambda e, fg=fg, pp=pp, ba=ba: fg(e, wg[pp], ba), r=[("wg", pp)] + h2k, w=[PSK(ba)])
                        S.add("pe", lambda e, fg=fg, pp=pp, bu=bu: fg(e, wu[pp], bu), r=[("wu", pp)] + h2k, w=[PSK(bu)])
                        S.add("act", lambda e, ba=ba, f=f: e.activation(out=sA[f][:], in_=psb[ba][:, 0:512], func=AF.Silu),
                              r=[PSK(ba)], w=[("sA", f)])
                        S.add("dve", lambda e, bu=bu, f=f: e.tensor_tensor(out=tA[f][:], in0=psb[bu][:, 0:512], in1=sA[f][:], op=ALU.mult),
                              r=[PSK(bu), ("sA", f)], w=[("tA", f)])
                        S.add("pool", lambda e, f=f, hs=hs: e.tensor_tensor(out=hid[hs][f][:], in0=tA[f][:], in1=cbs[hs][:], op=ALU.mult),
                              r=[("tA", f), ("cbs", hs)], w=[("hid", hs, f)])
                    def emit_down(tg=tg, hs=hs, pp=pp, e_=e_):
                        for t4 in range(4):
                            ti = tg * 4 + t4
                            for dh in range(2):
                                bank = 5 + (t4 * 2 + dh) % 3

                                def fd(e, t4=t4, dh=dh, bank=bank):
                                    ins = None
                                    for f in range(2):
                                        ins = e.matmul(psb[bank][:, 0:512], lhsT=hid[hs][f][:, t4 * 128:(t4 + 1) * 128],
                                                       rhs=wd[pp][:, f, dh * 512:(dh + 1) * 512], start=(f == 0), stop=(f == 1))
                                    return ins
                                S.add("pe", fd, r=[("hid", hs, 0), ("hid", hs, 1), ("wd", pp)], w=[PSK(bank)])
                                ds = slice(dh * 512, (dh + 1) * 512)
                                a_ap = acc(ti)[:, ds]
                                if e_ == 0:
                                    S.add("dve", lambda e, a_ap=a_ap, bank=bank: e.tensor_copy(out=a_ap, in_=psb[bank][:, 0:512]),
                                          r=[PSK(bank)], w=[("acc", ti, dh)])
                                else:
                                    S.add("dve", lambda e, a_ap=a_ap, bank=bank: e.tensor_tensor(
                                        out=a_ap, in0=psb[bank][:, 0:512], in1=a_ap, op=ALU.add),
                                        r=[PSK(bank), ("acc", ti, dh)], w=[("acc", ti, dh)])
                    if pending[0] is not None:
                        pending[0]()
                    pending[0] = emit_down
            pending[0]()
            def fin_1(ti):
                c = cond_of_tile(ti)
                a_ap = acc(ti)
                ak = [("acc", ti, 0), ("acc", ti, 1)]
                S.add("pool", lambda e: e.tensor_tensor(out=a_ap, in0=a_ap, in1=g2bc[c][:], op=ALU.mult),
                      r=ak + [("g2bc", c)], w=ak)
                S.add("dve", lambda e: e.tensor_tensor(out=a_ap, in0=a_ap, in1=x1s[:, ti, :], op=ALU.add),
                      r=ak + [("x1s", ti)], w=ak)
                S.add("act", lambda e: e.activation(out=junk4[:], in_=a_ap, func=AF.Square, accum_out=ssq4[:, ti:ti + 1]),
                      r=ak, w=["junk4", ("ssq4", ti)])
                S.add("act", lambda e: e.activation(out=rs4[:, ti:ti + 1], in_=ssq4[:, ti:ti + 1], func=AF.Ln, bias=epsb[:], scale=1.0 / D),
                      r=[("ssq4", ti), "epsb"], w=[("rs4", ti)])
                S.add("act", lambda e: e.activation(out=rs4[:, ti:ti + 1], in_=rs4[:, ti:ti + 1], func=AF.Exp, scale=-0.5),
                      r=[("rs4", ti)], w=[("rs4", ti)])

            def fin_2(ti):
                a_ap = acc(ti)
                ak = [("acc", ti, 0), ("acc", ti, 1)]
                S.add("dve", lambda e: e.tensor_tensor(out=a_ap, in0=a_ap, in1=fngbc[:], op=ALU.mult), r=ak + ["fngbc"], w=ak)
                S.add("act", lambda e: e.activation(out=a_ap, in_=a_ap, func=AF.Copy, scale=rs4[:, ti:ti + 1]),
                      r=ak + [("rs4", ti)], w=ak)
                S.add("sp", lambda e: e.dma_start(out=y_out[ti * 128:(ti + 1) * 128, :], in_=a_ap), r=ak, dma=True)

            pipeline([fin_1, fin_2], NT_O, [0, 1])
        if "hT" in dbg:
            tmp = sb("dbg_hT", [128, 8, TA], F32)
            S.add("dve", lambda e: e.tensor_copy(out=tmp[:], in_=hT[:]), r=[("hT", i) for i in range(NT_A)], w=["dbg_hT"])
            S.add("sp", lambda e: e.dma_start(out=dbg_out["hT"], in_=tmp[:]), r=["dbg_hT"], dma=True)
        if "modT" in dbg:
            S.add("sp", lambda e: e.dma_start(out=dbg_out["modT"], in_=modT[:]), r=["modT"], dma=True)

        S.emit(st)
    return nc


def _prep_core(c, I):
    b, par = c // 2, c % 2
    f = np.ascontiguousarray
    xs = I["x_sample"][b]
    if par:
        xs = xs[::-1]
    p0 = I["x_prompt"][2 * c]
    p1 = I["x_prompt"][2 * c + 1]
    if par:
        p0, p1 = p0[::-1], p1[::-1]
    x_all = np.concatenate([xs[:1024], p0, p1, xs[1024:]], 0)
    cond = np.stack([I["c"][b], I["c_ctx"]], 0)
    condT = cond.reshape(2, 8, 128).transpose(2, 1, 0).reshape(128, 16)
    w_in = I["w_in"][0]
    o = np.cumsum([0, 1024, 1024, 1024, 1024, 1024, 512, 512, 1024, 1024, 16, 16, 1024, 1024])
    hq, hf0, hf1, hi, hgate, gq, gk, gv, gr, ga0, ga1, ma, mb = [w_in[:, o[i]:o[i + 1]] for i in range(13)]
    d0, d1 = (1, 0) if par else (0, 1)
    if par:
        hf0, hf1 = hf1, hf0
        ga0, ga1 = ga1, ga0
    hd = lambda w, h, n: w[:, h * n:(h + 1) * n]
    w_hg = np.stack([np.concatenate([hd(hq, h, 128), hd(hf0, h, 128), hd(hf1, h, 128), hd(hi, h, 128),
                                     hd(hgate, h, 128)], 1) for h in range(8)], 0)
    w_gla = np.stack([np.concatenate([hd(gq, h, 128), hd(gk, h, 128), hd(gv, h, 256), hd(gr, h, 256)], 1)
                      for h in range(4)], 0)
    w_ga = np.concatenate([ga0, ga1], 1)
    w_m = np.concatenate([ma.reshape(D, 8, 128).transpose(1, 0, 2), mb.reshape(D, 8, 128).transpose(1, 0, 2)], 0)
    lbp = np.concatenate([I["hg_lb_param"][0].reshape(8, 128).T, I["hg_lb_param"][1].reshape(8, 128).T], 1)
    wa = I["gla_wa_up"][0][[d0, d1]]
    ba = I["gla_ba"][0][[d0, d1]]
    baT = ba.reshape(2, 4, 128).transpose(2, 0, 1).reshape(128, 8)
    m = {
        "x_all": x_all, "condT": condT, "ada_w": I["ada_w"][0],
        "ada_bT": I["ada_b"][0].reshape(48, 128).T,
        "g1T": I["norm1_g"][0].reshape(8, 128).T, "g2T": I["norm2_g"][0].reshape(8, 128).T,
        "w_hg": w_hg, "w_gla": w_gla, "w_ga": w_ga, "w_m": w_m, "lbp": lbp,
        "hgn": I["hg_norm_g"][0].reshape(128, 1), "glan": I["gla_norm_g"][0].reshape(2, 128).T,
        "wa_up": wa, "baT": baT,
        "w_bra": I["w_br_a"][0].reshape(D, 8, 128).transpose(1, 0, 2),
        "w_brb": I["w_br_b"][0].reshape(D, 8, 128).transpose(1, 0, 2),
        "w_out": I["w_out"][0],
        "w_rt": np.concatenate([I["w_router_group"][0]] + [I["w_router_expert"][0][g] for g in range(4)], 1),
        "w_eg": I["w_exp_gate"][0], "w_eu": I["w_exp_up"][0], "w_ed": I["w_exp_down"][0],
        "fng": I["final_norm_g"].reshape(1, D),
        "s0_hg": I["state_hgrn"][b, 0][[d0, d1]], "s0_gla": I["state_gla"][b, 0][[d0, d1]],
    }
    return {k: f(np.asarray(v, dtype=np.float32)) for k, v in m.items()}


_NC_CACHE = {}


def kernel(**inputs):
    I = {k: np.asarray(v) for k, v in inputs.items()}
    if "nc" not in _NC_CACHE:
        _NC_CACHE["nc"] = build()
    nc = _NC_CACHE["nc"]
    in_maps = [_prep_core(c, I) for c in range(8)]
    res = run_bass_kernel_spmd(nc, in_maps, core_ids=list(range(8)))
    y_prompt = np.zeros((16, 256, D), np.float32)
    y_sample = np.zeros((4, 2048, D), np.float32)
    st_h = np.zeros((16, 1, 2, 8, 128, 128), np.float32)
    st_g = np.zeros((16, 1, 2, 4, 128, 256), np.float32)
    for c in range(8):
        r = res.results[c]
        b, par = c // 2, c % 2
        y = r["y_out"]
        ys, yp0, yp1 = y[:1024], y[1024:1280], y[1280:1536]
        if par:
            y_sample[b, 1024:] = ys[::-1]
            y_prompt[2 * c] = yp0[::-1]
            y_prompt[2 * c + 1] = yp1[::-1]
        else:
            y_sample[b, :1024] = ys
            y_prompt[2 * c] = yp0
            y_prompt[2 * c + 1] = yp1
        sh, sg = r["st_hg"], r["st_gla"]
        if par:
            sh, sg = sh[:, ::-1], sg[:, ::-1]
        st_h[2 * c:2 * c + 2, 0] = sh
        st_g[2 * c:2 * c + 2, 0] = sg
    return (y_prompt, y_sample, st_h, st_g)
```

```python
import os
from contextlib import ExitStack
import numpy as np
import concourse.bass as bass
import concourse.mybir as mybir
from concourse.bass_utils import run_bass_kernel_spmd

F32 = mybir.dt.float32
BF16 = mybir.dt.bfloat16
AF = mybir.ActivationFunctionType
ALU = mybir.AluOpType
AX = mybir.AxisListType

D = 1024
TO = 1536
TOTH = 1024
TA = TO + TOTH
NT_O = TO // 128
NT_A = TA // 128
EPS = 1e-6
NEXP = 32


class Sched:
    def __init__(self, nc):
        self.nc = nc
        self.ops = []
        self.lw = {}
        self.rd = {}
        self.bar = set()

    def barrier(self):
        last = {}
        dmas = {}
        for i, op in enumerate(self.ops):
            last[op["eng"]] = i
            if op["dma"]:
                dmas.setdefault(op["eng"], []).append(i)
        b = set(last.values())
        for e, l in dmas.items():
            b.update(l[-8:])
        self.bar = b

    def add(self, eng, fn, r=(), w=(), dma=False):
        deps = set(self.bar)
        for k in r:
            if k in self.lw:
                deps.add(self.lw[k])
        for k in w:
            if k in self.lw:
                deps.add(self.lw[k])
            deps.update(self.rd.get(k, ()))
        i = len(self.ops)
        self.ops.append(dict(eng=eng, fn=fn, deps=deps, dma=dma, need=dma, sig=None, pre=None))
        for k in r:
            self.rd.setdefault(k, []).append(i)
        for k in w:
            self.lw[k] = i
            self.rd[k] = []
        return i

    def emit(self, stack):
        nc = self.nc
        ops = self.ops
        for op in ops:
            for d in op["deps"]:
                if ops[d]["dma"] or not (ops[d]["eng"] == "pe" and op["eng"] == "pe"):
                    ops[d]["need"] = True
        engs = ("pe", "act", "dve", "pool", "sp")
        esem = {e: stack.enter_context(nc.semaphore("c_" + e)) for e in engs}
        KD = 8
        dsem = {e: [stack.enter_context(nc.semaphore("d_%s%d" % (e, i))) for i in range(KD)]
                for e in ("sp", "pool", "act")}
        ecnt = {e: 0 for e in engs}
        dcnt = {e: 0 for e in dsem}
        dfinal = {}
        for op in ops:
            e = op["eng"]
            if op["dma"]:
                j = dcnt[e]
                dcnt[e] += 1
                s = dsem[e][j % KD]
                u = j // KD
                if u > 0:
                    op["pre"] = (s, 16 * u)
                op["sig"] = (s, 16 * (u + 1), 16)
                dfinal[id(s)] = (s, 16 * (u + 1))
            elif op["need"]:
                ecnt[e] += 1
                op["sig"] = (esem[e], ecnt[e], 1)

        def run(name, e):
            waited = {}

            def wait(s, v):
                if waited.get(id(s), 0) < v:
                    e.wait_ge(s, v)
                    waited[id(s)] = v

            for op in ops:
                if op["eng"] != name:
                    continue
                if op["pre"] is not None:
                    wait(*op["pre"])
                for d in sorted(op["deps"]):
                    dop = ops[d]
                    if dop["dma"] or not (dop["eng"] == "pe" and name == "pe"):
                        wait(dop["sig"][0], dop["sig"][1])
                ins = op["fn"](e)
                if op["sig"] is not None:
                    ins.then_inc(op["sig"][0], op["sig"][2])
            if name == "sp":
                for s, v in dfinal.values():
                    wait(s, v)

        with nc.Block() as block:
            @block.sync
            def _(e):
                run("sp", e)

            @block.tensor
            def _(e):
                run("pe", e)

            @block.scalar
            def _(e):
                run("act", e)

            @block.vector
            def _(e):
                run("dve", e)

            @block.gpsimd
            def _(e):
                run("pool", e)


def build(dbg=None):
    nc = bass.Bass("TRN2", target_bir_lowering=False)
    S = Sched(nc)
    dbg = dbg or {}

    def din(name, shape):
        return nc.dram_tensor(name, list(shape), F32, kind="ExternalInput").ap()

    def dout(name, shape, dt=F32):
        return nc.dram_tensor(name, list(shape), dt, kind="ExternalOutput").ap()

    x_all = din("x_all", [TA, D])
    condT_d = din("condT", [128, 16])
    ada_w_d = din("ada_w", [D, 6 * D])
    ada_bT_d = din("ada_bT", [128, 48])
    g1T_d = din("g1T", [128, 8])
    g2T_d = din("g2T", [128, 8])
    w_hg_d = din("w_hg", [8, D, 640])
    w_gla_d = din("w_gla", [4, D, 768])
    w_ga_d = din("w_ga", [D, 32])
    w_m_d = din("w_m", [16, D, 128])
    lbp_d = din("lbp", [128, 16])
    hgn_d = din("hgn", [128, 1])
    glan_d = din("glan", [128, 2])
    wa_up_d = din("wa_up", [2, 16, 512])
    nbaT_d = din("baT", [128, 8])
    w_bra_d = din("w_bra", [8, D, 128])
    w_brb_d = din("w_brb", [8, D, 128])
    w_out_d = din("w_out", [D, D])
    w_rt_d = din("w_rt", [D, 36])
    w_eg_d = din("w_eg", [NEXP, D, 256])
    w_eu_d = din("w_eu", [NEXP, D, 256])
    w_ed_d = din("w_ed", [NEXP, 256, D])
    fng_d = din("fng", [1, D])
    s0_hg_d = din("s0_hg", [2, 8, 128, 128])
    s0_gla_d = din("s0_gla", [2, 4, 128, 256])

    y_out = dout("y_out", [TO, D])
    st_hg = dout("st_hg", [2, 2, 8, 128, 128])
    st_gla = dout("st_gla", [2, 2, 4, 128, 256])
    dbg_out = {k: dout("dbg_" + k, shp, BF16 if k in ("yT",) else F32) for k, shp in dbg.items() if not k.startswith("_")}

    with ExitStack() as st:
        def sb(name, shape, dt=F32):
            return st.enter_context(nc.sbuf_tensor("s_" + name, list(shape), dt))

        psb = [st.enter_context(nc.psum_tensor("ps%d" % i, [128, 512], F32)) for i in range(8)]

        def PSK(b):
            return ("ps", b)

        ident_f = sb("ident_f", [128, 128])
        ident_b = sb("ident_b", [128, 128], BF16)
        ones_f = sb("ones_f", [128, 512], BF16)
        one_c = sb("one_c", [128, 1])
        mask32 = sb("mask32", [128, 1024], BF16)
        trimask = sb("trimask", [128, 512], BF16)
        S.add("pool", lambda e: e.memset(ident_f[:], 0.0), w=["ident_f"])
        S.add("pool", lambda e: e.affine_select(out=ident_f[:], in_=ident_f[:], pattern=[[-1, 128]],
                                                compare_op=ALU.not_equal, fill=1.0, base=0,
                                                channel_multiplier=1), r=["ident_f"], w=["ident_f"])
        S.add("dve", lambda e: e.tensor_copy(out=ident_b[:], in_=ident_f[:]), r=["ident_f"], w=["ident_b"])
        S.add("pool", lambda e: e.memset(ones_f[:], 1.0), w=["ones_f"])
        S.add("pool", lambda e: e.memset(one_c[:], 1.0), w=["one_c"])
        S.add("pool", lambda e: e.memset(mask32[:], 1.0), w=["mask32"])
        S.add("pool", lambda e: e.memset(mask32[:].rearrange("p (c j) -> p c j", j=32)[:, :, 0:1], 0.0),
              r=["mask32"], w=["mask32"])
        tmv = trimask[:].rearrange("p (a d t) -> p a d t", a=4, d=2)
        onv = ones_f[:].rearrange("p (a d t) -> p a d t", a=4, d=2)
        for half in range(2):
            pr = slice(half * 64, half * 64 + 64)
            S.add("pool", lambda e, pr=pr: e.affine_select(
                out=tmv[pr, :, 0, :], in_=onv[pr, :, 0, :], pattern=[[0, 4], [1, 64]],
                compare_op=ALU.is_ge, fill=0.0, base=0, channel_multiplier=-1),
                r=["ones_f"], w=["trimask"])
            S.add("pool", lambda e, pr=pr: e.affine_select(
                out=tmv[pr, :, 1, :], in_=onv[pr, :, 1, :], pattern=[[0, 4], [-1, 64]],
                compare_op=ALU.is_ge, fill=0.0, base=0, channel_multiplier=1),
                r=["ones_f"], w=["trimask"])

        condT = sb("condT", [128, 16])
        scT = sb("scT", [128, 16])
        ada_bT = sb("ada_bT", [128, 48])
        g1T = sb("g1T", [128, 8])
        g2T = sb("g2T", [128, 8])
        lbp = sb("lbp", [128, 16])
        hgn = sb("hgn", [128, 1])
        glan = sb("glan", [128, 2])
        baT = sb("baT_s", [128, 8])
        nbaT = sb("nbaT", [128, 8])
        modT = sb("modT", [128, 96])
        A1 = sb("A1", [128, 16])
        A2 = sb("A2", [128, 16])
        lb = sb("lb", [128, 8])
        oml = sb("oml", [128, 8])
        noml = sb("noml", [128, 8])
        epsb = sb("epsb", [128, 1])
        for t_, d_, nm in ((condT, condT_d, "condT"), (ada_bT, ada_bT_d, "ada_bT"), (g1T, g1T_d, "g1T"),
                           (g2T, g2T_d, "g2T"), (lbp, lbp_d, "lbp"), (hgn, hgn_d, "hgn"),
                           (glan, glan_d, "glan"), (baT, nbaT_d, "baT")):
            S.add("sp", lambda e, t_=t_, d_=d_: e.dma_start(out=t_[:], in_=d_), w=[nm], dma=True)
        S.add("pool", lambda e: e.memset(epsb[:], EPS), w=["epsb"])
        S.add("act", lambda e: e.activation(out=scT[:], in_=condT[:], func=AF.Silu), r=["condT"], w=["scT"])
        S.add("dve", lambda e: e.tensor_sub(out=lb[:], in0=lbp[:, 0:8], in1=lbp[:, 8:16]), r=["lbp"], w=["lb"])
        S.add("act", lambda e: e.activation(out=lb[:], in_=lb[:], func=AF.Sigmoid), r=["lb"], w=["lb"])
        S.add("dve", lambda e: e.tensor_scalar(out=oml[:], in0=lb[:], scalar1=-1.0, scalar2=1.0,
                                               op0=ALU.mult, op1=ALU.add), r=["lb"], w=["oml"])
        S.add("dve", lambda e: e.tensor_single_scalar(out=noml[:], in_=oml[:], scalar=-1.0, op=ALU.mult),
              r=["oml"], w=["noml"])
        S.add("dve", lambda e: e.tensor_single_scalar(out=nbaT[:], in_=baT[:], scalar=-1.0, op=ALU.mult),
              r=["baT"], w=["nbaT"])

        def pipeline(stages, n, skews):
            for i in range(n + max(skews)):
                for stg, sk in zip(stages, skews):
                    if 0 <= i - sk < n:
                        stg(i - sk)

        hT = sb("hT", [128, 8, TA], BF16)
        ydram = nc.dram_tensor("yscr", [TO, 2 * D], BF16).ap()
        ssq = sb("ssq", [128, NT_A])
        rstd = sb("rstd", [128, NT_A])
        mv = modT[:].rearrange("p (j c) -> p j c", c=2)

        def modkey(j):
            return "modT0" if j < 16 else "modT1"

        def modcol(j0, dc, c):
            return modT[:, (j0 + dc) * 2 + c:(j0 + dc) * 2 + c + 1]

        def cond_of_tile(ti):
            return 1 if 8 <= ti < 12 else 0

        with ExitStack() as st01:
            adaw = [st01.enter_context(nc.sbuf_tensor("adaw%d" % i, [128, 8, 512], F32)) for i in range(3)]
            scTb = st01.enter_context(nc.sbuf_tensor("scTb", [128, 16], BF16))
            modrow = st01.enter_context(nc.sbuf_tensor("modrow", [2, 6 * D], F32))
            xts = [st01.enter_context(nc.sbuf_tensor("xt%d" % i, [128, D], F32)) for i in range(3)]
            xns = [st01.enter_context(nc.sbuf_tensor("xn%d" % i, [128, D], BF16)) for i in range(3)]
            adv = ada_w_d.rearrange("(dc p) n -> p dc n", p=128)
            S.add("dve", lambda e: e.tensor_copy(out=scTb[:], in_=scT[:]), r=["scT"], w=["scTb"])

            def mod_block(blk):
                a = adaw[blk % 3]
                S.add("sp", lambda e: e.dma_start(out=a[:], in_=adv[:, :, blk * 512:(blk + 1) * 512]),
                      w=[("adaw", blk % 3)], dma=True)

                def f(e):
                    ins = None
                    for dc in range(8):
                        ins = e.matmul(psb[0][0:2, 0:512], lhsT=scT[:, dc * 2:dc * 2 + 2], rhs=a[:, dc, :],
                                       start=(dc == 0), stop=(dc == 7))
                    return ins
                S.add("pe", f, r=[("adaw", blk % 3), "scT"], w=[PSK(0)])
                S.add("dve", lambda e: e.tensor_copy(out=modrow[0:2, blk * 512:(blk + 1) * 512], in_=psb[0][0:2, 0:512]),
                      r=[PSK(0)], w=[("modrow", blk)])

                def ft(e):
                    ins = None
                    for j4 in range(4):
                        jc = blk * 4 + j4
                        ins = e.transpose(psb[7][:, jc * 2:jc * 2 + 2], modrow[0:2, jc * 128:(jc + 1) * 128], ident_f[0:2, 0:2])
                    return ins
                S.add("pe", ft, r=[("modrow", blk), "ident_f"], w=[PSK(7)])

            def mod_evac(j_lo, j_hi, key):
                for c in range(2):
                    S.add("dve", lambda e, c=c: e.tensor_tensor(
                        out=mv[:, j_lo:j_hi, c], in0=psb[7][:, 0:96].rearrange("p (j c) -> p j c", c=2)[:, j_lo:j_hi, c],
                        in1=ada_bT[:, j_lo:j_hi], op=ALU.add), r=[PSK(7), "ada_bT"], w=[key])

            for blk in range(4):
                mod_block(blk)
            mod_evac(0, 16, "modT0")
            Av = A1[:].rearrange("p (j c) -> p j c", c=2)
            for c in range(2):
                S.add("dve", lambda e, c=c: e.scalar_tensor_tensor(
                    out=Av[:, :, c], in0=mv[:, 8:16, c], scalar=1.0, in1=g1T[:],
                    op0=ALU.add, op1=ALU.mult), r=["modT0", "g1T"], w=["A1"])

            def p1_stage1(ti):
                xt = xts[ti % 3]
                xn = xns[ti % 3]
                S.add("sp", lambda e: e.dma_start(out=xt[:], in_=x_all[ti * 128:(ti + 1) * 128, :]),
                      w=[("xt", ti % 3)], dma=True)
                S.add("act", lambda e: e.activation(out=xn[:], in_=xt[:], func=AF.Square, accum_out=ssq[:, ti:ti + 1]),
                      r=[("xt", ti % 3)], w=[("xn", ti % 3), ("ssq", ti)])
                S.add("act", lambda e: e.activation(out=rstd[:, ti:ti + 1], in_=ssq[:, ti:ti + 1], func=AF.Sqrt,
                                                    bias=epsb[:], scale=1.0 / D),
                      r=[("ssq", ti), "epsb"], w=[("rstd", ti)])
                S.add("dve", lambda e: e.reciprocal(out=rstd[:, ti:ti + 1], in_=rstd[:, ti:ti + 1]),
                      r=[("rstd", ti)], w=[("rstd", ti)])
                S.add("dve", lambda e: e.tensor_scalar(out=xn[:], in0=xt[:], scalar1=rstd[:, ti:ti + 1], scalar2=None, op0=ALU.mult),
                      r=[("xt", ti % 3), ("rstd", ti)], w=[("xn", ti % 3)])

            def p1_stage2(ti):
                xn = xns[ti % 3]
                bank = 1 + (ti % 2)
                c = cond_of_tile(ti)
                pbv = psb[bank][:].bitcast(BF16)

                def ftr(e):
                    ins = None
                    for dc in range(8):
                        ins = e.transpose(pbv[:, dc * 128:(dc + 1) * 128], xn[:, dc * 128:(dc + 1) * 128], ident_b[:])
                    return ins
                S.add("pe", ftr, r=[("xn", ti % 3), "ident_b"], w=[PSK(bank)])
                for dc in range(8):
                    a_ap = A1[:, dc * 2 + c:dc * 2 + c + 1]
                    s_ap = modcol(0, dc, c)
                    o_ap = hT[:, dc, ti * 128:(ti + 1) * 128]
                    i_ap = pbv[:, dc * 128:(dc + 1) * 128]
                    if ti % 2 == 0:
                        S.add("dve", lambda e, o_ap=o_ap, i_ap=i_ap, a_ap=a_ap, s_ap=s_ap: e.tensor_scalar(
                            out=o_ap, in0=i_ap, scalar1=a_ap, scalar2=s_ap, op0=ALU.mult, op1=ALU.add),
                            r=[PSK(bank), "A1", "modT0"], w=[("hT", ti)])
                    else:
                        S.add("act", lambda e, o_ap=o_ap, i_ap=i_ap, a_ap=a_ap, s_ap=s_ap: e.activation(
                            out=o_ap, in_=i_ap, func=AF.Identity, bias=s_ap, scale=a_ap),
                            r=[PSK(bank), "A1", "modT0"], w=[("hT", ti)])

            def p1_mod(ti):
                if ti < 8:
                    mod_block(4 + ti)
                if ti == 8:
                    mod_evac(16, 48, "modT1")
                    Av2 = A2[:].rearrange("p (j c) -> p j c", c=2)
                    for c in range(2):
                        S.add("dve", lambda e, c=c: e.scalar_tensor_tensor(
                            out=Av2[:, :, c], in0=mv[:, 32:40, c], scalar=1.0, in1=g2T[:],
                            op0=ALU.add, op1=ALU.mult), r=["modT1", "g2T"], w=["A2"])
            pipeline([p1_stage1, p1_mod, p1_stage2], NT_A, [0, 0, 2])

        st2 = st.enter_context(ExitStack())
        S.barrier()

        def sb2(name, shape, dt=F32):
            return st2.enter_context(nc.sbuf_tensor("s_" + name, list(shape), dt))

        W = sb2("W", [128, 8, 768], BF16)
        NSET = 4
        B1 = sb2("B1", [128, NSET, 512])
        B2 = sb2("B2", [128, NSET, 512])
        B3 = sb2("B3", [128, NSET, 512])
        q_bf = sb2("q_bf", [128, TO], BF16)
        k_bf = sb2("k_bf", [128, 2048], BF16)
        k_o = sb2("k_o", [128, TOTH], BF16)
        qI = [sb2("qI%d" % d, [128, TO], BF16) for d in range(2)]
        qX2 = [sb2("qX2%d" % d, [128, TO], BF16) for d in range(2)]
        kX1 = [sb2("kX1%d" % d, [128, TO], BF16) for d in range(2)]
        kX2 = [sb2("kX2%d" % d, [128, TO], BF16) for d in range(2)]
        kSb = sb2("kSb", [128, NSET, 512], BF16)
        kS_tm = [sb2("kStm0", [128, NT_O, 128], BF16), sb2("kStm1", [128, NT_A, 128], BF16)]
        Dd = [sb2("Dd%d" % d, [128, 40]) for d in range(2)]
        Eo = sb2("Eo", [128, 8 * NSET, 2])
        v_tm = sb2("v_tm", [128, NT_A, 256], BF16)
        G_tm = sb2("G_tm", [128, NT_O, 256], BF16)
        S32 = sb2("S32", [128, 6, 256])
        S_bf = [sb2("S_bf0", [128, 4, 256], BF16), sb2("S_bf1", [128, 24, 256], BF16)]
        scS = sb2("scS", [128, 512], BF16)
        y_tm = [sb2("y_tm%d" % i, [128, 256], BF16) for i in range(4)]
        ssq2 = sb2("ssq2", [128, 160])
        rs2 = sb2("rs2", [128, 160])
        gaT = sb2("gaT", [64, TA], BF16)
        wa_bf = sb2("wa_bf", [64, 512], BF16)
        junk2 = sb2("junk2", [128, 256], BF16)

        for d in range(2):
            for i_, (arr, nm) in enumerate(((qX2[d], "qX2"), (kX1[d], "kX1"), (kX2[d], "kX2"))):
                S.add("dve" if (i_ + d) % 2 == 0 else "pool", lambda e, arr=arr: e.memset(arr[:], 0.0), w=[(nm, d)])
        S.add("pool", lambda e: e.memset(ssq2[:], 0.0), w=["ssq2"])
        for d in range(2):
            S.add("pool", lambda e, d=d: e.dma_start(out=wa_bf[d * 32:d * 32 + 16, :], in_=wa_up_d[d]),
                  w=[("wa_bf", d)], dma=True)

        GEO = [dict(fh=slice(0, 32), sh=slice(32, 64), pf=31, pse=63),
               dict(fh=slice(32, 64), sh=slice(0, 32), pf=32, pse=0)]
        cm_ctr = [0]
        tm_ctr = [0]
        st_ctr = [0]

        def c64(ap):
            return ap.rearrange("p (c j) -> p c j", j=64)

        def cmaj(col0, tok0, ntok, consume, M=128, wkey="Wc", wsrc=None, pbase=0):
            bank = cm_ctr[0] % 2
            cm_ctr[0] += 1
            src = W if wsrc is None else wsrc

            def f(e):
                ins = None
                for dc in range(8):
                    ins = e.matmul(psb[bank][pbase:pbase + M, 0:ntok], lhsT=src[:, dc, col0:col0 + M],
                                   rhs=hT[:, dc, tok0:tok0 + ntok], start=(dc == 0), stop=(dc == 7))
                return ins
            tiles = range(tok0 // 128, (tok0 + ntok) // 128)
            S.add("pe", f, r=[wkey] + [("hT", t) for t in tiles], w=[PSK(bank)])
            consume(psb[bank][pbase:pbase + M, 0:ntok], PSK(bank))

        def gating(d, n, tokbase, gbase, k_ap, kkey, se, own, par):
            g = GEO[d]
            fh, sh, pf, pse = g["fh"], g["sh"], g["pf"], g["pse"]
            ncnk = n // 64
            b1, b2, b3 = B1[:, par, 0:n], B2[:, par, 0:n], B3[:, par, 0:n]
            k1, k2, k3 = ("B1", par), ("B2", par), ("B3", par)
            c32v = c64(b3)
            e32v = c64(b1)
            iev = c64(b2)
            kv = c64(k_ap)
            Dv = Dd[d][:, gbase:gbase + ncnk]
            Dv3 = Dv.rearrange("p (c o) -> p c o", o=1)
            bc = lambda ap: ap.to_broadcast([128, ncnk, 32])
            S.add("act", lambda e: e.activation(out=b2, in_=b3, func=AF.Exp, scale=-se), r=[k3], w=[k2])
            if own:
                S.add("act", lambda e: e.activation(out=b1, in_=b3, func=AF.Exp, scale=se), r=[k3], w=[k1])
                EF = e32v[:, :, pf:pf + 1]
                ES = e32v[:, :, pse:pse + 1]
                ekey = k1
            else:
                eo = Eo[:, par * 8:par * 8 + ncnk, :]
                S.add("act", lambda e: e.activation(out=eo[:, :, 0:1], in_=c32v[:, :, pf:pf + 1], func=AF.Exp, scale=se),
                      r=[k3], w=[("Eo", par)])
                S.add("act", lambda e: e.activation(out=eo[:, :, 1:2], in_=c32v[:, :, pse:pse + 1], func=AF.Exp, scale=se),
                      r=[k3], w=[("Eo", par)])
                EF = eo[:, :, 0:1]
                ES = eo[:, :, 1:2]
                ekey = ("Eo", par)
            S.add("dve", lambda e: e.tensor_tensor(out=Dv3, in0=EF, in1=ES, op=ALU.mult), r=[ekey], w=[("Dd", d)])
            if own:
                qv = c64(q_bf[:, tokbase:tokbase + n])
                for (eng, dst, nm, a_, b_, hs) in (
                        ("pool", kX1[d], "kX1", kv, iev, fh), ("pool", kX2[d], "kX2", kv, iev, sh),
                        ("dve", qX2[d], "qX2", qv, e32v, sh), ("dve", qI[d], "qI", qv, e32v, fh)):
                    dv_ = c64(dst[:, tokbase:tokbase + n])
                    S.add(eng, lambda e, dv_=dv_, a_=a_, b_=b_, hs=hs: e.tensor_tensor(
                        out=dv_[:, :, hs], in0=a_[:, :, hs], in1=b_[:, :, hs], op=ALU.mult),
                        r=[kkey, "q_bf", k1, k2], w=[(nm, d)])
            S.add("pool", lambda e: e.tensor_tensor(out=iev[:, :, fh], in0=iev[:, :, fh], in1=bc(Dv3), op=ALU.mult),
                  r=[k2, ("Dd", d)], w=[k2])
            S.add("pool", lambda e: e.tensor_tensor(out=iev[:, :, sh], in0=iev[:, :, sh], in1=bc(ES), op=ALU.mult),
                  r=[k2, ekey], w=[k2])
            if own:
                S.add("dve", lambda e: e.tensor_tensor(out=e32v[:, :, sh], in0=e32v[:, :, sh], in1=bc(EF), op=ALU.mult),
                      r=[k1], w=[k1])
                dv_ = c64(qI[d][:, tokbase:tokbase + n])
                S.add("dve", lambda e: e.tensor_tensor(out=dv_[:, :, sh], in0=qv[:, :, sh], in1=e32v[:, :, sh], op=ALU.mult),
                      r=["q_bf", k1], w=[("qI", d)])
            ksb = kSb[:, par, 0:n]
            S.add("dve", lambda e: e.tensor_tensor(out=ksb, in0=k_ap, in1=b2, op=ALU.mult), r=[kkey, k2], w=[("kSb", par)])

        def ks_transpose(d, n, tokbase, par):
            ksb = kSb[:, par, 0:n]
            nt = n // 128
            tile0 = tokbase // 128
            pbv = psb[4][:].bitcast(BF16)

            def ftr(e):
                ins = None
                for j in range(nt):
                    ins = e.transpose(pbv[:, j * 128:(j + 1) * 128], ksb[:, j * 128:(j + 1) * 128], ident_b[:])
                return ins
            S.add("pe", ftr, r=[("kSb", par), "ident_b"], w=[PSK(4)])
            S.add("act", lambda e: e.copy(out=kS_tm[d][:, tile0:tile0 + nt, :].rearrange("p a b -> p (a b)"),
                                          in_=pbv[:, 0:nt * 128]), r=[PSK(4)], w=[("kStm", d)])

        def seg_scan(d, n, par):
            b2, b3 = B2[:, par, 0:n], B3[:, par, 0:n]
            if d == 0:
                S.add("dve", lambda e: e.tensor_tensor_scan(out=b3, data0=mask32[:, 0:n], data1=b2,
                                                            initial=0.0, op0=ALU.mult, op1=ALU.add),
                      r=[("B2", par), "mask32"], w=[("B3", par)])
            else:
                S.add("dve", lambda e: e.tensor_tensor_scan(out=b3[:, ::-1], data0=mask32[:, 0:n], data1=b2[:, ::-1],
                                                            initial=0.0, op0=ALU.mult, op1=ALU.add),
                      r=[("B2", par), "mask32"], w=[("B3", par)])

        def tok_of_chunk(g):
            return g * 64

        zero_state = set()

        def state_step(dv, ci, d, g, first_zero, copy_to=None):
            ti, hf = g // 2, g % 2
            pr = slice(hf * 64, hf * 64 + 64)
            bank = 5 + st_ctr[0] % 2
            st_ctr[0] += 1
            pS = psb[bank][:, 0:dv]
            pkey = PSK(bank)
            S.add("pe", lambda e: e.matmul(pS, lhsT=kS_tm[d][pr, ti, :], rhs=v_tm[pr, ti, 0:dv], start=True, stop=True),
                  r=[("kStm", d), ("v_tm", ti)], w=[pkey])
            if copy_to is not None:
                if first_zero:
                    zero_state.add((d, g))
                else:
                    S.add("act", lambda e: e.copy(out=copy_to[0], in_=S32[:, ci, 0:dv]), r=[("S32", ci)], w=[copy_to[1]])
            if first_zero:
                S.add("dve", lambda e: e.tensor_copy(out=S32[:, ci, 0:dv], in_=pS), r=[pkey], w=[("S32", ci)])
            else:
                S.add("dve", lambda e: e.scalar_tensor_tensor(
                    out=S32[:, ci, 0:dv], in0=S32[:, ci, 0:dv], scalar=Dd[d][:, g:g + 1], in1=pS,
                    op0=ALU.mult, op1=ALU.add), r=[pkey, ("S32", ci), ("Dd", d)], w=[("S32", ci)])

        def sweep_dir1(kind, h, dv):
            s0d = s0_hg_d if kind == "hg" else s0_gla_d
            std = st_hg if kind == "hg" else st_gla
            zero_state.clear()
            ch1 = [dict(ci=3, seq=list(range(39, 23, -1)) + list(range(15, -1, -1)), init=1, prompt=None),
                   dict(ci=4, seq=list(range(19, 15, -1)), init=None, prompt=0),
                   dict(ci=5, seq=list(range(23, 19, -1)), init=None, prompt=1)]
            S.add("sp", lambda e: e.dma_start(out=S32[:, 3, 0:dv], in_=s0d[1, h]), w=[("S32", 3)], dma=True)
            S.add("sp", lambda e: e.dma_start(out=S32[:, 0, 0:dv], in_=s0d[0, h]), w=[("S32", 0)], dma=True)
            for step in range(32):
                for ch in ch1:
                    if step >= len(ch["seq"]):
                        continue
                    g = ch["seq"][step]
                    ci = ch["ci"]
                    fz = ch["init"] is None and step == 0
                    cp = (S_bf[1][:, g, 0:dv], ("S_bf", 1, g)) if g < 24 else None
                    state_step(dv, ci, 1, g, fz, cp)
                    if ch["prompt"] is not None and step == len(ch["seq"]) - 1:
                        S.add("sp", lambda e, ci=ci, ch=ch: e.dma_start(out=std[ch["prompt"], 1, h], in_=S32[:, ci, 0:dv]),
                              r=[("S32", ci)], dma=True)
                if step % 4 == 3:
                    yield

        def dir0_and_output(kind, h, dv, ychunk0):
            std = st_hg if kind == "hg" else st_gla
            pscb = psb[7]
            pbv = psb[4][:].bitcast(BF16)
            nvc = dv // 128

            def stageA(ti):
                ci = 0 if ti < 8 else (1 if ti < 10 else 2)
                for hf in range(2):
                    g = 2 * ti + hf
                    fz = ci > 0 and g in (16, 20)
                    rs = g % 4
                    state_step(dv, ci, 0, g, fz, (S_bf[0][:, rs, 0:dv], ("S_bf", 0, rs)))
                if ti in (9, 11):
                    S.add("sp", lambda e, ci=ci: e.dma_start(out=std[ci - 1, 0, h], in_=S32[:, ci, 0:dv]),
                          r=[("S32", ci)], dma=True)
                slot = ti % 4

                def fsc(e):
                    ins = None
                    for hf in range(2):
                        g = 2 * ti + hf
                        tk = slice(g * 64, g * 64 + 64)
                        pr = slice(hf * 64, hf * 64 + 64)
                        for d in range(2):
                            o_ap = pscb[pr, slot * 128 + d * 64:slot * 128 + d * 64 + 64]
                            e.matmul(o_ap, lhsT=kX1[d][:, tk], rhs=qI[d][:, tk], start=True, stop=False)
                            ins = e.matmul(o_ap, lhsT=kX2[d][:, tk], rhs=qX2[d][:, tk], start=False, stop=True)
                    return ins
                S.add("pe", fsc, r=[("kX1", 0), ("kX1", 1), ("kX2", 0), ("kX2", 1), ("qI", 0), ("qI", 1),
                                    ("qX2", 0), ("qX2", 1)], w=[PSK(7)])
                S.add("dve", lambda e: e.tensor_tensor(
                    out=scS[:, slot * 128:(slot + 1) * 128], in0=pscb[:, slot * 128:(slot + 1) * 128],
                    in1=trimask[:, slot * 128:(slot + 1) * 128], op=ALU.mult),
                    r=[PSK(7), "trimask"], w=[("scS", slot)])

            def stageB(ti):
                slot = ti % 4
                ob = 2 + (ti % 2)
                po = psb[ob][:, 0:dv]

                def fo(e):
                    ins = None
                    for hf in range(2):
                        g = 2 * ti + hf
                        tk = slice(g * 64, g * 64 + 64)
                        pr = slice(hf * 64, hf * 64 + 64)
                        mms = []
                        if (0, g) not in zero_state:
                            mms.append((qI[0][:, tk], S_bf[0][:, g % 4, 0:dv]))
                        if (1, g) not in zero_state:
                            mms.append((qI[1][:, tk], S_bf[1][:, g, 0:dv]))
                        for d in range(2):
                            mms.append((scS[pr, slot * 128 + d * 64:slot * 128 + d * 64 + 64], v_tm[pr, ti, 0:dv]))
                        for i, (l, r_) in enumerate(mms):
                            ins = e.matmul(psb[ob][pr, 0:dv], lhsT=l, rhs=r_, start=(i == 0), stop=(i == len(mms) - 1))
                    return ins
                S.add("pe", fo, r=[("qI", 0), ("qI", 1), ("scS", slot), ("v_tm", ti)] +
                      [("S_bf", 0, (2 * ti + hf) % 4) for hf in range(2)] +
                      [("S_bf", 1, 2 * ti + hf) for hf in range(2)], w=[PSK(ob)])
                si = (ychunk0 if kind == "hg" else 8 + h) * 12 + ti
                sc_ = ssq2[:, si:si + 1]
                rc_ = rs2[:, si:si + 1]
                S.add("act", lambda e: e.activation(out=junk2[:, 0:dv], in_=po, func=AF.Square, accum_out=sc_),
                      r=[PSK(ob)], w=["junk2", ("ssq2", ti % 2)])
                S.add("act", lambda e: e.activation(out=rc_, in_=sc_, func=AF.Sqrt, bias=epsb[:], scale=1.0 / dv),
                      r=[("ssq2", ti % 2), "epsb"], w=[("rs2", ti % 2)])
                S.add("dve", lambda e: e.reciprocal(out=rc_, in_=rc_), r=[("rs2", ti % 2)], w=[("rs2", ti % 2)])
                yt = y_tm[ti % 4]
                S.add("dve", lambda e: e.scalar_tensor_tensor(
                    out=yt[:, 0:dv], in0=po, scalar=rc_, in1=G_tm[:, ti, 0:dv], op0=ALU.mult, op1=ALU.mult),
                    r=[PSK(ob), ("rs2", ti % 2), ("G_tm", ti)], w=[("y_tm", ti % 4)])

            def stageC(ti):
                yt = y_tm[ti % 4]
                S.add("sp", lambda e: e.dma_start(out=ydram[ti * 128:(ti + 1) * 128, ychunk0 * 128:ychunk0 * 128 + dv], in_=yt[:, 0:dv]),
                      r=[("y_tm", ti % 4)], dma=True)

            for i in range(NT_O + 2):
                if i < NT_O:
                    stageA(i)
                if 0 <= i - 1 < NT_O:
                    stageB(i - 1)
                if 0 <= i - 2 < NT_O:
                    stageC(i - 2)

        def tmaj_pass(col_v, ncol_v, col_g, ncol_g):
            for ti in range(NT_A):
                own = ti < NT_O
                bank = 2 + tm_ctr[0] % 2
                tm_ctr[0] += 1
                ncol = ncol_v + (ncol_g if own else 0)

                def f(e, ti=ti, bank=bank, ncol=ncol):
                    ins = None
                    for dc in range(8):
                        ins = e.matmul(psb[bank][:, 0:ncol], lhsT=hT[:, dc, ti * 128:(ti + 1) * 128],
                                       rhs=W[:, dc, col_v:col_v + ncol], start=(dc == 0), stop=(dc == 7))
                    return ins
                S.add("pe", f, r=["Wt", ("hT", ti)], w=[PSK(bank)])
                S.add("act", lambda e, ti=ti, bank=bank: e.copy(out=v_tm[:, ti, 0:ncol_v], in_=psb[bank][:, 0:ncol_v]),
                      r=[PSK(bank)], w=[("v_tm", ti)])
                if own:
                    S.add("act", lambda e, ti=ti, bank=bank: e.activation(
                        out=G_tm[:, ti, 0:ncol_g], in_=psb[bank][:, ncol_v:ncol_v + ncol_g], func=AF.Silu),
                        r=[PSK(bank)], w=[("G_tm", ti)])

        PASSES = ([(1, TO + i * 512, 512, 24 + i * 8, False) for i in range(2)] +
                  [(1, i * 512, 512, i * 8, True) for i in range(3)] +
                  [(0, i * 512, 512, i * 8, True) for i in range(3)])
        N_D1 = 5

        def run_passes(p0, p1, p2_, sweep):
            n = len(PASSES)
            p0(0)
            sw = None
            for i in range(n + 2):
                if i + 1 < n:
                    p0(i + 1)
                if i < n:
                    p1(i)
                if 0 <= i - 1 < n:
                    p2_(i - 1)
                if 0 <= i - 2 < n:
                    d, tok0, nn, gb, own = PASSES[i - 2]
                    ks_transpose(d, nn, tok0, (i - 2) % NSET)
                    if i - 2 == N_D1 - 1:
                        sw = sweep()
                if sw is not None:
                    for _ in range(2):
                        next(sw, None)
            if sw is not None:
                for _ in sw:
                    pass

        def hg_head(h):
            wv = w_hg_d[h].rearrange("(dc p) n -> p dc n", p=128)
            S.add("pool", lambda e: e.dma_start(out=W[:, :, 0:384], in_=wv[:, :, 0:384]), w=["Wc"], dma=True)
            S.add("pool", lambda e: e.dma_start(out=W[:, :, 384:640], in_=wv[:, :, 384:640]), w=["Wt"], dma=True)
            for gi in range(3):
                cmaj(0, gi * 512, 512, lambda ps, pk, gi=gi: S.add(
                    "act", lambda e: e.activation(out=q_bf[:, gi * 512:(gi + 1) * 512], in_=ps, func=AF.Copy, scale=128 ** -0.5),
                    r=[pk], w=["q_bf"]))
            tmaj_pass(384, 128, 512, 128)

            pbank = {}

            def p0(pi):
                d, tok0, n, gb, own = PASSES[pi]
                col = 128 if d == 0 else 256
                cmaj(col, tok0, 512, lambda ps, pk: pbank.__setitem__(pi, (ps, pk)))

            def p1(pi):
                d, tok0, n, gb, own = PASSES[pi]
                par = pi % NSET
                ps, pk = pbank[pi]
                b1, b2, b3 = B1[:, par, :], B2[:, par, :], B3[:, par, :]
                k1, k2, k3 = ("B1", par), ("B2", par), ("B3", par)
                S.add("act", lambda e: e.activation(out=b1, in_=ps, func=AF.Exp, scale=-1.0), r=[pk], w=[k1])
                S.add("act", lambda e: e.activation(out=b3, in_=b1, func=AF.Ln, bias=one_c[:], scale=1.0), r=[k1, "one_c"], w=[k3])
                S.add("act", lambda e: e.activation(out=b2, in_=b1, func=AF.Ln, bias=one_c[:], scale=lb[:, h:h + 1]),
                      r=[k1, "one_c", "lb"], w=[k2])
                S.add("dve", lambda e: e.tensor_sub(out=b2, in0=b2, in1=b3), r=[k2, k3], w=[k2])
                S.add("act", lambda e: e.activation(out=b1, in_=b3, func=AF.Exp, scale=-1.0), r=[k3], w=[k1])
                k_ap = k_bf[:, par * 512:(par + 1) * 512]
                S.add("pool", lambda e: e.tensor_scalar(out=k_ap, in0=b1, scalar1=-1.0,
                                                        scalar2=noml[:, h:h + 1], op0=ALU.add, op1=ALU.mult),
                      r=[k1, "noml"], w=[("k_src", par)])
                seg_scan(d, n, par)

            def p2_(pi):
                d, tok0, n, gb, own = PASSES[pi]
                par = pi % NSET
                gating(d, n, tok0, gb, k_bf[:, par * 512:(par + 1) * 512], ("k_src", par), 1.0, own, par)
            run_passes(p0, p1, p2_, lambda: sweep_dir1("hg", h, 128))
            dir0_and_output("hg", h, 128, h)

        def gla_prep():
            wga = sb2("wga", [128, 8, 32], BF16)
            S.add("pool", lambda e: e.dma_start(out=wga[:], in_=w_ga_d.rearrange("(dc p) n -> p dc n", p=128)),
                  w=["wga"], dma=True)
            for d in range(2):
                ntok = TO if d == 0 else TA
                for gi in range(ntok // 512):
                    cmaj(d * 16, gi * 512, 512, lambda ps, pk, gi=gi, d=d: S.add(
                        "act", lambda e: e.copy(out=gaT[d * 32:d * 32 + 16, gi * 512:(gi + 1) * 512], in_=ps),
                        r=[pk], w=[("gaT", d)]), M=16, wkey="wga", wsrc=wga, pbase=d * 32)

        def gla_head(h):
            wv = w_gla_d[h].rearrange("(dc p) n -> p dc n", p=128)
            S.add("pool", lambda e: e.dma_start(out=W[:, :, 0:256], in_=wv[:, :, 0:256]), w=["Wc"], dma=True)
            S.add("pool", lambda e: e.dma_start(out=W[:, :, 256:768], in_=wv[:, :, 256:768]), w=["Wt"], dma=True)
            for gi in range(3):
                cmaj(0, gi * 512, 512, lambda ps, pk, gi=gi: S.add(
                    "act", lambda e: e.activation(out=q_bf[:, gi * 512:(gi + 1) * 512], in_=ps, func=AF.Copy, scale=128 ** -0.5),
                    r=[pk], w=["q_bf"]))
            for gi in range(5):
                dst = k_bf[:, gi * 512:(gi + 1) * 512] if gi < 3 else k_o[:, (gi - 3) * 512:(gi - 2) * 512]
                cmaj(128, gi * 512, 512, lambda ps, pk, dst=dst: S.add(
                    "act", lambda e: e.copy(out=dst, in_=ps), r=[pk], w=["k_gla"]))
            tmaj_pass(256, 256, 512, 256)

            pbank = {}

            def p0(pi):
                d, tok0, n, gb, own = PASSES[pi]
                bank = cm_ctr[0] % 2
                cm_ctr[0] += 1
                S.add("pe", lambda e: e.matmul(
                    psb[bank][:, 0:512], lhsT=wa_bf[d * 32:d * 32 + 16, h * 128:(h + 1) * 128],
                    rhs=gaT[d * 32:d * 32 + 16, tok0:tok0 + 512], start=True, stop=True),
                    r=[("wa_bf", d), ("gaT", d)], w=[PSK(bank)])
                pbank[pi] = bank

            def p1(pi):
                d, tok0, n, gb, own = PASSES[pi]
                par = pi % NSET
                bank = pbank[pi]
                S.add("act", lambda e: e.activation(
                    out=B1[:, par, :], in_=psb[bank][:, 0:512], func=AF.Exp,
                    bias=nbaT[:, d * 4 + h:d * 4 + h + 1], scale=-1.0),
                    r=[PSK(bank), "nbaT"], w=[("B1", par)])
                S.add("act", lambda e: e.activation(out=B2[:, par, :], in_=B1[:, par, :], func=AF.Ln, bias=one_c[:], scale=1.0),
                      r=[("B1", par), "one_c"], w=[("B2", par)])
                seg_scan(d, n, par)

            def p2_(pi):
                d, tok0, n, gb, own = PASSES[pi]
                par = pi % NSET
                k_ap = k_bf[:, tok0:tok0 + n] if own else k_o[:, tok0 - TO:tok0 - TO + n]
                gating(d, n, tok0, gb, k_ap, "k_gla", -1.0 / 16.0, own, par)
            run_passes(p0, p1, p2_, lambda: sweep_dir1("gla", h, 256))
            dir0_and_output("gla", h, 256, 8 + 2 * h)

        NH_HG = dbg.get("_nh_hg", 8)
        NH_GLA = dbg.get("_nh_gla", 4)
        for h in range(NH_HG):
            hg_head(h)
        if NH_GLA:
            gla_prep()
        for h in range(NH_GLA):
            gla_head(h)

        if dbg.get("_p2only"):
            if "modT" in dbg:
                S.add("sp", lambda e: e.dma_start(out=dbg_out["modT"], in_=modT[:]), r=["modT0", "modT1"], dma=True)
            S.emit(st)
            return nc
        st2.close()
        S.barrier()
        st3 = st.enter_context(ExitStack())

        def sb3(name, shape, dt=F32):
            return st3.enter_context(nc.sbuf_tensor("s_" + name, list(shape), dt))

        mergedT = sb3("mergedT", [128, 8, TO], BF16)
        yT = sb3("yT", [128, 16, TO], BF16)
        with ExitStack() as st3a:
            def sb3a(name, shape, dt=F32):
                return st3a.enter_context(nc.sbuf_tensor("s_" + name, list(shape), dt))
            stgA = [sb3a("stgA%d" % i, [128, 8, 128]) for i in range(2)]
            stgB = [sb3a("stgB%d" % i, [128, 8, 128]) for i in range(2)]
            wbA = [sb3a("wbA%d" % i, [128, 8, 128], BF16) for i in range(2)]
            wbB = [sb3a("wbB%d" % i, [128, 8, 128], BF16) for i in range(2)]
            wmA = [sb3a("wmA%d" % i, [128, 8, 128], BF16) for i in range(2)]
            wmB = [sb3a("wmB%d" % i, [128, 8, 128], BF16) for i in range(2)]
            sgA = [sb3a("sgA%d" % i, [128, 512]) for i in range(2)]
            sgB = [sb3a("sgB%d" % i, [128, 512]) for i in range(2)]
            tpA = [sb3a("tpA%d" % i, [128, 512]) for i in range(2)]
            tpB = [sb3a("tpB%d" % i, [128, 512]) for i in range(2)]
            S.barrier()
            ytok = [sb3a("ytok%d" % i, [128, 2 * D], BF16) for i in range(2)]
            for ti in range(NT_O):
                sl = ti % 2
                S.add("sp", lambda e, ti=ti, sl=sl: e.dma_start(out=ytok[sl][:], in_=ydram[ti * 128:(ti + 1) * 128, :]),
                      w=[("ytok", sl)], dma=True)
                for hb in range(2):
                    bank = (2 * ti + hb) % 4
                    pbv = psb[bank][:].bitcast(BF16)

                    def fyt(e, sl=sl, hb=hb, pbv=pbv):
                        ins = None
                        for c in range(8):
                            cc = hb * 8 + c
                            ins = e.transpose(pbv[:, c * 128:(c + 1) * 128], ytok[sl][:, cc * 128:(cc + 1) * 128], ident_b[:])
                        return ins
                    S.add("pe", fyt, r=[("ytok", sl), "ident_b"], w=[PSK(bank)])
                    S.add("act" if hb == 0 else "dve",
                          (lambda e, ti=ti, hb=hb, pbv=pbv: e.copy(out=yT[:, hb * 8:hb * 8 + 8, ti * 128:(ti + 1) * 128],
                                                                 in_=pbv[:, 0:1024].rearrange("p (c t) -> p c t", t=128)))
                          if hb == 0 else
                          (lambda e, ti=ti, hb=hb, pbv=pbv: e.tensor_copy(out=yT[:, hb * 8:hb * 8 + 8, ti * 128:(ti + 1) * 128],
                                                                        in_=pbv[:, 0:1024].rearrange("p (c t) -> p c t", t=128))),
                          r=[PSK(bank)], w=[("yT", c, ti) for c in range(hb * 8, hb * 8 + 8)])
            it = 0
            for ncn in range(8):
                p = ncn % 2
                S.add("sp", lambda e, p=p, ncn=ncn: e.dma_start(out=stgA[p][:], in_=w_bra_d[ncn].rearrange("(vc q) n -> q vc n", q=128)),
                      w=[("stgA", p)], dma=True)
                S.add("sp", lambda e, p=p, ncn=ncn: e.dma_start(out=stgB[p][:], in_=w_brb_d[ncn].rearrange("(vc q) n -> q vc n", q=128)),
                      w=[("stgB", p)], dma=True)
                S.add("pool", lambda e, p=p, ncn=ncn: e.dma_start(out=wmA[p][:], in_=w_m_d[ncn].rearrange("(dc q) n -> q dc n", q=128)),
                      w=[("wmA", p)], dma=True)
                S.add("pool", lambda e, p=p, ncn=ncn: e.dma_start(out=wmB[p][:], in_=w_m_d[8 + ncn].rearrange("(dc q) n -> q dc n", q=128)),
                      w=[("wmB", p)], dma=True)
                S.add("act", lambda e, p=p: e.activation(out=wbA[p][:], in_=stgA[p][:], func=AF.Copy, scale=hgn[:, 0:1]),
                      r=[("stgA", p), "hgn"], w=[("wbA", p)])
                for v2 in range(2):
                    S.add("act", lambda e, p=p, v2=v2: e.activation(
                        out=wbB[p][:].rearrange("q (h v) n -> q h v n", v=2)[:, :, v2, :],
                        in_=stgB[p][:].rearrange("q (h v) n -> q h v n", v=2)[:, :, v2, :],
                        func=AF.Copy, scale=glan[:, v2:v2 + 1]),
                        r=[("stgB", p), "glan"], w=[("wbB", p)])
                for tg in range(3):
                    b0 = 4 * (it % 2)
                    q = it % 2
                    it += 1
                    tk = slice(tg * 512, (tg + 1) * 512)
                    tiles = [("hT", t) for t in range(tg * 4, tg * 4 + 4)]
                    ytl = lambda c0: [("yT", c, t) for c in range(c0, c0 + 8) for t in range(tg * 4, tg * 4 + 4)]

                    def mm8(e, bank, wt, src, c0, tk=tk):
                        ins = None
                        for c in range(8):
                            ins = e.matmul(psb[bank][:, 0:512], lhsT=wt[:, c, :], rhs=src[:, c0 + c, tk],
                                           start=(c == 0), stop=(c == 7))
                        return ins
                    S.add("pe", lambda e, b0=b0, p=p, mm8=mm8: mm8(e, b0 + 2, wmA[p], hT, 0), r=[("wmA", p)] + tiles, w=[PSK(b0 + 2)])
                    S.add("pe", lambda e, b0=b0, p=p, mm8=mm8: mm8(e, b0 + 3, wmB[p], hT, 0), r=[("wmB", p)] + tiles, w=[PSK(b0 + 3)])
                    S.add("pe", lambda e, b0=b0, p=p, mm8=mm8: mm8(e, b0 + 0, wbA[p], yT, 0), r=[("wbA", p)] + ytl(0), w=[PSK(b0 + 0)])
                    S.add("pe", lambda e, b0=b0, p=p, mm8=mm8: mm8(e, b0 + 1, wbB[p], yT, 8), r=[("wbB", p)] + ytl(8), w=[PSK(b0 + 1)])
                    S.add("act", lambda e, b0=b0, q=q: e.activation(out=sgA[q][:], in_=psb[b0 + 2][:, 0:512], func=AF.Sigmoid),
                          r=[PSK(b0 + 2)], w=[("sgA", q)])
                    S.add("act", lambda e, b0=b0, q=q: e.activation(out=sgB[q][:], in_=psb[b0 + 3][:, 0:512], func=AF.Sigmoid),
                          r=[PSK(b0 + 3)], w=[("sgB", q)])
                    S.add("dve", lambda e, b0=b0, q=q: e.tensor_tensor(out=tpA[q][:], in0=psb[b0 + 0][:, 0:512], in1=sgA[q][:], op=ALU.mult),
                          r=[PSK(b0 + 0), ("sgA", q)], w=[("tpA", q)])
                    S.add("dve", lambda e, b0=b0, q=q: e.tensor_tensor(out=tpB[q][:], in0=psb[b0 + 1][:, 0:512], in1=sgB[q][:], op=ALU.mult),
                          r=[PSK(b0 + 1), ("sgB", q)], w=[("tpB", q)])
                    S.add("pool", lambda e, q=q, ncn=ncn, tk=tk: e.tensor_tensor(out=mergedT[:, ncn, tk], in0=tpA[q][:], in1=tpB[q][:], op=ALU.add),
                          r=[("tpA", q), ("tpB", q)], w=[("mergedT", ncn, tg)])
        if "mergedT" in dbg:
            S.add("sp", lambda e: e.dma_start(out=dbg_out["mergedT"], in_=mergedT[:]),
                  r=[("mergedT", n_, t_) for n_ in range(8) for t_ in range(3)], dma=True)
        S.barrier()
        x1s = yT[:].bitcast(F32).rearrange("p a b -> p (a b)").rearrange("p (t d) -> p t d", d=D)
        hflat = hT[:].rearrange("p a b -> p (a b)")
        h2T = hflat[:, 0:8 * TO].rearrange("p (a b) -> p a b", b=TO)
        cwT = sb3("cwT", [32, TO])
        ssq3 = sb3("ssq3", [128, NT_O])
        rs3 = sb3("rs3", [128, NT_O])
        with ExitStack() as st3b:
            def sb3b(name, shape, dt=F32):
                return st3b.enter_context(nc.sbuf_tensor("s_" + name, list(shape), dt))
            wout = sb3b("wout", [128, 8, D], BF16)
            wrt = sb3b("wrt", [128, 8, 36])
            g1bc = [sb3b("g1bc%d" % c, [128, D]) for c in range(2)]
            ones32 = sb3b("ones32", [128, 128])
            dg = [sb3b("dg%d" % i, [128, 128]) for i in range(2)]
            xr = [sb3b("xr%d" % i, [128, D]) for i in range(2)]
            tmpx = [sb3b("tmpx%d" % i, [128, D]) for i in range(2)]
            xn2 = [sb3b("xn2_%d" % i, [128, D]) for i in range(2)]
            h2f = [sb3b("h2f%d" % i, [128, 8, 128]) for i in range(2)]
            junk3 = sb3b("junk3", [128, D], BF16)
            NT = NT_O
            lga = sb3b("lga", [128, NT, 36])
            cwa = sb3b("cwa", [128, NT, 32])
            rA = sb3b("rA", [128, NT, 4])
            rB = sb3b("rB", [128, NT, 4])
            rC = sb3b("rC", [128, NT, 32])
            rsel = sb3b("rsel", [128, NT, 8])
            rsel2 = sb3b("rsel2", [128, NT, 8])
            rmk1 = sb3b("rmk1", [128, NT, 8])
            rmk2 = sb3b("rmk2", [128, NT, 8])
            rcw8 = sb3b("rcw8", [128, NT, 8])
            rs = sb3b("rs", [128, 10, NT])
            S.add("pool", lambda e: e.dma_start(out=wout[:], in_=w_out_d.rearrange("(c q) j -> q c j", q=128)), w=["wout"], dma=True)
            S.add("sp", lambda e: e.dma_start(out=wrt[:], in_=w_rt_d.rearrange("(c q) j -> q c j", q=128)), w=["wrt"], dma=True)
            S.add("pool", lambda e: e.memset(ones32[:], 1.0), w=["ones32"])

            def bcast_tile(dst, j0, c, nm):
                for jc in range(8):
                    d_ = dg[jc % 2]
                    S.add("dve", lambda e, d_=d_, jc=jc: e.tensor_scalar(out=d_[:], in0=ident_f[:], scalar1=modcol(j0, jc, c),
                                                                         scalar2=None, op0=ALU.mult),
                          r=["ident_f", "modT1"], w=[("dg", jc % 2)])
                    bank = jc % 2
                    S.add("pe", lambda e, d_=d_, bank=bank: e.matmul(psb[bank][:, 0:128], lhsT=ones32[:], rhs=d_[:], start=True, stop=True),
                          r=["ones32", ("dg", jc % 2)], w=[PSK(bank)])
                    S.add("act", lambda e, bank=bank, jc=jc: e.copy(out=dst[:, jc * 128:(jc + 1) * 128], in_=psb[bank][:, 0:128]),
                          r=[PSK(bank)], w=[nm])
            for c in range(2):
                bcast_tile(g1bc[c], 16, c, ("g1bc", c))

            def s3b_1(ti):
                c = cond_of_tile(ti)
                sl = ti % 2
                S.add("sp", lambda e, ti=ti, sl=sl: e.dma_start(out=xr[sl][:], in_=x_all[ti * 128:(ti + 1) * 128, :]),
                      w=[("xr", sl)], dma=True)
                for jh in range(2):
                    bank = 2 + jh

                    def fm(e, ti=ti, jh=jh, bank=bank):
                        ins = None
                        for n_ in range(8):
                            ins = e.matmul(psb[bank][:, 0:512], lhsT=mergedT[:, n_, ti * 128:(ti + 1) * 128],
                                           rhs=wout[:, n_, jh * 512:(jh + 1) * 512], start=(n_ == 0), stop=(n_ == 7))
                        return ins
                    S.add("pe", fm, r=["wout"] + [("mergedT", n_, ti // 4) for n_ in range(8)], w=[PSK(bank)])
                    js = slice(jh * 512, (jh + 1) * 512)
                    S.add("dve", lambda e, sl=sl, bank=bank, js=js, c=c: e.tensor_tensor(
                        out=tmpx[sl][:, js], in0=psb[bank][:, 0:512], in1=g1bc[c][:, js], op=ALU.mult),
                        r=[PSK(bank), ("g1bc", c)], w=[("tmpx", sl)])
                    S.add("pool", lambda e, sl=sl, js=js, ti=ti: e.tensor_tensor(
                        out=x1s[:, ti, js], in0=tmpx[sl][:, js], in1=xr[sl][:, js], op=ALU.add),
                        r=[("tmpx", sl), ("xr", sl)], w=[("x1s", ti)])

            def s3b_2(ti):
                c = cond_of_tile(ti)
                sl = ti % 2
                S.add("act", lambda e, ti=ti: e.activation(out=junk3[:], in_=x1s[:, ti, :], func=AF.Square, accum_out=ssq3[:, ti:ti + 1]),
                      r=[("x1s", ti)], w=["junk3", ("ssq3", ti)])
                S.add("act", lambda e, ti=ti: e.activation(out=rs3[:, ti:ti + 1], in_=ssq3[:, ti:ti + 1], func=AF.Ln, bias=epsb[:], scale=1.0 / D),
                      r=[("ssq3", ti), "epsb"], w=[("rs3", ti)])
                S.add("act", lambda e, ti=ti: e.activation(out=rs3[:, ti:ti + 1], in_=rs3[:, ti:ti + 1], func=AF.Exp, scale=-0.5),
                      r=[("rs3", ti)], w=[("rs3", ti)])
                S.add("dve", lambda e, ti=ti, sl=sl: e.tensor_scalar(out=xn2[sl][:], in0=x1s[:, ti, :], scalar1=rs3[:, ti:ti + 1],
                                                                     scalar2=None, op0=ALU.mult),
                      r=[("x1s", ti), ("rs3", ti)], w=[("xn2", sl)])
                for half in range(2):
                    bank = 4 + half

                    def ft(e, sl=sl, half=half, bank=bank):
                        ins = None
                        for j in range(4):
                            dc = half * 4 + j
                            ins = e.transpose(psb[bank][:, j * 128:(j + 1) * 128], xn2[sl][:, dc * 128:(dc + 1) * 128], ident_f[:])
                        return ins
                    S.add("pe", ft, r=[("xn2", sl), "ident_f"], w=[PSK(bank)])
                    for j in range(4):
                        dc = half * 4 + j
                        S.add("dve" if half == 0 else "act",
                              (lambda e, sl=sl, bank=bank, j=j, dc=dc, c=c: e.tensor_scalar(
                                  out=h2f[sl][:, dc, :], in0=psb[bank][:, j * 128:(j + 1) * 128],
                                  scalar1=A2[:, dc * 2 + c:dc * 2 + c + 1], scalar2=modcol(24, dc, c), op0=ALU.mult, op1=ALU.add))
                              if half == 0 else
                              (lambda e, sl=sl, bank=bank, j=j, dc=dc, c=c: e.activation(
                                  out=h2f[sl][:, dc, :], in_=psb[bank][:, j * 128:(j + 1) * 128], func=AF.Identity,
                                  bias=modcol(24, dc, c), scale=A2[:, dc * 2 + c:dc * 2 + c + 1])),
                              r=[PSK(bank), "A2", "modT1"], w=[("h2f", sl)])
                S.add("act", lambda e, sl=sl, ti=ti: e.copy(out=h2T[:, :, ti * 128:(ti + 1) * 128], in_=h2f[sl][:]),
                      r=[("h2f", sl)], w=[("h2T", ti)])
                def frt(e, sl=sl):
                    ins = None
                    for dc in range(8):
                        ins = e.matmul(psb[6][:, 0:36], lhsT=h2f[sl][:, dc, :], rhs=wrt[:, dc, :], start=(dc == 0), stop=(dc == 7))
                    return ins
                S.add("pe", frt, r=[("h2f", sl), "wrt"], w=[PSK(6)])
                S.add("act", lambda e, ti=ti: e.copy(out=lga[:, ti, :], in_=psb[6][:, 0:36]), r=[PSK(6)], w=[("lga", ti)])

            pipeline([s3b_1, s3b_2], NT_O, [0, 1])
            lgk = [("lga", t) for t in range(NT)]
            gm, gs, ptop, m1, m2, e2, den, w1, w2 = (rs[:, i, :] for i in range(9))
            lg = lga[:, :, 0:4]
            le = lga[:, :, 4:36].rearrange("p t (g e) -> p t g e", e=8)
            b3 = lambda ap, k: ap.rearrange("p (t o) -> p t o", o=1).to_broadcast([128, NT, k])
            V = lambda fn, r, w: S.add("dve", fn, r=r, w=w)
            V(lambda e: e.tensor_reduce(out=gm, in_=lg, axis=AX.X, op=ALU.max), lgk, ["r_gm"])
            V(lambda e: e.tensor_tensor(out=rA[:], in0=lg, in1=b3(gm, 4), op=ALU.is_ge), lgk + ["r_gm"], ["rA"])
            V(lambda e: e.tensor_tensor(out=rB[:], in0=lg, in1=b3(gm, 4), op=ALU.subtract), lgk + ["r_gm"], ["rB"])
            S.add("act", lambda e: e.activation(out=rB[:], in_=rB[:], func=AF.Exp), r=["rB"], w=["rB"])
            V(lambda e: e.tensor_reduce(out=gs, in_=rB[:], axis=AX.X, op=ALU.add), ["rB"], ["r_gs"])
            V(lambda e: e.reciprocal(out=ptop, in_=gs), ["r_gs"], ["r_ptop"])
            rC4 = rC[:].rearrange("p t (g e) -> p t g e", e=8)
            V(lambda e: e.tensor_tensor(out=rC4, in0=le, in1=rA[:].rearrange("p t (g o) -> p t g o", o=1).to_broadcast([128, NT, 4, 8]),
                                        op=ALU.mult), lgk + ["rA"], ["rC"])
            V(lambda e: e.tensor_reduce(out=rsel[:], in_=rC[:].rearrange("p t (g e) -> p t e g", e=8), axis=AX.X, op=ALU.add),
              ["rC"], ["rsel"])
            V(lambda e: e.tensor_reduce(out=m1, in_=rsel[:], axis=AX.X, op=ALU.max), ["rsel"], ["r_m1"])
            V(lambda e: e.tensor_tensor(out=rmk1[:], in0=rsel[:], in1=b3(m1, 8), op=ALU.is_ge), ["rsel", "r_m1"], ["rmk1"])
            V(lambda e: e.scalar_tensor_tensor(out=rsel2[:], in0=rmk1[:], scalar=-1e30, in1=rsel[:], op0=ALU.mult, op1=ALU.add),
              ["rmk1", "rsel"], ["rsel2"])
            V(lambda e: e.tensor_reduce(out=m2, in_=rsel2[:], axis=AX.X, op=ALU.max), ["rsel2"], ["r_m2"])
            V(lambda e: e.tensor_tensor(out=rmk2[:], in0=rsel2[:], in1=b3(m2, 8), op=ALU.is_ge), ["rsel2", "r_m2"], ["rmk2"])
            V(lambda e: e.tensor_sub(out=e2, in0=m2, in1=m1), ["r_m1", "r_m2"], ["r_e2"])
            S.add("act", lambda e: e.activation(out=e2, in_=e2, func=AF.Exp), r=["r_e2"], w=["r_e2"])
            V(lambda e: e.tensor_scalar(out=den, in0=e2, scalar1=1.0, scalar2=None, op0=ALU.add), ["r_e2"], ["r_den"])
            V(lambda e: e.reciprocal(out=den, in_=den), ["r_den"], ["r_den"])
            V(lambda e: e.tensor_tensor(out=w1, in0=den, in1=ptop, op=ALU.mult), ["r_den", "r_ptop"], ["r_w1"])
            V(lambda e: e.tensor_tensor(out=w2, in0=w1, in1=e2, op=ALU.mult), ["r_w1", "r_e2"], ["r_w2"])
            V(lambda e: e.tensor_tensor(out=rcw8[:], in0=rmk1[:], in1=b3(w1, 8), op=ALU.mult), ["rmk1", "r_w1"], ["rcw8"])
            V(lambda e: e.tensor_tensor(out=rmk2[:], in0=rmk2[:], in1=b3(w2, 8), op=ALU.mult), ["rmk2", "r_w2"], ["rmk2"])
            V(lambda e: e.tensor_tensor(out=rcw8[:], in0=rcw8[:], in1=rmk2[:], op=ALU.add), ["rcw8", "rmk2"], ["rcw8"])
            cwa4 = cwa[:].rearrange("p t (g e) -> p t g e", e=8)
            for g_ in range(4):
                V(lambda e, g_=g_: e.tensor_tensor(out=cwa4[:, :, g_, :], in0=rcw8[:], in1=rA[:, :, g_:g_ + 1].to_broadcast([128, NT, 8]),
                                                   op=ALU.mult), ["rcw8", "rA"], ["cwa"])
            for t3 in range(3):
                def ftc(e, t3=t3):
                    ins = None
                    for j in range(4):
                        ti = t3 * 4 + j
                        ins = e.transpose(psb[5 + t3][0:32, j * 128:(j + 1) * 128], cwa[:, ti, :], ident_f[:])
                    return ins
                S.add("pe", ftc, r=["cwa", "ident_f"], w=[PSK(5 + t3)])
                S.add("act", lambda e, t3=t3: e.copy(out=cwT[:, t3 * 512:(t3 + 1) * 512], in_=psb[5 + t3][0:32, 0:512]),
                      r=[PSK(5 + t3)], w=[("cwT", t3 * 4 + j) for j in range(4)])
        if "x1" in dbg:
            S.add("sp", lambda e: e.dma_start(out=dbg_out["x1"], in_=x1s[:]), r=[("x1s", t) for t in range(NT_O)], dma=True)
        if "cwT" in dbg:
            S.add("sp", lambda e: e.dma_start(out=dbg_out["cwT"], in_=cwT[:]), r=[("cwT", t) for t in range(NT_O)], dma=True)
        S.barrier()

        with ExitStack() as st4:
            def sb4(name, shape, dt=F32):
                return st4.enter_context(nc.sbuf_tensor("s_" + name, list(shape), dt))
            accA = mergedT[:].bitcast(F32).rearrange("p a b -> p (a b)").rearrange("p (t d) -> p t d", d=D)
            accB = sb4("accB", [128, 6, D])
            acc = lambda ti: accA[:, ti, :] if ti < 6 else accB[:, ti - 6, :]
            spare = hflat[:, 8 * TO:8 * TA]
            wsl = lambda i: spare[:, i * 2048:(i + 1) * 2048].rearrange("p (c f) -> p c f", f=256)
            wg = [wsl(0), wsl(1)]
            wu = [wsl(2), wsl(3)]
            wd = [sb4("wd%d" % i, [128, 2, D], BF16) for i in range(2)]
            selE = [sb4("selE%d" % i, [32, 128]) for i in range(2)]
            sA = [sb4("sA%d" % i, [128, 512]) for i in range(2)]
            tA = [sb4("tA%d" % i, [128, 512]) for i in range(2)]
            cbs = [sb4("cbs%d" % i, [128, 512]) for i in range(2)]
            hid = [[sb4("hid%d_%d" % (i, j), [128, 512], BF16) for j in range(2)] for i in range(2)]
            g2bc = [sb4("g2bc%d" % c, [128, D]) for c in range(2)]
            fngbc = sb4("fngbc", [128, D])
            ones32b = sb4("ones32b", [128, 128])
            dgb = [sb4("dgb%d" % i, [128, 128]) for i in range(2)]
            junk4 = sb4("junk4", [128, D], BF16)
            ssq4 = sb4("ssq4", [128, NT_O])
            rs4 = sb4("rs4", [128, NT_O])
            S.add("pool", lambda e: e.memset(ones32b[:], 1.0), w=["ones32b"])
            S.add("sp", lambda e: e.dma_start(out=fngbc[:], in_=fng_d.to_broadcast([128, D])), w=["fngbc"], dma=True)
            for c in range(2):
                for jc in range(8):
                    d_ = dgb[jc % 2]
                    bank = jc % 2
                    S.add("dve", lambda e, d_=d_, jc=jc, c=c: e.tensor_scalar(out=d_[:], in0=ident_f[:], scalar1=modcol(40, jc, c),
                                                                              scalar2=None, op0=ALU.mult),
                          r=["ident_f", "modT1"], w=[("dgb", jc % 2)])
                    S.add("pe", lambda e, d_=d_, bank=bank: e.matmul(psb[bank][:, 0:128], lhsT=ones32b[:], rhs=d_[:], start=True, stop=True),
                          r=["ones32b", ("dgb", jc % 2)], w=[PSK(bank)])
                    S.add("act", lambda e, bank=bank, jc=jc, c=c: e.copy(out=g2bc[c][:, jc * 128:(jc + 1) * 128], in_=psb[bank][:, 0:128]),
                          r=[PSK(bank)], w=[("g2bc", c)])

            NE = dbg.get("_nexp", NEXP)
            itc = 0
            pending = [None]
            for e_ in range(NE):
                pp = e_ % 2
                S.add("pool", lambda e, pp=pp, e_=e_: e.dma_start(out=wg[pp], in_=w_eg_d[e_].rearrange("(c q) f -> q c f", q=128)),
                      w=[("wg", pp)], dma=True)
                S.add("pool", lambda e, pp=pp, e_=e_: e.dma_start(out=wu[pp], in_=w_eu_d[e_].rearrange("(c q) f -> q c f", q=128)),
                      w=[("wu", pp)], dma=True)
                S.add("pool", lambda e, pp=pp, e_=e_: e.dma_start(out=wd[pp][:], in_=w_ed_d[e_].rearrange("(c q) d -> q c d", q=128)),
                      w=[("wd", pp)], dma=True)
                S.add("pool", lambda e, pp=pp: e.memset(selE[pp][:], 0.0), w=[("selE", pp)])
                S.add("pool", lambda e, pp=pp, e_=e_: e.affine_select(out=selE[pp][:], in_=selE[pp][:], pattern=[[0, 128]],
                                                                     compare_op=ALU.not_equal, fill=1.0, base=-e_, channel_multiplier=1),
                      r=[("selE", pp)], w=[("selE", pp)])
                for tg in range(3):
                    tk = slice(tg * 512, (tg + 1) * 512)
                    hs = itc % 2
                    itc += 1
                    S.add("pe", lambda e, pp=pp, tk=tk: e.matmul(psb[4][:, 0:512], lhsT=selE[pp][:], rhs=cwT[:, tk], start=True, stop=True),
                          r=[("selE", pp)] + [("cwT", t) for t in range(tg * 4, tg * 4 + 4)], w=[PSK(4)])
                    S.add("act", lambda e, hs=hs: e.copy(out=cbs[hs][:], in_=psb[4][:, 0:512]), r=[PSK(4)], w=[("cbs", hs)])
                    for f in range(2):
                        def fg(e, wt, bank, f=f, tk=tk):
                            ins = None
                            for dc in range(8):
                                ins = e.matmul(psb[bank][:, 0:512], lhsT=wt[:, dc, f * 128:(f + 1) * 128], rhs=h2T[:, dc, tk],
                                               start=(dc == 0), stop=(dc == 7))
                            return ins
                        ba, bu = 2 * f, 2 * f + 1
                        h2k = [("h2T", t) for t in range(tg * 4, tg * 4 + 4)]
                        S.add("pe", lambda e, fg=fg, pp=pp, ba=ba: fg(e, wg[pp], ba), r=[("wg", pp)] + h2k, w=[PSK(ba)])
                        S.add("pe", lambda e, fg=fg, pp=pp, bu=bu: fg(e, wu[pp], bu), r=[("wu", pp)] + h2k, w=[PSK(bu)])
                        S.add("act", lambda e, ba=ba, f=f: e.activation(out=sA[f][:], in_=psb[ba][:, 0:512], func=AF.Silu),
                              r=[PSK(ba)], w=[("sA", f)])
                        S.add("dve", lambda e, bu=bu, f=f: e.tensor_tensor(out=tA[f][:], in0=psb[bu][:, 0:512], in1=sA[f][:], op=ALU.mult),
                              r=[PSK(bu), ("sA", f)], w=[("tA", f)])
                        S.add("pool", lambda e, f=f, hs=hs: e.tensor_tensor(out=hid[hs][f][:], in0=tA[f][:], in1=cbs[hs][:], op=ALU.mult),
                              r=[("tA", f), ("cbs", hs)], w=[("hid", hs, f)])
                    def emit_down(tg=tg, hs=hs, pp=pp, e_=e_):
                        for t4 in range(4):
                            ti = tg * 4 + t4
                            for dh in range(2):
                                bank = 5 + (t4 * 2 + dh) % 3

                                def fd(e, t4=t4, dh=dh, bank=bank):
                                    ins = None
                                    for f in range(2):
                                        ins = e.matmul(psb[bank][:, 0:512], lhsT=hid[hs][f][:, t4 * 128:(t4 + 1) * 128],
                                                       rhs=wd[pp][:, f, dh * 512:(dh + 1) * 512], start=(f == 0), stop=(f == 1))
                                    return ins
                                S.add("pe", fd, r=[("hid", hs, 0), ("hid", hs, 1), ("wd", pp)], w=[PSK(bank)])
                                ds = slice(dh * 512, (dh + 1) * 512)
                                a_ap = acc(ti)[:, ds]
                                if e_ == 0:
                                    S.add("dve", lambda e, a_ap=a_ap, bank=bank: e.tensor_copy(out=a_ap, in_=psb[bank][:, 0:512]),
                                          r=[PSK(bank)], w=[("acc", ti, dh)])
                                else:
                                    S.add("dve", lambda e, a_ap=a_ap, bank=bank: e.tensor_tensor(
                                        out=a_ap, in0=psb[bank][:, 0:512], in1=a_ap, op=ALU.add),
                                        r=[PSK(bank), ("acc", ti, dh)], w=[("acc", ti, dh)])
                    if pending[0] is not None:
                        pending[0]()
                    pending[0] = emit_down
            pending[0]()
            def fin_1(ti):
                c = cond_of_tile(ti)
                a_ap = acc(ti)
                ak = [("acc", ti, 0), ("acc", ti, 1)]
                S.add("pool", lambda e: e.tensor_tensor(out=a_ap, in0=a_ap, in1=g2bc[c][:], op=ALU.mult),
                      r=ak + [("g2bc", c)], w=ak)
                S.add("dve", lambda e: e.tensor_tensor(out=a_ap, in0=a_ap, in1=x1s[:, ti, :], op=ALU.add),
                      r=ak + [("x1s", ti)], w=ak)
                S.add("act", lambda e: e.activation(out=junk4[:], in_=a_ap, func=AF.Square, accum_out=ssq4[:, ti:ti + 1]),
                      r=ak, w=["junk4", ("ssq4", ti)])
                S.add("act", lambda e: e.activation(out=rs4[:, ti:ti + 1], in_=ssq4[:, ti:ti + 1], func=AF.Ln, bias=epsb[:], scale=1.0 / D),
                      r=[("ssq4", ti), "epsb"], w=[("rs4", ti)])
                S.add("act", lambda e: e.activation(out=rs4[:, ti:ti + 1], in_=rs4[:, ti:ti + 1], func=AF.Exp, scale=-0.5),
                      r=[("rs4", ti)], w=[("rs4", ti)])

            def fin_2(ti):
                a_ap = acc(ti)
                ak = [("acc", ti, 0), ("acc", ti, 1)]
                S.add("dve", lambda e: e.tensor_tensor(out=a_ap, in0=a_ap, in1=fngbc[:], op=ALU.mult), r=ak + ["fngbc"], w=ak)
                S.add("act", lambda e: e.activation(out=a_ap, in_=a_ap, func=AF.Copy, scale=rs4[:, ti:ti + 1]),
                      r=ak + [("rs4", ti)], w=ak)
                S.add("sp", lambda e: e.dma_start(out=y_out[ti * 128:(ti + 1) * 128, :], in_=a_ap), r=ak, dma=True)

            pipeline([fin_1, fin_2], NT_O, [0, 1])
        if "hT" in dbg:
            tmp = sb("dbg_hT", [128, 8, TA], F32)
            S.add("dve", lambda e: e.tensor_copy(out=tmp[:], in_=hT[:]), r=[("hT", i) for i in range(NT_A)], w=["dbg_hT"])
            S.add("sp", lambda e: e.dma_start(out=dbg_out["hT"], in_=tmp[:]), r=["dbg_hT"], dma=True)
        if "modT" in dbg:
            S.add("sp", lambda e: e.dma_start(out=dbg_out["modT"], in_=modT[:]), r=["modT"], dma=True)

        S.emit(st)
    return nc


def _prep_core(c, I):
    b, par = c // 2, c % 2
    f = np.ascontiguousarray
    xs = I["x_sample"][b]
    if par:
        xs = xs[::-1]
    p0 = I["x_prompt"][2 * c]
    p1 = I["x_prompt"][2 * c + 1]
    if par:
        p0, p1 = p0[::-1], p1[::-1]
    x_all = np.concatenate([xs[:1024], p0, p1, xs[1024:]], 0)
    cond = np.stack([I["c"][b], I["c_ctx"]], 0)
    condT = cond.reshape(2, 8, 128).transpose(2, 1, 0).reshape(128, 16)
    w_in = I["w_in"][0]
    o = np.cumsum([0, 1024, 1024, 1024, 1024, 1024, 512, 512, 1024, 1024, 16, 16, 1024, 1024])
    hq, hf0, hf1, hi, hgate, gq, gk, gv, gr, ga0, ga1, ma, mb = [w_in[:, o[i]:o[i + 1]] for i in range(13)]
    d0, d1 = (1, 0) if par else (0, 1)
    if par:
        hf0, hf1 = hf1, hf0
        ga0, ga1 = ga1, ga0
    hd = lambda w, h, n: w[:, h * n:(h + 1) * n]
    w_hg = np.stack([np.concatenate([hd(hq, h, 128), hd(hf0, h, 128), hd(hf1, h, 128), hd(hi, h, 128),
                                     hd(hgate, h, 128)], 1) for h in range(8)], 0)
    w_gla = np.stack([np.concatenate([hd(gq, h, 128), hd(gk, h, 128), hd(gv, h, 256), hd(gr, h, 256)], 1)
                      for h in range(4)], 0)
    w_ga = np.concatenate([ga0, ga1], 1)
    w_m = np.concatenate([ma.reshape(D, 8, 128).transpose(1, 0, 2), mb.reshape(D, 8, 128).transpose(1, 0, 2)], 0)
    lbp = np.concatenate([I["hg_lb_param"][0].reshape(8, 128).T, I["hg_lb_param"][1].reshape(8, 128).T], 1)
    wa = I["gla_wa_up"][0][[d0, d1]]
    ba = I["gla_ba"][0][[d0, d1]]
    baT = ba.reshape(2, 4, 128).transpose(2, 0, 1).reshape(128, 8)
    m = {
        "x_all": x_all, "condT": condT, "ada_w": I["ada_w"][0],
        "ada_bT": I["ada_b"][0].reshape(48, 128).T,
        "g1T": I["norm1_g"][0].reshape(8, 128).T, "g2T": I["norm2_g"][0].reshape(8, 128).T,
        "w_hg": w_hg, "w_gla": w_gla, "w_ga": w_ga, "w_m": w_m, "lbp": lbp,
        "hgn": I["hg_norm_g"][0].reshape(128, 1), "glan": I["gla_norm_g"][0].reshape(2, 128).T,
        "wa_up": wa, "baT": baT,
        "w_bra": I["w_br_a"][0].reshape(D, 8, 128).transpose(1, 0, 2),
        "w_brb": I["w_br_b"][0].reshape(D, 8, 128).transpose(1, 0, 2),
        "w_out": I["w_out"][0],
        "w_rt": np.concatenate([I["w_router_group"][0]] + [I["w_router_expert"][0][g] for g in range(4)], 1),
        "w_eg": I["w_exp_gate"][0], "w_eu": I["w_exp_up"][0], "w_ed": I["w_exp_down"][0],
        "fng": I["final_norm_g"].reshape(1, D),
        "s0_hg": I["state_hgrn"][b, 0][[d0, d1]], "s0_gla": I["state_gla"][b, 0][[d0, d1]],
    }
    return {k: f(np.asarray(v, dtype=np.float32)) for k, v in m.items()}


_NC_CACHE = {}


def kernel(**inputs):
    I = {k: np.asarray(v) for k, v in inputs.items()}
    if "nc" not in _NC_CACHE:
        _NC_CACHE["nc"] = build()
    nc = _NC_CACHE["nc"]
    in_maps = [_prep_core(c, I) for c in range(8)]
    res = run_bass_kernel_spmd(nc, in_maps, core_ids=list(range(8)))
    y_prompt = np.zeros((16, 256, D), np.float32)
    y_sample = np.zeros((4, 2048, D), np.float32)
    st_h = np.zeros((16, 1, 2, 8, 128, 128), np.float32)
    st_g = np.zeros((16, 1, 2, 4, 128, 256), np.float32)
    for c in range(8):
        r = res.results[c]
        b, par = c // 2, c % 2
        y = r["y_out"]
        ys, yp0, yp1 = y[:1024], y[1024:1280], y[1280:1536]
        if par:
            y_sample[b, 1024:] = ys[::-1]
            y_prompt[2 * c] = yp0[::-1]
            y_prompt[2 * c + 1] = yp1[::-1]
        else:
            y_sample[b, :1024] = ys
            y_prompt[2 * c] = yp0
            y_prompt[2 * c + 1] = yp1
        sh, sg = r["st_hg"], r["st_gla"]
        if par:
            sh, sg = sh[:, ::-1], sg[:, ::-1]
        st_h[2 * c:2 * c + 2, 0] = sh
        st_g[2 * c:2 * c + 2, 0] = sg
    return (y_prompt, y_sample, st_h, st_g)
```

```python
import os
from contextlib import ExitStack
import numpy as np
import concourse.bass as bass
import concourse.mybir as mybir
from concourse.bass_utils import run_bass_kernel_spmd

F32 = mybir.dt.float32
BF16 = mybir.dt.bfloat16
AF = mybir.ActivationFunctionType
ALU = mybir.AluOpType
AX = mybir.AxisListType

D = 1024
TO = 1536
TOTH = 1024
TA = TO + TOTH
NT_O = TO // 128
NT_A = TA // 128
EPS = 1e-6
NEXP = 32


class Sched:
    def __init__(self, nc):
        self.nc = nc
        self.ops = []
        self.lw = {}
        self.rd = {}
        self.bar = set()

    def barrier(self):
        last = {}
        dmas = {}
        for i, op in enumerate(self.ops):
            last[op["eng"]] = i
            if op["dma"]:
                dmas.setdefault(op["eng"], []).append(i)
        b = set(last.values())
        for e, l in dmas.items():
            b.update(l[-8:])
        self.bar = b

    def add(self, eng, fn, r=(), w=(), dma=False):
        deps = set(self.bar)
        for k in r:
            if k in self.lw:
                deps.add(self.lw[k])
        for k in w:
            if k in self.lw:
                deps.add(self.lw[k])
            deps.update(self.rd.get(k, ()))
        i = len(self.ops)
        self.ops.append(dict(eng=eng, fn=fn, deps=deps, dma=dma, need=dma, sig=None, pre=None))
        for k in r:
            self.rd.setdefault(k, []).append(i)
        for k in w:
            self.lw[k] = i
            self.rd[k] = []
        return i

    def emit(self, stack):
        nc = self.nc
        ops = self.ops
        for op in ops:
            for d in op["deps"]:
                if ops[d]["dma"] or not (ops[d]["eng"] == "pe" and op["eng"] == "pe"):
                    ops[d]["need"] = True
        engs = ("pe", "act", "dve", "pool", "sp")
        esem = {e: stack.enter_context(nc.semaphore("c_" + e)) for e in engs}
        KD = 8
        dsem = {e: [stack.enter_context(nc.semaphore("d_%s%d" % (e, i))) for i in range(KD)]
                for e in ("sp", "pool", "act")}
        ecnt = {e: 0 for e in engs}
        dcnt = {e: 0 for e in dsem}
        dfinal = {}
        for op in ops:
            e = op["eng"]
            if op["dma"]:
                j = dcnt[e]
                dcnt[e] += 1
                s = dsem[e][j % KD]
                u = j // KD
                if u > 0:
                    op["pre"] = (s, 16 * u)
                op["sig"] = (s, 16 * (u + 1), 16)
                dfinal[id(s)] = (s, 16 * (u + 1))
            elif op["need"]:
                ecnt[e] += 1
                op["sig"] = (esem[e], ecnt[e], 1)

        def run(name, e):
            waited = {}

            def wait(s, v):
                if waited.get(id(s), 0) < v:
                    e.wait_ge(s, v)
                    waited[id(s)] = v

            for op in ops:
                if op["eng"] != name:
                    continue
                if op["pre"] is not None:
                    wait(*op["pre"])
                for d in sorted(op["deps"]):
                    dop = ops[d]
                    if dop["dma"] or not (dop["eng"] == "pe" and name == "pe"):
                        wait(dop["sig"][0], dop["sig"][1])
                ins = op["fn"](e)
                if op["sig"] is not None:
                    ins.then_inc(op["sig"][0], op["sig"][2])
            if name == "sp":
                for s, v in dfinal.values():
                    wait(s, v)

        with nc.Block() as block:
            @block.sync
            def _(e):
                run("sp", e)

            @block.tensor
            def _(e):
                run("pe", e)

            @block.scalar
            def _(e):
                run("act", e)

            @block.vector
            def _(e):
                run("dve", e)

            @block.gpsimd
            def _(e):
                run("pool", e)


def build(dbg=None):
    nc = bass.Bass("TRN2", target_bir_lowering=False)
    S = Sched(nc)
    dbg = dbg or {}

    def din(name, shape):
        return nc.dram_tensor(name, list(shape), F32, kind="ExternalInput").ap()

    def dout(name, shape, dt=F32):
        return nc.dram_tensor(name, list(shape), dt, kind="ExternalOutput").ap()

    x_all = din("x_all", [TA, D])
    condT_d = din("condT", [128, 16])
    ada_w_d = din("ada_w", [D, 6 * D])
    ada_bT_d = din("ada_bT", [128, 48])
    g1T_d = din("g1T", [128, 8])
    g2T_d = din("g2T", [128, 8])
    w_hg_d = din("w_hg", [8, D, 640])
    w_gla_d = din("w_gla", [4, D, 768])
    w_ga_d = din("w_ga", [D, 32])
    w_m_d = din("w_m", [16, D, 128])
    lbp_d = din("lbp", [128, 16])
    hgn_d = din("hgn", [128, 1])
    glan_d = din("glan", [128, 2])
    wa_up_d = din("wa_up", [2, 16, 512])
    nbaT_d = din("baT", [128, 8])
    w_bra_d = din("w_bra", [8, D, 128])
    w_brb_d = din("w_brb", [8, D, 128])
    w_out_d = din("w_out", [D, D])
    w_rt_d = din("w_rt", [D, 36])
    w_eg_d = din("w_eg", [NEXP, D, 256])
    w_eu_d = din("w_eu", [NEXP, D, 256])
    w_ed_d = din("w_ed", [NEXP, 256, D])
    fng_d = din("fng", [1, D])
    s0_hg_d = din("s0_hg", [2, 8, 128, 128])
    s0_gla_d = din("s0_gla", [2, 4, 128, 256])

    y_out = dout("y_out", [TO, D])
    st_hg = dout("st_hg", [2, 2, 8, 128, 128])
    st_gla = dout("st_gla", [2, 2, 4, 128, 256])
    dbg_out = {k: dout("dbg_" + k, shp, BF16 if k in ("yT",) else F32) for k, shp in dbg.items() if not k.startswith("_")}

    with ExitStack() as st:
        def sb(name, shape, dt=F32):
            return st.enter_context(nc.sbuf_tensor("s_" + name, list(shape), dt))

        psb = [st.enter_context(nc.psum_tensor("ps%d" % i, [128, 512], F32)) for i in range(8)]

        def PSK(b):
            return ("ps", b)

        ident_f = sb("ident_f", [128, 128])
        ident_b = sb("ident_b", [128, 128], BF16)
        ones_f = sb("ones_f", [128, 512], BF16)
        one_c = sb("one_c", [128, 1])
        mask32 = sb("mask32", [128, 1024], BF16)
        trimask = sb("trimask", [128, 512], BF16)
        S.add("pool", lambda e: e.memset(ident_f[:], 0.0), w=["ident_f"])
        S.add("pool", lambda e: e.affine_select(out=ident_f[:], in_=ident_f[:], pattern=[[-1, 128]],
                                                compare_op=ALU.not_equal, fill=1.0, base=0,
                                                channel_multiplier=1), r=["ident_f"], w=["ident_f"])
        S.add("dve", lambda e: e.tensor_copy(out=ident_b[:], in_=ident_f[:]), r=["ident_f"], w=["ident_b"])
        S.add("pool", lambda e: e.memset(ones_f[:], 1.0), w=["ones_f"])
        S.add("pool", lambda e: e.memset(one_c[:], 1.0), w=["one_c"])
        S.add("pool", lambda e: e.memset(mask32[:], 1.0), w=["mask32"])
        S.add("pool", lambda e: e.memset(mask32[:].rearrange("p (c j) -> p c j", j=32)[:, :, 0:1], 0.0),
              r=["mask32"], w=["mask32"])
        tmv = trimask[:].rearrange("p (a d t) -> p a d t", a=4, d=2)
        onv = ones_f[:].rearrange("p (a d t) -> p a d t", a=4, d=2)
        for half in range(2):
            pr = slice(half * 64, half * 64 + 64)
            S.add("pool", lambda e, pr=pr: e.affine_select(
                out=tmv[pr, :, 0, :], in_=onv[pr, :, 0, :], pattern=[[0, 4], [1, 64]],
                compare_op=ALU.is_ge, fill=0.0, base=0, channel_multiplier=-1),
                r=["ones_f"], w=["trimask"])
            S.add("pool", lambda e, pr=pr: e.affine_select(
                out=tmv[pr, :, 1, :], in_=onv[pr, :, 1, :], pattern=[[0, 4], [-1, 64]],
                compare_op=ALU.is_ge, fill=0.0, base=0, channel_multiplier=1),
                r=["ones_f"], w=["trimask"])

        condT = sb("condT", [128, 16])
        scT = sb("scT", [128, 16])
        ada_bT = sb("ada_bT", [128, 48])
        g1T = sb("g1T", [128, 8])
        g2T = sb("g2T", [128, 8])
        lbp = sb("lbp", [128, 16])
        hgn = sb("hgn", [128, 1])
        glan = sb("glan", [128, 2])
        baT = sb("baT_s", [128, 8])
        nbaT = sb("nbaT", [128, 8])
        modT = sb("modT", [128, 96])
        A1 = sb("A1", [128, 16])
        A2 = sb("A2", [128, 16])
        lb = sb("lb", [128, 8])
        oml = sb("oml", [128, 8])
        noml = sb("noml", [128, 8])
        epsb = sb("epsb", [128, 1])
        for t_, d_, nm in ((condT, condT_d, "condT"), (ada_bT, ada_bT_d, "ada_bT"), (g1T, g1T_d, "g1T"),
                           (g2T, g2T_d, "g2T"), (lbp, lbp_d, "lbp"), (hgn, hgn_d, "hgn"),
                           (glan, glan_d, "glan"), (baT, nbaT_d, "baT")):
            S.add("sp", lambda e, t_=t_, d_=d_: e.dma_start(out=t_[:], in_=d_), w=[nm], dma=True)
        S.add("pool", lambda e: e.memset(epsb[:], EPS), w=["epsb"])
        S.add("act", lambda e: e.activation(out=scT[:], in_=condT[:], func=AF.Silu), r=["condT"], w=["scT"])
        S.add("dve", lambda e: e.tensor_sub(out=lb[:], in0=lbp[:, 0:8], in1=lbp[:, 8:16]), r=["lbp"], w=["lb"])
        S.add("act", lambda e: e.activation(out=lb[:], in_=lb[:], func=AF.Sigmoid), r=["lb"], w=["lb"])
        S.add("dve", lambda e: e.tensor_scalar(out=oml[:], in0=lb[:], scalar1=-1.0, scalar2=1.0,
                                               op0=ALU.mult, op1=ALU.add), r=["lb"], w=["oml"])
        S.add("dve", lambda e: e.tensor_single_scalar(out=noml[:], in_=oml[:], scalar=-1.0, op=ALU.mult),
              r=["oml"], w=["noml"])
        S.add("dve", lambda e: e.tensor_single_scalar(out=nbaT[:], in_=baT[:], scalar=-1.0, op=ALU.mult),
              r=["baT"], w=["nbaT"])

        def pipeline(stages, n, skews):
            for i in range(n + max(skews)):
                for stg, sk in zip(stages, skews):
                    if 0 <= i - sk < n:
                        stg(i - sk)

        hT = sb("hT", [128, 8, TA], BF16)
        ydram = nc.dram_tensor("yscr", [TO, 2 * D], BF16).ap()
        ssq = sb("ssq", [128, NT_A])
        rstd = sb("rstd", [128, NT_A])
        mv = modT[:].rearrange("p (j c) -> p j c", c=2)

        def modkey(j):
            return "modT0" if j < 16 else "modT1"

        def modcol(j0, dc, c):
            return modT[:, (j0 + dc) * 2 + c:(j0 + dc) * 2 + c + 1]

        def cond_of_tile(ti):
            return 1 if 8 <= ti < 12 else 0

        with ExitStack() as st01:
            adaw = [st01.enter_context(nc.sbuf_tensor("adaw%d" % i, [128, 8, 512], F32)) for i in range(3)]
            scTb = st01.enter_context(nc.sbuf_tensor("scTb", [128, 16], BF16))
            modrow = st01.enter_context(nc.sbuf_tensor("modrow", [2, 6 * D], F32))
            xts = [st01.enter_context(nc.sbuf_tensor("xt%d" % i, [128, D], F32)) for i in range(3)]
            xns = [st01.enter_context(nc.sbuf_tensor("xn%d" % i, [128, D], BF16)) for i in range(3)]
            adv = ada_w_d.rearrange("(dc p) n -> p dc n", p=128)
            S.add("dve", lambda e: e.tensor_copy(out=scTb[:], in_=scT[:]), r=["scT"], w=["scTb"])

            def mod_block(blk):
                a = adaw[blk % 3]
                S.add("sp", lambda e: e.dma_start(out=a[:], in_=adv[:, :, blk * 512:(blk + 1) * 512]),
                      w=[("adaw", blk % 3)], dma=True)

                def f(e):
                    ins = None
                    for dc in range(8):
                        ins = e.matmul(psb[0][0:2, 0:512], lhsT=scT[:, dc * 2:dc * 2 + 2], rhs=a[:, dc, :],
                                       start=(dc == 0), stop=(dc == 7))
                    return ins
                S.add("pe", f, r=[("adaw", blk % 3), "scT"], w=[PSK(0)])
                S.add("dve", lambda e: e.tensor_copy(out=modrow[0:2, blk * 512:(blk + 1) * 512], in_=psb[0][0:2, 0:512]),
                      r=[PSK(0)], w=[("modrow", blk)])

                def ft(e):
                    ins = None
                    for j4 in range(4):
                        jc = blk * 4 + j4
                        ins = e.transpose(psb[7][:, jc * 2:jc * 2 + 2], modrow[0:2, jc * 128:(jc + 1) * 128], ident_f[0:2, 0:2])
                    return ins
                S.add("pe", ft, r=[("modrow", blk), "ident_f"], w=[PSK(7)])

            def mod_evac(j_lo, j_hi, key):
                for c in range(2):
                    S.add("dve", lambda e, c=c: e.tensor_tensor(
                        out=mv[:, j_lo:j_hi, c], in0=psb[7][:, 0:96].rearrange("p (j c) -> p j c", c=2)[:, j_lo:j_hi, c],
                        in1=ada_bT[:, j_lo:j_hi], op=ALU.add), r=[PSK(7), "ada_bT"], w=[key])

            for blk in range(4):
                mod_block(blk)
            mod_evac(0, 16, "modT0")
            Av = A1[:].rearrange("p (j c) -> p j c", c=2)
            for c in range(2):
                S.add("dve", lambda e, c=c: e.scalar_tensor_tensor(
                    out=Av[:, :, c], in0=mv[:, 8:16, c], scalar=1.0, in1=g1T[:],
                    op0=ALU.add, op1=ALU.mult), r=["modT0", "g1T"], w=["A1"])

            def p1_stage1(ti):
                xt = xts[ti % 3]
                xn = xns[ti % 3]
                S.add("sp", lambda e: e.dma_start(out=xt[:], in_=x_all[ti * 128:(ti + 1) * 128, :]),
                      w=[("xt", ti % 3)], dma=True)
                S.add("act", lambda e: e.activation(out=xn[:], in_=xt[:], func=AF.Square, accum_out=ssq[:, ti:ti + 1]),
                      r=[("xt", ti % 3)], w=[("xn", ti % 3), ("ssq", ti)])
                S.add("act", lambda e: e.activation(out=rstd[:, ti:ti + 1], in_=ssq[:, ti:ti + 1], func=AF.Sqrt,
                                                    bias=epsb[:], scale=1.0 / D),
                      r=[("ssq", ti), "epsb"], w=[("rstd", ti)])
                S.add("dve", lambda e: e.reciprocal(out=rstd[:, ti:ti + 1], in_=rstd[:, ti:ti + 1]),
                      r=[("rstd", ti)], w=[("rstd", ti)])
                S.add("dve", lambda e: e.tensor_scalar(out=xn[:], in0=xt[:], scalar1=rstd[:, ti:ti + 1], scalar2=None, op0=ALU.mult),
                      r=[("xt", ti % 3), ("rstd", ti)], w=[("xn", ti % 3)])

            def p1_stage2(ti):
                xn = xns[ti % 3]
                bank = 1 + (ti % 2)
                c = cond_of_tile(ti)
                pbv = psb[bank][:].bitcast(BF16)

                def ftr(e):
                    ins = None
                    for dc in range(8):
                        ins = e.transpose(pbv[:, dc * 128:(dc + 1) * 128], xn[:, dc * 128:(dc + 1) * 128], ident_b[:])
                    return ins
                S.add("pe", ftr, r=[("xn", ti % 3), "ident_b"], w=[PSK(bank)])
                for dc in range(8):
                    a_ap = A1[:, dc * 2 + c:dc * 2 + c + 1]
                    s_ap = modcol(0, dc, c)
                    o_ap = hT[:, dc, ti * 128:(ti + 1) * 128]
                    i_ap = pbv[:, dc * 128:(dc + 1) * 128]
                    if ti % 2 == 0:
                        S.add("dve", lambda e, o_ap=o_ap, i_ap=i_ap, a_ap=a_ap, s_ap=s_ap: e.tensor_scalar(
                            out=o_ap, in0=i_ap, scalar1=a_ap, scalar2=s_ap, op0=ALU.mult, op1=ALU.add),
                            r=[PSK(bank), "A1", "modT0"], w=[("hT", ti)])
                    else:
                        S.add("act", lambda e, o_ap=o_ap, i_ap=i_ap, a_ap=a_ap, s_ap=s_ap: e.activation(
                            out=o_ap, in_=i_ap, func=AF.Identity, bias=s_ap, scale=a_ap),
                            r=[PSK(bank), "A1", "modT0"], w=[("hT", ti)])

            def p1_mod(ti):
                if ti < 8:
                    mod_block(4 + ti)
                if ti == 8:
                    mod_evac(16, 48, "modT1")
                    Av2 = A2[:].rearrange("p (j c) -> p j c", c=2)
                    for c in range(2):
                        S.add("dve", lambda e, c=c: e.scalar_tensor_tensor(
                            out=Av2[:, :, c], in0=mv[:, 32:40, c], scalar=1.0, in1=g2T[:],
                            op0=ALU.add, op1=ALU.mult), r=["modT1", "g2T"], w=["A2"])
            pipeline([p1_stage1, p1_mod, p1_stage2], NT_A, [0, 0, 2])

        st2 = st.enter_context(ExitStack())
        S.barrier()

        def sb2(name, shape, dt=F32):
            return st2.enter_context(nc.sbuf_tensor("s_" + name, list(shape), dt))

        W = sb2("W", [128, 8, 768], BF16)
        NSET = 4
        B1 = sb2("B1", [128, NSET, 512])
        B2 = sb2("B2", [128, NSET, 512])
        B3 = sb2("B3", [128, NSET, 512])
        q_bf = sb2("q_bf", [128, TO], BF16)
        k_bf = sb2("k_bf", [128, 2048], BF16)
        k_o = sb2("k_o", [128, TOTH], BF16)
        qI = [sb2("qI%d" % d, [128, TO], BF16) for d in range(2)]
        qX2 = [sb2("qX2%d" % d, [128, TO], BF16) for d in range(2)]
        kX1 = [sb2("kX1%d" % d, [128, TO], BF16) for d in range(2)]
        kX2 = [sb2("kX2%d" % d, [128, TO], BF16) for d in range(2)]
        kSb = sb2("kSb", [128, NSET, 512], BF16)
        kS_tm = [sb2("kStm0", [128, NT_O, 128], BF16), sb2("kStm1", [128, NT_A, 128], BF16)]
        Dd = [sb2("Dd%d" % d, [128, 40]) for d in range(2)]
        Eo = sb2("Eo", [128, 8 * NSET, 2])
        v_tm = sb2("v_tm", [128, NT_A, 256], BF16)
        G_tm = sb2("G_tm", [128, NT_O, 256], BF16)
        S32 = sb2("S32", [128, 6, 256])
        S_bf = [sb2("S_bf0", [128, 4, 256], BF16), sb2("S_bf1", [128, 24, 256], BF16)]
        scS = sb2("scS", [128, 512], BF16)
        y_tm = [sb2("y_tm%d" % i, [128, 256], BF16) for i in range(4)]
        ssq2 = sb2("ssq2", [128, 160])
        rs2 = sb2("rs2", [128, 160])
        gaT = sb2("gaT", [64, TA], BF16)
        wa_bf = sb2("wa_bf", [64, 512], BF16)
        junk2 = sb2("junk2", [128, 256], BF16)

        for d in range(2):
            for i_, (arr, nm) in enumerate(((qX2[d], "qX2"), (kX1[d], "kX1"), (kX2[d], "kX2"))):
                S.add("dve" if (i_ + d) % 2 == 0 else "pool", lambda e, arr=arr: e.memset(arr[:], 0.0), w=[(nm, d)])
        S.add("pool", lambda e: e.memset(ssq2[:], 0.0), w=["ssq2"])
        for d in range(2):
            S.add("pool", lambda e, d=d: e.dma_start(out=wa_bf[d * 32:d * 32 + 16, :], in_=wa_up_d[d]),
                  w=[("wa_bf", d)], dma=True)

        GEO = [dict(fh=slice(0, 32), sh=slice(32, 64), pf=31, pse=63),
               dict(fh=slice(32, 64), sh=slice(0, 32), pf=32, pse=0)]
        cm_ctr = [0]
        tm_ctr = [0]
        st_ctr = [0]

        def c64(ap):
            return ap.rearrange("p (c j) -> p c j", j=64)

        def cmaj(col0, tok0, ntok, consume, M=128, wkey="Wc", wsrc=None, pbase=0):
            bank = cm_ctr[0] % 2
            cm_ctr[0] += 1
            src = W if wsrc is None else wsrc

            def f(e):
                ins = None
                for dc in range(8):
                    ins = e.matmul(psb[bank][pbase:pbase + M, 0:ntok], lhsT=src[:, dc, col0:col0 + M],
                                   rhs=hT[:, dc, tok0:tok0 + ntok], start=(dc == 0), stop=(dc == 7))
                return ins
            tiles = range(tok0 // 128, (tok0 + ntok) // 128)
            S.add("pe", f, r=[wkey] + [("hT", t) for t in tiles], w=[PSK(bank)])
            consume(psb[bank][pbase:pbase + M, 0:ntok], PSK(bank))

        def gating(d, n, tokbase, gbase, k_ap, kkey, se, own, par):
            g = GEO[d]
            fh, sh, pf, pse = g["fh"], g["sh"], g["pf"], g["pse"]
            ncnk = n // 64
            b1, b2, b3 = B1[:, par, 0:n], B2[:, par, 0:n], B3[:, par, 0:n]
            k1, k2, k3 = ("B1", par), ("B2", par), ("B3", par)
            c32v = c64(b3)
            e32v = c64(b1)
            iev = c64(b2)
            kv = c64(k_ap)
            Dv = Dd[d][:, gbase:gbase + ncnk]
            Dv3 = Dv.rearrange("p (c o) -> p c o", o=1)
            bc = lambda ap: ap.to_broadcast([128, ncnk, 32])
            S.add("act", lambda e: e.activation(out=b2, in_=b3, func=AF.Exp, scale=-se), r=[k3], w=[k2])
            if own:
                S.add("act", lambda e: e.activation(out=b1, in_=b3, func=AF.Exp, scale=se), r=[k3], w=[k1])
                EF = e32v[:, :, pf:pf + 1]
                ES = e32v[:, :, pse:pse + 1]
                ekey = k1
            else:
                eo = Eo[:, par * 8:par * 8 + ncnk, :]
                S.add("act", lambda e: e.activation(out=eo[:, :, 0:1], in_=c32v[:, :, pf:pf + 1], func=AF.Exp, scale=se),
                      r=[k3], w=[("Eo", par)])
                S.add("act", lambda e: e.activation(out=eo[:, :, 1:2], in_=c32v[:, :, pse:pse + 1], func=AF.Exp, scale=se),
                      r=[k3], w=[("Eo", par)])
                EF = eo[:, :, 0:1]
                ES = eo[:, :, 1:2]
                ekey = ("Eo", par)
            S.add("dve", lambda e: e.tensor_tensor(out=Dv3, in0=EF, in1=ES, op=ALU.mult), r=[ekey], w=[("Dd", d)])
            if own:
                qv = c64(q_bf[:, tokbase:tokbase + n])
                for (eng, dst, nm, a_, b_, hs) in (
                        ("pool", kX1[d], "kX1", kv, iev, fh), ("pool", kX2[d], "kX2", kv, iev, sh),
                        ("dve", qX2[d], "qX2", qv, e32v, sh), ("dve", qI[d], "qI", qv, e32v, fh)):
                    dv_ = c64(dst[:, tokbase:tokbase + n])
                    S.add(eng, lambda e, dv_=dv_, a_=a_, b_=b_, hs=hs: e.tensor_tensor(
                        out=dv_[:, :, hs], in0=a_[:, :, hs], in1=b_[:, :, hs], op=ALU.mult),
                        r=[kkey, "q_bf", k1, k2], w=[(nm, d)])
            S.add("pool", lambda e: e.tensor_tensor(out=iev[:, :, fh], in0=iev[:, :, fh], in1=bc(Dv3), op=ALU.mult),
                  r=[k2, ("Dd", d)], w=[k2])
            S.add("pool", lambda e: e.tensor_tensor(out=iev[:, :, sh], in0=iev[:, :, sh], in1=bc(ES), op=ALU.mult),
                  r=[k2, ekey], w=[k2])
            if own:
                S.add("dve", lambda e: e.tensor_tensor(out=e32v[:, :, sh], in0=e32v[:, :, sh], in1=bc(EF), op=ALU.mult),
                      r=[k1], w=[k1])
                dv_ = c64(qI[d][:, tokbase:tokbase + n])
                S.add("dve", lambda e: e.tensor_tensor(out=dv_[:, :, sh], in0=qv[:, :, sh], in1=e32v[:, :, sh], op=ALU.mult),
                      r=["q_bf", k1], w=[("qI", d)])
            ksb = kSb[:, par, 0:n]
            S.add("dve", lambda e: e.tensor_tensor(out=ksb, in0=k_ap, in1=b2, op=ALU.mult), r=[kkey, k2], w=[("kSb", par)])

        def ks_transpose(d, n, tokbase, par):
            ksb = kSb[:, par, 0:n]
            nt = n // 128
            tile0 = tokbase // 128
            pbv = psb[4][:].bitcast(BF16)

            def ftr(e):
                ins = None
                for j in range(nt):
                    ins = e.transpose(pbv[:, j * 128:(j + 1) * 128], ksb[:, j * 128:(j + 1) * 128], ident_b[:])
                return ins
            S.add("pe", ftr, r=[("kSb", par), "ident_b"], w=[PSK(4)])
            S.add("act", lambda e: e.copy(out=kS_tm[d][:, tile0:tile0 + nt, :].rearrange("p a b -> p (a b)"),
                                          in_=pbv[:, 0:nt * 128]), r=[PSK(4)], w=[("kStm", d)])

        def seg_scan(d, n, par):
            b2, b3 = B2[:, par, 0:n], B3[:, par, 0:n]
            if d == 0:
                S.add("dve", lambda e: e.tensor_tensor_scan(out=b3, data0=mask32[:, 0:n], data1=b2,
                                                            initial=0.0, op0=ALU.mult, op1=ALU.add),
                      r=[("B2", par), "mask32"], w=[("B3", par)])
            else:
                S.add("dve", lambda e: e.tensor_tensor_scan(out=b3[:, ::-1], data0=mask32[:, 0:n], data1=b2[:, ::-1],
                                                            initial=0.0, op0=ALU.mult, op1=ALU.add),
                      r=[("B2", par), "mask32"], w=[("B3", par)])

        def tok_of_chunk(g):
            return g * 64

        zero_state = set()

        def state_step(dv, ci, d, g, first_zero, copy_to=None):
            ti, hf = g // 2, g % 2
            pr = slice(hf * 64, hf * 64 + 64)
            bank = 5 + st_ctr[0] % 2
            st_ctr[0] += 1
            pS = psb[bank][:, 0:dv]
            pkey = PSK(bank)
            S.add("pe", lambda e: e.matmul(pS, lhsT=kS_tm[d][pr, ti, :], rhs=v_tm[pr, ti, 0:dv], start=True, stop=True),
                  r=[("kStm", d), ("v_tm", ti)], w=[pkey])
            if copy_to is not None:
                if first_zero:
                    zero_state.add((d, g))
                else:
                    S.add("act", lambda e: e.copy(out=copy_to[0], in_=S32[:, ci, 0:dv]), r=[("S32", ci)], w=[copy_to[1]])
            if first_zero:
                S.add("dve", lambda e: e.tensor_copy(out=S32[:, ci, 0:dv], in_=pS), r=[pkey], w=[("S32", ci)])
            else:
                S.add("dve", lambda e: e.scalar_tensor_tensor(
                    out=S32[:, ci, 0:dv], in0=S32[:, ci, 0:dv], scalar=Dd[d][:, g:g + 1], in1=pS,
                    op0=ALU.mult, op1=ALU.add), r=[pkey, ("S32", ci), ("Dd", d)], w=[("S32", ci)])

        def sweep_dir1(kind, h, dv):
            s0d = s0_hg_d if kind == "hg" else s0_gla_d
            std = st_hg if kind == "hg" else st_gla
            zero_state.clear()
            ch1 = [dict(ci=3, seq=list(range(39, 23, -1)) + list(range(15, -1, -1)), init=1, prompt=None),
                   dict(ci=4, seq=list(range(19, 15, -1)), init=None, prompt=0),
                   dict(ci=5, seq=list(range(23, 19, -1)), init=None, prompt=1)]
            S.add("sp", lambda e: e.dma_start(out=S32[:, 3, 0:dv], in_=s0d[1, h]), w=[("S32", 3)], dma=True)
            S.add("sp", lambda e: e.dma_start(out=S32[:, 0, 0:dv], in_=s0d[0, h]), w=[("S32", 0)], dma=True)
            for step in range(32):
                for ch in ch1:
                    if step >= len(ch["seq"]):
                        continue
                    g = ch["seq"][step]
                    ci = ch["ci"]
                    fz = ch["init"] is None and step == 0
                    cp = (S_bf[1][:, g, 0:dv], ("S_bf", 1, g)) if g < 24 else None
                    state_step(dv, ci, 1, g, fz, cp)
                    if ch["prompt"] is not None and step == len(ch["seq"]) - 1:
                        S.add("sp", lambda e, ci=ci, ch=ch: e.dma_start(out=std[ch["prompt"], 1, h], in_=S32[:, ci, 0:dv]),
                              r=[("S32", ci)], dma=True)
                if step % 4 == 3:
                    yield

        def dir0_and_output(kind, h, dv, ychunk0):
            std = st_hg if kind == "hg" else st_gla
            pscb = psb[7]
            pbv = psb[4][:].bitcast(BF16)
            nvc = dv // 128

            def stageA(ti):
                ci = 0 if ti < 8 else (1 if ti < 10 else 2)
                for hf in range(2):
                    g = 2 * ti + hf
                    fz = ci > 0 and g in (16, 20)
                    rs = g % 4
                    state_step(dv, ci, 0, g, fz, (S_bf[0][:, rs, 0:dv], ("S_bf", 0, rs)))
                if ti in (9, 11):
                    S.add("sp", lambda e, ci=ci: e.dma_start(out=std[ci - 1, 0, h], in_=S32[:, ci, 0:dv]),
                          r=[("S32", ci)], dma=True)
                slot = ti % 4

                def fsc(e):
                    ins = None
                    for hf in range(2):
                        g = 2 * ti + hf
                        tk = slice(g * 64, g * 64 + 64)
                        pr = slice(hf * 64, hf * 64 + 64)
                        for d in range(2):
                            o_ap = pscb[pr, slot * 128 + d * 64:slot * 128 + d * 64 + 64]
                            e.matmul(o_ap, lhsT=kX1[d][:, tk], rhs=qI[d][:, tk], start=True, stop=False)
                            ins = e.matmul(o_ap, lhsT=kX2[d][:, tk], rhs=qX2[d][:, tk], start=False, stop=True)
                    return ins
                S.add("pe", fsc, r=[("kX1", 0), ("kX1", 1), ("kX2", 0), ("kX2", 1), ("qI", 0), ("qI", 1),
                                    ("qX2", 0), ("qX2", 1)], w=[PSK(7)])
                S.add("dve", lambda e: e.tensor_tensor(
                    out=scS[:, slot * 128:(slot + 1) * 128], in0=pscb[:, slot * 128:(slot + 1) * 128],
                    in1=trimask[:, slot * 128:(slot + 1) * 128], op=ALU.mult),
                    r=[PSK(7), "trimask"], w=[("scS", slot)])

            def stageB(ti):
                slot = ti % 4
                ob = 2 + (ti % 2)
                po = psb[ob][:, 0:dv]

                def fo(e):
                    ins = None
                    for hf in range(2):
                        g = 2 * ti + hf
                        tk = slice(g * 64, g * 64 + 64)
                        pr = slice(hf * 64, hf * 64 + 64)
                        mms = []
                        if (0, g) not in zero_state:
                            mms.append((qI[0][:, tk], S_bf[0][:, g % 4, 0:dv]))
                        if (1, g) not in zero_state:
                            mms.append((qI[1][:, tk], S_bf[1][:, g, 0:dv]))
                        for d in range(2):
                            mms.append((scS[pr, slot * 128 + d * 64:slot * 128 + d * 64 + 64], v_tm[pr, ti, 0:dv]))
                        for i, (l, r_) in enumerate(mms):
                            ins = e.matmul(psb[ob][pr, 0:dv], lhsT=l, rhs=r_, start=(i == 0), stop=(i == len(mms) - 1))
                    return ins
                S.add("pe", fo, r=[("qI", 0), ("qI", 1), ("scS", slot), ("v_tm", ti)] +
                      [("S_bf", 0, (2 * ti + hf) % 4) for hf in range(2)] +
                      [("S_bf", 1, 2 * ti + hf) for hf in range(2)], w=[PSK(ob)])
                si = (ychunk0 if kind == "hg" else 8 + h) * 12 + ti
                sc_ = ssq2[:, si:si + 1]
                rc_ = rs2[:, si:si + 1]
                S.add("act", lambda e: e.activation(out=junk2[:, 0:dv], in_=po, func=AF.Square, accum_out=sc_),
                      r=[PSK(ob)], w=["junk2", ("ssq2", ti % 2)])
                S.add("act", lambda e: e.activation(out=rc_, in_=sc_, func=AF.Sqrt, bias=epsb[:], scale=1.0 / dv),
                      r=[("ssq2", ti % 2), "epsb"], w=[("rs2", ti % 2)])
                S.add("dve", lambda e: e.reciprocal(out=rc_, in_=rc_), r=[("rs2", ti % 2)], w=[("rs2", ti % 2)])
                yt = y_tm[ti % 4]
                S.add("dve", lambda e: e.scalar_tensor_tensor(
                    out=yt[:, 0:dv], in0=po, scalar=rc_, in1=G_tm[:, ti, 0:dv], op0=ALU.mult, op1=ALU.mult),
                    r=[PSK(ob), ("rs2", ti % 2), ("G_tm", ti)], w=[("y_tm", ti % 4)])

            def stageC(ti):
                yt = y_tm[ti % 4]
                S.add("sp", lambda e: e.dma_start(out=ydram[ti * 128:(ti + 1) * 128, ychunk0 * 128:ychunk0 * 128 + dv], in_=yt[:, 0:dv]),
                      r=[("y_tm", ti % 4)], dma=True)

            for i in range(NT_O + 2):
                if i < NT_O:
                    stageA(i)
                if 0 <= i - 1 < NT_O:
                    stageB(i - 1)
                if 0 <= i - 2 < NT_O:
                    stageC(i - 2)

        def tmaj_pass(col_v, ncol_v, col_g, ncol_g):
            for ti in range(NT_A):
                own = ti < NT_O
                bank = 2 + tm_ctr[0] % 2
                tm_ctr[0] += 1
                ncol = ncol_v + (ncol_g if own else 0)

                def f(e, ti=ti, bank=bank, ncol=ncol):
                    ins = None
                    for dc in range(8):
                        ins = e.matmul(psb[bank][:, 0:ncol], lhsT=hT[:, dc, ti * 128:(ti + 1) * 128],
                                       rhs=W[:, dc, col_v:col_v + ncol], start=(dc == 0), stop=(dc == 7))
                    return ins
                S.add("pe", f, r=["Wt", ("hT", ti)], w=[PSK(bank)])
                S.add("act", lambda e, ti=ti, bank=bank: e.copy(out=v_tm[:, ti, 0:ncol_v], in_=psb[bank][:, 0:ncol_v]),
                      r=[PSK(bank)], w=[("v_tm", ti)])
                if own:
                    S.add("act", lambda e, ti=ti, bank=bank: e.activation(
                        out=G_tm[:, ti, 0:ncol_g], in_=psb[bank][:, ncol_v:ncol_v + ncol_g], func=AF.Silu),
                        r=[PSK(bank)], w=[("G_tm", ti)])

        PASSES = ([(1, TO + i * 512, 512, 24 + i * 8, False) for i in range(2)] +
                  [(1, i * 512, 512, i * 8, True) for i in range(3)] +
                  [(0, i * 512, 512, i * 8, True) for i in range(3)])
        N_D1 = 5

        def run_passes(p0, p1, p2_, sweep):
            n = len(PASSES)
            p0(0)
            sw = None
            for i in range(n + 2):
                if i + 1 < n:
                    p0(i + 1)
                if i < n:
                    p1(i)
                if 0 <= i - 1 < n:
                    p2_(i - 1)
                if 0 <= i - 2 < n:
                    d, tok0, nn, gb, own = PASSES[i - 2]
                    ks_transpose(d, nn, tok0, (i - 2) % NSET)
                    if i - 2 == N_D1 - 1:
                        sw = sweep()
                if sw is not None:
                    for _ in range(2):
                        next(sw, None)
            if sw is not None:
                for _ in sw:
                    pass

        def hg_head(h):
            wv = w_hg_d[h].rearrange("(dc p) n -> p dc n", p=128)
            S.add("pool", lambda e: e.dma_start(out=W[:, :, 0:384], in_=wv[:, :, 0:384]), w=["Wc"], dma=True)
            S.add("pool", lambda e: e.dma_start(out=W[:, :, 384:640], in_=wv[:, :, 384:640]), w=["Wt"], dma=True)
            for gi in range(3):
                cmaj(0, gi * 512, 512, lambda ps, pk, gi=gi: S.add(
                    "act", lambda e: e.activation(out=q_bf[:, gi * 512:(gi + 1) * 512], in_=ps, func=AF.Copy, scale=128 ** -0.5),
                    r=[pk], w=["q_bf"]))
            tmaj_pass(384, 128, 512, 128)

            pbank = {}

            def p0(pi):
                d, tok0, n, gb, own = PASSES[pi]
                col = 128 if d == 0 else 256
                cmaj(col, tok0, 512, lambda ps, pk: pbank.__setitem__(pi, (ps, pk)))

            def p1(pi):
                d, tok0, n, gb, own = PASSES[pi]
                par = pi % NSET
                ps, pk = pbank[pi]
                b1, b2, b3 = B1[:, par, :], B2[:, par, :], B3[:, par, :]
                k1, k2, k3 = ("B1", par), ("B2", par), ("B3", par)
                S.add("act", lambda e: e.activation(out=b1, in_=ps, func=AF.Exp, scale=-1.0), r=[pk], w=[k1])
                S.add("act", lambda e: e.activation(out=b3, in_=b1, func=AF.Ln, bias=one_c[:], scale=1.0), r=[k1, "one_c"], w=[k3])
                S.add("act", lambda e: e.activation(out=b2, in_=b1, func=AF.Ln, bias=one_c[:], scale=lb[:, h:h + 1]),
                      r=[k1, "one_c", "lb"], w=[k2])
                S.add("dve", lambda e: e.tensor_sub(out=b2, in0=b2, in1=b3), r=[k2, k3], w=[k2])
                S.add("act", lambda e: e.activation(out=b1, in_=b3, func=AF.Exp, scale=-1.0), r=[k3], w=[k1])
                k_ap = k_bf[:, par * 512:(par + 1) * 512]
                S.add("pool", lambda e: e.tensor_scalar(out=k_ap, in0=b1, scalar1=-1.0,
                                                        scalar2=noml[:, h:h + 1], op0=ALU.add, op1=ALU.mult),
                      r=[k1, "noml"], w=[("k_src", par)])
                seg_scan(d, n, par)

            def p2_(pi):
                d, tok0, n, gb, own = PASSES[pi]
                par = pi % NSET
                gating(d, n, tok0, gb, k_bf[:, par * 512:(par + 1) * 512], ("k_src", par), 1.0, own, par)
            run_passes(p0, p1, p2_, lambda: sweep_dir1("hg", h, 128))
            dir0_and_output("hg", h, 128, h)

        def gla_prep():
            wga = sb2("wga", [128, 8, 32], BF16)
            S.add("pool", lambda e: e.dma_start(out=wga[:], in_=w_ga_d.rearrange("(dc p) n -> p dc n", p=128)),
                  w=["wga"], dma=True)
            for d in range(2):
                ntok = TO if d == 0 else TA
                for gi in range(ntok // 512):
                    cmaj(d * 16, gi * 512, 512, lambda ps, pk, gi=gi, d=d: S.add(
                        "act", lambda e: e.copy(out=gaT[d * 32:d * 32 + 16, gi * 512:(gi + 1) * 512], in_=ps),
                        r=[pk], w=[("gaT", d)]), M=16, wkey="wga", wsrc=wga, pbase=d * 32)

        def gla_head(h):
            wv = w_gla_d[h].rearrange("(dc p) n -> p dc n", p=128)
            S.add("pool", lambda e: e.dma_start(out=W[:, :, 0:256], in_=wv[:, :, 0:256]), w=["Wc"], dma=True)
            S.add("pool", lambda e: e.dma_start(out=W[:, :, 256:768], in_=wv[:, :, 256:768]), w=["Wt"], dma=True)
            for gi in range(3):
                cmaj(0, gi * 512, 512, lambda ps, pk, gi=gi: S.add(
                    "act", lambda e: e.activation(out=q_bf[:, gi * 512:(gi + 1) * 512], in_=ps, func=AF.Copy, scale=128 ** -0.5),
                    r=[pk], w=["q_bf"]))
            for gi in range(5):
                dst = k_bf[:, gi * 512:(gi + 1) * 512] if gi < 3 else k_o[:, (gi - 3) * 512:(gi - 2) * 512]
                cmaj(128, gi * 512, 512, lambda ps, pk, dst=dst: S.add(
                    "act", lambda e: e.copy(out=dst, in_=ps), r=[pk], w=["k_gla"]))
            tmaj_pass(256, 256, 512, 256)

            pbank = {}

            def p0(pi):
                d, tok0, n, gb, own = PASSES[pi]
                bank = cm_ctr[0] % 2
                cm_ctr[0] += 1
                S.add("pe", lambda e: e.matmul(
                    psb[bank][:, 0:512], lhsT=wa_bf[d * 32:d * 32 + 16, h * 128:(h + 1) * 128],
                    rhs=gaT[d * 32:d * 32 + 16, tok0:tok0 + 512], start=True, stop=True),
                    r=[("wa_bf", d), ("gaT", d)], w=[PSK(bank)])
                pbank[pi] = bank

            def p1(pi):
                d, tok0, n, gb, own = PASSES[pi]
                par = pi % NSET
                bank = pbank[pi]
                S.add("act", lambda e: e.activation(
                    out=B1[:, par, :], in_=psb[bank][:, 0:512], func=AF.Exp,
                    bias=nbaT[:, d * 4 + h:d * 4 + h + 1], scale=-1.0),
                    r=[PSK(bank), "nbaT"], w=[("B1", par)])
                S.add("act", lambda e: e.activation(out=B2[:, par, :], in_=B1[:, par, :], func=AF.Ln, bias=one_c[:], scale=1.0),
                      r=[("B1", par), "one_c"], w=[("B2", par)])
                seg_scan(d, n, par)

            def p2_(pi):
                d, tok0, n, gb, own = PASSES[pi]
                par = pi % NSET
                k_ap = k_bf[:, tok0:tok0 + n] if own else k_o[:, tok0 - TO:tok0 - TO + n]
                gating(d, n, tok0, gb, k_ap, "k_gla", -1.0 / 16.0, own, par)
            run_passes(p0, p1, p2_, lambda: sweep_dir1("gla", h, 256))
            dir0_and_output("gla", h, 256, 8 + 2 * h)

        NH_HG = dbg.get("_nh_hg", 8)
        NH_GLA = dbg.get("_nh_gla", 4)
        for h in range(NH_HG):
            hg_head(h)
        if NH_GLA:
            gla_prep()
        for h in range(NH_GLA):
            gla_head(h)

        if dbg.get("_p2only"):
            if "modT" in dbg:
                S.add("sp", lambda e: e.dma_start(out=dbg_out["modT"], in_=modT[:]), r=["modT0", "modT1"], dma=True)
            S.emit(st)
            return nc
        st2.close()
        S.barrier()
        st3 = st.enter_context(ExitStack())

        def sb3(name, shape, dt=F32):
            return st3.enter_context(nc.sbuf_tensor("s_" + name, list(shape), dt))

        mergedT = sb3("mergedT", [128, 8, TO], BF16)
        yT = sb3("yT", [128, 16, TO], BF16)
        with ExitStack() as st3a:
            def sb3a(name, shape, dt=F32):
                return st3a.enter_context(nc.sbuf_tensor("s_" + name, list(shape), dt))
            stgA = [sb3a("stgA%d" % i, [128, 8, 128]) for i in range(2)]
            stgB = [sb3a("stgB%d" % i, [128, 8, 128]) for i in range(2)]
            wbA = [sb3a("wbA%d" % i, [128, 8, 128], BF16) for i in range(2)]
            wbB = [sb3a("wbB%d" % i, [128, 8, 128], BF16) for i in range(2)]
            wmA = [sb3a("wmA%d" % i, [128, 8, 128], BF16) for i in range(2)]
            wmB = [sb3a("wmB%d" % i, [128, 8, 128], BF16) for i in range(2)]
            sgA = [sb3a("sgA%d" % i, [128, 512]) for i in range(2)]
            sgB = [sb3a("sgB%d" % i, [128, 512]) for i in range(2)]
            tpA = [sb3a("tpA%d" % i, [128, 512]) for i in range(2)]
            tpB = [sb3a("tpB%d" % i, [128, 512]) for i in range(2)]
            S.barrier()
            ytok = [sb3a("ytok%d" % i, [128, 2 * D], BF16) for i in range(2)]
            for ti in range(NT_O):
                sl = ti % 2
                S.add("sp", lambda e, ti=ti, sl=sl: e.dma_start(out=ytok[sl][:], in_=ydram[ti * 128:(ti + 1) * 128, :]),
                      w=[("ytok", sl)], dma=True)
                for hb in range(2):
                    bank = (2 * ti + hb) % 4
                    pbv = psb[bank][:].bitcast(BF16)

                    def fyt(e, sl=sl, hb=hb, pbv=pbv):
                        ins = None
                        for c in range(8):
                            cc = hb * 8 + c
                            ins = e.transpose(pbv[:, c * 128:(c + 1) * 128], ytok[sl][:, cc * 128:(cc + 1) * 128], ident_b[:])
                        return ins
                    S.add("pe", fyt, r=[("ytok", sl), "ident_b"], w=[PSK(bank)])
                    S.add("act" if hb == 0 else "dve",
                          (lambda e, ti=ti, hb=hb, pbv=pbv: e.copy(out=yT[:, hb * 8:hb * 8 + 8, ti * 128:(ti + 1) * 128],
                                                                 in_=pbv[:, 0:1024].rearrange("p (c t) -> p c t", t=128)))
                          if hb == 0 else
                          (lambda e, ti=ti, hb=hb, pbv=pbv: e.tensor_copy(out=yT[:, hb * 8:hb * 8 + 8, ti * 128:(ti + 1) * 128],
                                                                        in_=pbv[:, 0:1024].rearrange("p (c t) -> p c t", t=128))),
                          r=[PSK(bank)], w=[("yT", c, ti) for c in range(hb * 8, hb * 8 + 8)])
            it = 0
            for ncn in range(8):
                p = ncn % 2
                S.add("sp", lambda e, p=p, ncn=ncn: e.dma_start(out=stgA[p][:], in_=w_bra_d[ncn].rearrange("(vc q) n -> q vc n", q=128)),
                      w=[("stgA", p)], dma=True)
                S.add("sp", lambda e, p=p, ncn=ncn: e.dma_start(out=stgB[p][:], in_=w_brb_d[ncn].rearrange("(vc q) n -> q vc n", q=128)),
                      w=[("stgB", p)], dma=True)
                S.add("pool", lambda e, p=p, ncn=ncn: e.dma_start(out=wmA[p][:], in_=w_m_d[ncn].rearrange("(dc q) n -> q dc n", q=128)),
                      w=[("wmA", p)], dma=True)
                S.add("pool", lambda e, p=p, ncn=ncn: e.dma_start(out=wmB[p][:], in_=w_m_d[8 + ncn].rearrange("(dc q) n -> q dc n", q=128)),
                      w=[("wmB", p)], dma=True)
                S.add("act", lambda e, p=p: e.activation(out=wbA[p][:], in_=stgA[p][:], func=AF.Copy, scale=hgn[:, 0:1]),
                      r=[("stgA", p), "hgn"], w=[("wbA", p)])
                for v2 in range(2):
                    S.add("act", lambda e, p=p, v2=v2: e.activation(
                        out=wbB[p][:].rearrange("q (h v) n -> q h v n", v=2)[:, :, v2, :],
                        in_=stgB[p][:].rearrange("q (h v) n -> q h v n", v=2)[:, :, v2, :],
                        func=AF.Copy, scale=glan[:, v2:v2 + 1]),
                        r=[("stgB", p), "glan"], w=[("wbB", p)])
                for tg in range(3):
                    b0 = 4 * (it % 2)
                    q = it % 2
                    it += 1
                    tk = slice(tg * 512, (tg + 1) * 512)
                    tiles = [("hT", t) for t in range(tg * 4, tg * 4 + 4)]
                    ytl = lambda c0: [("yT", c, t) for c in range(c0, c0 + 8) for t in range(tg * 4, tg * 4 + 4)]

                    def mm8(e, bank, wt, src, c0, tk=tk):
                        ins = None
                        for c in range(8):
                            ins = e.matmul(psb[bank][:, 0:512], lhsT=wt[:, c, :], rhs=src[:, c0 + c, tk],
                                           start=(c == 0), stop=(c == 7))
                        return ins
                    S.add("pe", lambda e, b0=b0, p=p, mm8=mm8: mm8(e, b0 + 2, wmA[p], hT, 0), r=[("wmA", p)] + tiles, w=[PSK(b0 + 2)])
                    S.add("pe", lambda e, b0=b0, p=p, mm8=mm8: mm8(e, b0 + 3, wmB[p], hT, 0), r=[("wmB", p)] + tiles, w=[PSK(b0 + 3)])
                    S.add("pe", lambda e, b0=b0, p=p, mm8=mm8: mm8(e, b0 + 0, wbA[p], yT, 0), r=[("wbA", p)] + ytl(0), w=[PSK(b0 + 0)])
                    S.add("pe", lambda e, b0=b0, p=p, mm8=mm8: mm8(e, b0 + 1, wbB[p], yT, 8), r=[("wbB", p)] + ytl(8), w=[PSK(b0 + 1)])
                    S.add("act", lambda e, b0=b0, q=q: e.activation(out=sgA[q][:], in_=psb[b0 + 2][:, 0:512], func=AF.Sigmoid),
                          r=[PSK(b0 + 2)], w=[("sgA", q)])
                    S.add("act", lambda e, b0=b0, q=q: e.activation(out=sgB[q][:], in_=psb[b0 + 3][:, 0:512], func=AF.Sigmoid),
                          r=[PSK(b0 + 3)], w=[("sgB", q)])
                    S.add("dve", lambda e, b0=b0, q=q: e.tensor_tensor(out=tpA[q][:], in0=psb[b0 + 0][:, 0:512], in1=sgA[q][:], op=ALU.mult),
                          r=[PSK(b0 + 0), ("sgA", q)], w=[("tpA", q)])
                    S.add("dve", lambda e, b0=b0, q=q: e.tensor_tensor(out=tpB[q][:], in0=psb[b0 + 1][:, 0:512], in1=sgB[q][:], op=ALU.mult),
                          r=[PSK(b0 + 1), ("sgB", q)], w=[("tpB", q)])
                    S.add("pool", lambda e, q=q, ncn=ncn, tk=tk: e.tensor_tensor(out=mergedT[:, ncn, tk], in0=tpA[q][:], in1=tpB[q][:], op=ALU.add),
                          r=[("tpA", q), ("tpB", q)], w=[("mergedT", ncn, tg)])
        if "mergedT" in dbg:
            S.add("sp", lambda e: e.dma_start(out=dbg_out["mergedT"], in_=mergedT[:]),
                  r=[("mergedT", n_, t_) for n_ in range(8) for t_ in range(3)], dma=True)
        S.barrier()
        x1s = yT[:].bitcast(F32).rearrange("p a b -> p (a b)").rearrange("p (t d) -> p t d", d=D)
        hflat = hT[:].rearrange("p a b -> p (a b)")
        h2T = hflat[:, 0:8 * TO].rearrange("p (a b) -> p a b", b=TO)
        cwT = sb3("cwT", [32, TO])
        ssq3 = sb3("ssq3", [128, NT_O])
        rs3 = sb3("rs3", [128, NT_O])
        with ExitStack() as st3b:
            def sb3b(name, shape, dt=F32):
                return st3b.enter_context(nc.sbuf_tensor("s_" + name, list(shape), dt))
            wout = sb3b("wout", [128, 8, D], BF16)
            wrt = sb3b("wrt", [128, 8, 36])
            g1bc = [sb3b("g1bc%d" % c, [128, D]) for c in range(2)]
            ones32 = sb3b("ones32", [128, 128])
            dg = [sb3b("dg%d" % i, [128, 128]) for i in range(2)]
            xr = [sb3b("xr%d" % i, [128, D]) for i in range(2)]
            tmpx = [sb3b("tmpx%d" % i, [128, D]) for i in range(2)]
            xn2 = [sb3b("xn2_%d" % i, [128, D]) for i in range(2)]
            h2f = [sb3b("h2f%d" % i, [128, 8, 128]) for i in range(2)]
            junk3 = sb3b("junk3", [128, D], BF16)
            NT = NT_O
            lga = sb3b("lga", [128, NT, 36])
            cwa = sb3b("cwa", [128, NT, 32])
            rA = sb3b("rA", [128, NT, 4])
            rB = sb3b("rB", [128, NT, 4])
            rC = sb3b("rC", [128, NT, 32])
            rsel = sb3b("rsel", [128, NT, 8])
            rsel2 = sb3b("rsel2", [128, NT, 8])
            rmk1 = sb3b("rmk1", [128, NT, 8])
            rmk2 = sb3b("rmk2", [128, NT, 8])
            rcw8 = sb3b("rcw8", [128, NT, 8])
            rs = sb3b("rs", [128, 10, NT])
            S.add("pool", lambda e: e.dma_start(out=wout[:], in_=w_out_d.rearrange("(c q) j -> q c j", q=128)), w=["wout"], dma=True)
            S.add("sp", lambda e: e.dma_start(out=wrt[:], in_=w_rt_d.rearrange("(c q) j -> q c j", q=128)), w=["wrt"], dma=True)
            S.add("pool", lambda e: e.memset(ones32[:], 1.0), w=["ones32"])

            def bcast_tile(dst, j0, c, nm):
                for jc in range(8):
                    d_ = dg[jc % 2]
                    S.add("dve", lambda e, d_=d_, jc=jc: e.tensor_scalar(out=d_[:], in0=ident_f[:], scalar1=modcol(j0, jc, c),
                                                                         scalar2=None, op0=ALU.mult),
                          r=["ident_f", "modT1"], w=[("dg", jc % 2)])
                    bank = jc % 2
                    S.add("pe", lambda e, d_=d_, bank=bank: e.matmul(psb[bank][:, 0:128], lhsT=ones32[:], rhs=d_[:], start=True, stop=True),
                          r=["ones32", ("dg", jc % 2)], w=[PSK(bank)])
                    S.add("act", lambda e, bank=bank, jc=jc: e.copy(out=dst[:, jc * 128:(jc + 1) * 128], in_=psb[bank][:, 0:128]),
                          r=[PSK(bank)], w=[nm])
            for c in range(2):
                bcast_tile(g1bc[c], 16, c, ("g1bc", c))

            def s3b_1(ti):
                c = cond_of_tile(ti)
                sl = ti % 2
                S.add("sp", lambda e, ti=ti, sl=sl: e.dma_start(out=xr[sl][:], in_=x_all[ti * 128:(ti + 1) * 128, :]),
                      w=[("xr", sl)], dma=True)
                for jh in range(2):
                    bank = 2 + jh

                    def fm(e, ti=ti, jh=jh, bank=bank):
                        ins = None
                        for n_ in range(8):
                            ins = e.matmul(psb[bank][:, 0:512], lhsT=mergedT[:, n_, ti * 128:(ti + 1) * 128],
                                           rhs=wout[:, n_, jh * 512:(jh + 1) * 512], start=(n_ == 0), stop=(n_ == 7))
                        return ins
                    S.add("pe", fm, r=["wout"] + [("mergedT", n_, ti // 4) for n_ in range(8)], w=[PSK(bank)])
                    js = slice(jh * 512, (jh + 1) * 512)
                    S.add("dve", lambda e, sl=sl, bank=bank, js=js, c=c: e.tensor_tensor(
                        out=tmpx[sl][:, js], in0=psb[bank][:, 0:512], in1=g1bc[c][:, js], op=ALU.mult),
                        r=[PSK(bank), ("g1bc", c)], w=[("tmpx", sl)])
                    S.add("pool", lambda e, sl=sl, js=js, ti=ti: e.tensor_tensor(
                        out=x1s[:, ti, js], in0=tmpx[sl][:, js], in1=xr[sl][:, js], op=ALU.add),
                        r=[("tmpx", sl), ("xr", sl)], w=[("x1s", ti)])

            def s3b_2(ti):
                c = cond_of_tile(ti)
                sl = ti % 2
                S.add("act", lambda e, ti=ti: e.activation(out=junk3[:], in_=x1s[:, ti, :], func=AF.Square, accum_out=ssq3[:, ti:ti + 1]),
                      r=[("x1s", ti)], w=["junk3", ("ssq3", ti)])
                S.add("act", lambda e, ti=ti: e.activation(out=rs3[:, ti:ti + 1], in_=ssq3[:, ti:ti + 1], func=AF.Ln, bias=epsb[:], scale=1.0 / D),
                      r=[("ssq3", ti), "epsb"], w=[("rs3", ti)])
                S.add("act", lambda e, ti=ti: e.activation(out=rs3[:, ti:ti + 1], in_=rs3[:, ti:ti + 1], func=AF.Exp, scale=-0.5),
                      r=[("rs3", ti)], w=[("rs3", ti)])
                S.add("dve", lambda e, ti=ti, sl=sl: e.tensor_scalar(out=xn2[sl][:], in0=x1s[:, ti, :], scalar1=rs3[:, ti:ti + 1],
                                                                     scalar2=None, op0=ALU.mult),
                      r=[("x1s", ti), ("rs3", ti)], w=[("xn2", sl)])
                for half in range(2):
                    bank = 4 + half

                    def ft(e, sl=sl, half=half, bank=bank):
                        ins = None
                        for j in range(4):
                            dc = half * 4 + j
                            ins = e.transpose(psb[bank][:, j * 128:(j + 1) * 128], xn2[sl][:, dc * 128:(dc + 1) * 128], ident_f[:])
                        return ins
                    S.add("pe", ft, r=[("xn2", sl), "ident_f"], w=[PSK(bank)])
                    for j in range(4):
                        dc = half * 4 + j
                        S.add("dve" if half == 0 else "act",
                              (lambda e, sl=sl, bank=bank, j=j, dc=dc, c=c: e.tensor_scalar(
                                  out=h2f[sl][:, dc, :], in0=psb[bank][:, j * 128:(j + 1) * 128],
                                  scalar1=A2[:, dc * 2 + c:dc * 2 + c + 1], scalar2=modcol(24, dc, c), op0=ALU.mult, op1=ALU.add))
                              if half == 0 else
                              (lambda e, sl=sl, bank=bank, j=j, dc=dc, c=c: e.activation(
                                  out=h2f[sl][:, dc, :], in_=psb[bank][:, j * 128:(j + 1) * 128], func=AF.Identity,
                                  bias=modcol(24, dc, c), scale=A2[:, dc * 2 + c:dc * 2 + c + 1])),
                              r=[PSK(bank), "A2", "modT1"], w=[("h2f", sl)])
                S.add("act", lambda e, sl=sl, ti=ti: e.copy(out=h2T[:, :, ti * 128:(ti + 1) * 128], in_=h2f[sl][:]),
                      r=[("h2f", sl)], w=[("h2T", ti)])
                def frt(e, sl=sl):
                    ins = None
                    for dc in range(8):
                        ins = e.matmul(psb[6][:, 0:36], lhsT=h2f[sl][:, dc, :], rhs=wrt[:, dc, :], start=(dc == 0), stop=(dc == 7))
                    return ins
                S.add("pe", frt, r=[("h2f", sl), "wrt"], w=[PSK(6)])
                S.add("act", lambda e, ti=ti: e.copy(out=lga[:, ti, :], in_=psb[6][:, 0:36]), r=[PSK(6)], w=[("lga", ti)])

            pipeline([s3b_1, s3b_2], NT_O, [0, 1])
            lgk = [("lga", t) for t in range(NT)]
            gm, gs, ptop, m1, m2, e2, den, w1, w2 = (rs[:, i, :] for i in range(9))
            lg = lga[:, :, 0:4]
            le = lga[:, :, 4:36].rearrange("p t (g e) -> p t g e", e=8)
            b3 = lambda ap, k: ap.rearrange("p (t o) -> p t o", o=1).to_broadcast([128, NT, k])
            V = lambda fn, r, w: S.add("dve", fn, r=r, w=w)
            V(lambda e: e.tensor_reduce(out=gm, in_=lg, axis=AX.X, op=ALU.max), lgk, ["r_gm"])
            V(lambda e: e.tensor_tensor(out=rA[:], in0=lg, in1=b3(gm, 4), op=ALU.is_ge), lgk + ["r_gm"], ["rA"])
            V(lambda e: e.tensor_tensor(out=rB[:], in0=lg, in1=b3(gm, 4), op=ALU.subtract), lgk + ["r_gm"], ["rB"])
            S.add("act", lambda e: e.activation(out=rB[:], in_=rB[:], func=AF.Exp), r=["rB"], w=["rB"])
            V(lambda e: e.tensor_reduce(out=gs, in_=rB[:], axis=AX.X, op=ALU.add), ["rB"], ["r_gs"])
            V(lambda e: e.reciprocal(out=ptop, in_=gs), ["r_gs"], ["r_ptop"])
            rC4 = rC[:].rearrange("p t (g e) -> p t g e", e=8)
            V(lambda e: e.tensor_tensor(out=rC4, in0=le, in1=rA[:].rearrange("p t (g o) -> p t g o", o=1).to_broadcast([128, NT, 4, 8]),
                                        op=ALU.mult), lgk + ["rA"], ["rC"])
            V(lambda e: e.tensor_reduce(out=rsel[:], in_=rC[:].rearrange("p t (g e) -> p t e g", e=8), axis=AX.X, op=ALU.add),
              ["rC"], ["rsel"])
            V(lambda e: e.tensor_reduce(out=m1, in_=rsel[:], axis=AX.X, op=ALU.max), ["rsel"], ["r_m1"])
            V(lambda e: e.tensor_tensor(out=rmk1[:], in0=rsel[:], in1=b3(m1, 8), op=ALU.is_ge), ["rsel", "r_m1"], ["rmk1"])
            V(lambda e: e.scalar_tensor_tensor(out=rsel2[:], in0=rmk1[:], scalar=-1e30, in1=rsel[:], op0=ALU.mult, op1=ALU.add),
              ["rmk1", "rsel"], ["rsel2"])
            V(lambda e: e.tensor_reduce(out=m2, in_=rsel2[:], axis=AX.X, op=ALU.max), ["rsel2"], ["r_m2"])
            V(lambda e: e.tensor_tensor(out=rmk2[:], in0=rsel2[:], in1=b3(m2, 8), op=ALU.is_ge), ["rsel2", "r_m2"], ["rmk2"])
            V(lambda e: e.tensor_sub(out=e2, in0=m2, in1=m1), ["r_m1", "r_m2"], ["r_e2"])
            S.add("act", lambda e: e.activation(out=e2, in_=e2, func=AF.Exp), r=["r_e2"], w=["r_e2"])
            V(lambda e: e.tensor_scalar(out=den, in0=e2, scalar1=1.0, scalar2=None, op0=ALU.add), ["r_e2"], ["r_den"])
            V(lambda e: e.reciprocal(out=den, in_=den), ["r_den"], ["r_den"])
            V(lambda e: e.tensor_tensor(out=w1, in0=den, in1=ptop, op=ALU.mult), ["r_den", "r_ptop"], ["r_w1"])
            V(lambda e: e.tensor_tensor(out=w2, in0=w1, in1=e2, op=ALU.mult), ["r_w1", "r_e2"], ["r_w2"])
            V(lambda e: e.tensor_tensor(out=rcw8[:], in0=rmk1[:], in1=b3(w1, 8), op=ALU.mult), ["rmk1", "r_w1"], ["rcw8"])
            V(lambda e: e.tensor_tensor(out=rmk2[:], in0=rmk2[:], in1=b3(w2, 8), op=ALU.mult), ["rmk2", "r_w2"], ["rmk2"])
            V(lambda e: e.tensor_tensor(out=rcw8[:], in0=rcw8[:], in1=rmk2[:], op=ALU.add), ["rcw8", "rmk2"], ["rcw8"])
            cwa4 = cwa[:].rearrange("p t (g e) -> p t g e", e=8)
            for g_ in range(4):
                V(lambda e, g_=g_: e.tensor_tensor(out=cwa4[:, :, g_, :], in0=rcw8[:], in1=rA[:, :, g_:g_ + 1].to_broadcast([128, NT, 8]),
                                                   op=ALU.mult), ["rcw8", "rA"], ["cwa"])
            for t3 in range(3):
                def ftc(e, t3=t3):
                    ins = None
                    for j in range(4):
                        ti = t3 * 4 + j
                        ins = e.transpose(psb[5 + t3][0:32, j * 128:(j + 1) * 128], cwa[:, ti, :], ident_f[:])
                    return ins
                S.add("pe", ftc, r=["cwa", "ident_f"], w=[PSK(5 + t3)])
                S.add("act", lambda e, t3=t3: e.copy(out=cwT[:, t3 * 512:(t3 + 1) * 512], in_=psb[5 + t3][0:32, 0:512]),
                      r=[PSK(5 + t3)], w=[("cwT", t3 * 4 + j) for j in range(4)])
        if "x1" in dbg:
            S.add("sp", lambda e: e.dma_start(out=dbg_out["x1"], in_=x1s[:]), r=[("x1s", t) for t in range(NT_O)], dma=True)
        if "cwT" in dbg:
            S.add("sp", lambda e: e.dma_start(out=dbg_out["cwT"], in_=cwT[:]), r=[("cwT", t) for t in range(NT_O)], dma=True)
        S.barrier()

        with ExitStack() as st4:
            def sb4(name, shape, dt=F32):
                return st4.enter_context(nc.sbuf_tensor("s_" + name, list(shape), dt))
            accA = mergedT[:].bitcast(F32).rearrange("p a b -> p (a b)").rearrange("p (t d) -> p t d", d=D)
            accB = sb4("accB", [128, 6, D])
            acc = lambda ti: accA[:, ti, :] if ti < 6 else accB[:, ti - 6, :]
            spare = hflat[:, 8 * TO:8 * TA]
            wsl = lambda i: spare[:, i * 2048:(i + 1) * 2048].rearrange("p (c f) -> p c f", f=256)
            wg = [wsl(0), wsl(1)]
            wu = [wsl(2), wsl(3)]
            wd = [sb4("wd%d" % i, [128, 2, D], BF16) for i in range(2)]
            selE = [sb4("selE%d" % i, [32, 128]) for i in range(2)]
            selEb = [sb4("selEb%d" % i, [32, 128], BF16) for i in range(2)]
            cw_hi = sb4("cw_hi", [32, TO], BF16)
            cw_lo = sb4("cw_lo", [32, TO], BF16)
            cw_d = sb4("cw_d", [32, TO])
            cwk = [("cwT", t) for t in range(NT_O)]
            S.add("dve", lambda e: e.tensor_copy(out=cw_hi[:], in_=cwT[:]), r=cwk, w=["cw_hi"])
            S.add("dve", lambda e: e.tensor_tensor(out=cw_d[:], in0=cwT[:], in1=cw_hi[:], op=ALU.subtract), r=cwk + ["cw_hi"], w=["cw_d"])
            S.add("dve", lambda e: e.tensor_copy(out=cw_lo[:], in_=cw_d[:]), r=["cw_d"], w=["cw_lo"])
            sA = [sb4("sA%d" % i, [128, 512]) for i in range(2)]
            tA = [sb4("tA%d" % i, [128, 512]) for i in range(2)]
            cbs = [sb4("cbs%d" % i, [128, 512]) for i in range(2)]
            hid = [[sb4("hid%d_%d" % (i, j), [128, 512], BF16) for j in range(2)] for i in range(2)]
            g2bc = [sb4("g2bc%d" % c, [128, D]) for c in range(2)]
            fngbc = sb4("fngbc", [128, D])
            ones32b = sb4("ones32b", [128, 128])
            dgb = [sb4("dgb%d" % i, [128, 128]) for i in range(2)]
            junk4 = sb4("junk4", [128, D], BF16)
            ssq4 = sb4("ssq4", [128, NT_O])
            rs4 = sb4("rs4", [128, NT_O])
            S.add("pool", lambda e: e.memset(ones32b[:], 1.0), w=["ones32b"])
            S.add("sp", lambda e: e.dma_start(out=fngbc[:], in_=fng_d.to_broadcast([128, D])), w=["fngbc"], dma=True)
            for c in range(2):
                for jc in range(8):
                    d_ = dgb[jc % 2]
                    bank = jc % 2
                    S.add("dve", lambda e, d_=d_, jc=jc, c=c: e.tensor_scalar(out=d_[:], in0=ident_f[:], scalar1=modcol(40, jc, c),
                                                                              scalar2=None, op0=ALU.mult),
                          r=["ident_f", "modT1"], w=[("dgb", jc % 2)])
                    S.add("pe", lambda e, d_=d_, bank=bank: e.matmul(psb[bank][:, 0:128], lhsT=ones32b[:], rhs=d_[:], start=True, stop=True),
                          r=["ones32b", ("dgb", jc % 2)], w=[PSK(bank)])
                    S.add("act", lambda e, bank=bank, jc=jc, c=c: e.copy(out=g2bc[c][:, jc * 128:(jc + 1) * 128], in_=psb[bank][:, 0:128]),
                          r=[PSK(bank)], w=[("g2bc", c)])

            NE = dbg.get("_nexp", NEXP)
            itc = 0
            pending = [None]
            for e_ in range(NE):
                pp = e_ % 2
                S.add("pool", lambda e, pp=pp, e_=e_: e.dma_start(out=wg[pp], in_=w_eg_d[e_].rearrange("(c q) f -> q c f", q=128)),
                      w=[("wg", pp)], dma=True)
                S.add("pool", lambda e, pp=pp, e_=e_: e.dma_start(out=wu[pp], in_=w_eu_d[e_].rearrange("(c q) f -> q c f", q=128)),
                      w=[("wu", pp)], dma=True)
                S.add("pool", lambda e, pp=pp, e_=e_: e.dma_start(out=wd[pp][:], in_=w_ed_d[e_].rearrange("(c q) d -> q c d", q=128)),
                      w=[("wd", pp)], dma=True)
                S.add("pool", lambda e, pp=pp: e.memset(selE[pp][:], 0.0), w=[("selE", pp)])
                S.add("pool", lambda e, pp=pp, e_=e_: e.affine_select(out=selE[pp][:], in_=selE[pp][:], pattern=[[0, 128]],
                                                                     compare_op=ALU.not_equal, fill=1.0, base=-e_, channel_multiplier=1),
                      r=[("selE", pp)], w=[("selE", pp)])
                S.add("pool", lambda e, pp=pp: e.tensor_copy(out=selEb[pp][:], in_=selE[pp][:]), r=[("selE", pp)], w=[("selEb", pp)])
                for tg in range(3):
                    tk = slice(tg * 512, (tg + 1) * 512)
                    hs = itc % 2
                    itc += 1
                    def fsel(e, pp=pp, tk=tk):
                        e.matmul(psb[4][:, 0:512], lhsT=selEb[pp][:], rhs=cw_hi[:, tk], start=True, stop=False)
                        return e.matmul(psb[4][:, 0:512], lhsT=selEb[pp][:], rhs=cw_lo[:, tk], start=False, stop=True)
                    S.add("pe", fsel, r=[("selEb", pp), "cw_hi", "cw_lo"], w=[PSK(4)])
                    S.add("act", lambda e, hs=hs: e.copy(out=cbs[hs][:], in_=psb[4][:, 0:512]), r=[PSK(4)], w=[("cbs", hs)])
                    for f in range(2):
                        def fg(e, wt, bank, f=f, tk=tk):
                            ins = None
                            for dc in range(8):
                                ins = e.matmul(psb[bank][:, 0:512], lhsT=wt[:, dc, f * 128:(f + 1) * 128], rhs=h2T[:, dc, tk],
                                               start=(dc == 0), stop=(dc == 7))
                            return ins
                        ba, bu = 2 * f, 2 * f + 1
                        h2k = [("h2T", t) for t in range(tg * 4, tg * 4 + 4)]
                        S.add("pe", lambda e, fg=fg, pp=pp, ba=ba: fg(e, wg[pp], ba), r=[("wg", pp)] + h2k, w=[PSK(ba)])
                        S.add("pe", lambda e, fg=fg, pp=pp, bu=bu: fg(e, wu[pp], bu), r=[("wu", pp)] + h2k, w=[PSK(bu)])
                        S.add("act", lambda e, ba=ba, f=f: e.activation(out=sA[f][:], in_=psb[ba][:, 0:512], func=AF.Silu),
                              r=[PSK(ba)], w=[("sA", f)])
                        S.add("dve", lambda e, bu=bu, f=f: e.tensor_tensor(out=tA[f][:], in0=psb[bu][:, 0:512], in1=sA[f][:], op=ALU.mult),
                              r=[PSK(bu), ("sA", f)], w=[("tA", f)])
                        S.add("pool", lambda e, f=f, hs=hs: e.tensor_tensor(out=hid[hs][f][:], in0=tA[f][:], in1=cbs[hs][:], op=ALU.mult),
                              r=[("tA", f), ("cbs", hs)], w=[("hid", hs, f)])
                    def emit_down(tg=tg, hs=hs, pp=pp, e_=e_):
                        for t4 in range(4):
                            ti = tg * 4 + t4
                            for dh in range(2):
                                bank = 5 + (t4 * 2 + dh) % 3

                                def fd(e, t4=t4, dh=dh, bank=bank):
                                    ins = None
                                    for f in range(2):
                                        ins = e.matmul(psb[bank][:, 0:512], lhsT=hid[hs][f][:, t4 * 128:(t4 + 1) * 128],
                                                       rhs=wd[pp][:, f, dh * 512:(dh + 1) * 512], start=(f == 0), stop=(f == 1))
                                    return ins
                                S.add("pe", fd, r=[("hid", hs, 0), ("hid", hs, 1), ("wd", pp)], w=[PSK(bank)])
                                ds = slice(dh * 512, (dh + 1) * 512)
                                a_ap = acc(ti)[:, ds]
                                if e_ == 0:
                                    S.add("dve", lambda e, a_ap=a_ap, bank=bank: e.tensor_copy(out=a_ap, in_=psb[bank][:, 0:512]),
                                          r=[PSK(bank)], w=[("acc", ti, dh)])
                                else:
                                    S.add("dve", lambda e, a_ap=a_ap, bank=bank: e.tensor_tensor(
                                        out=a_ap, in0=psb[bank][:, 0:512], in1=a_ap, op=ALU.add),
                                        r=[PSK(bank), ("acc", ti, dh)], w=[("acc", ti, dh)])
                    if pending[0] is not None:
                        pending[0]()
                    pending[0] = emit_down
            pending[0]()
            def fin_1(ti):
                c = cond_of_tile(ti)
                a_ap = acc(ti)
                ak = [("acc", ti, 0), ("acc", ti, 1)]
                S.add("pool", lambda e: e.tensor_tensor(out=a_ap, in0=a_ap, in1=g2bc[c][:], op=ALU.mult),
                      r=ak + [("g2bc", c)], w=ak)
                S.add("dve", lambda e: e.tensor_tensor(out=a_ap, in0=a_ap, in1=x1s[:, ti, :], op=ALU.add),
                      r=ak + [("x1s", ti)], w=ak)
                S.add("act", lambda e: e.activation(out=junk4[:], in_=a_ap, func=AF.Square, accum_out=ssq4[:, ti:ti + 1]),
                      r=ak, w=["junk4", ("ssq4", ti)])
                S.add("act", lambda e: e.activation(out=rs4[:, ti:ti + 1], in_=ssq4[:, ti:ti + 1], func=AF.Ln, bias=epsb[:], scale=1.0 / D),
                      r=[("ssq4", ti), "epsb"], w=[("rs4", ti)])
                S.add("act", lambda e: e.activation(out=rs4[:, ti:ti + 1], in_=rs4[:, ti:ti + 1], func=AF.Exp, scale=-0.5),
                      r=[("rs4", ti)], w=[("rs4", ti)])

            def fin_2(ti):
                a_ap = acc(ti)
                ak = [("acc", ti, 0), ("acc", ti, 1)]
                S.add("dve", lambda e: e.tensor_tensor(out=a_ap, in0=a_ap, in1=fngbc[:], op=ALU.mult), r=ak + ["fngbc"], w=ak)
                S.add("act", lambda e: e.activation(out=a_ap, in_=a_ap, func=AF.Copy, scale=rs4[:, ti:ti + 1]),
                      r=ak + [("rs4", ti)], w=ak)
                S.add("sp", lambda e: e.dma_start(out=y_out[ti * 128:(ti + 1) * 128, :], in_=a_ap), r=ak, dma=True)

            pipeline([fin_1, fin_2], NT_O, [0, 1])
        if "hT" in dbg:
            tmp = sb("dbg_hT", [128, 8, TA], F32)
            S.add("dve", lambda e: e.tensor_copy(out=tmp[:], in_=hT[:]), r=[("hT", i) for i in range(NT_A)], w=["dbg_hT"])
            S.add("sp", lambda e: e.dma_start(out=dbg_out["hT"], in_=tmp[:]), r=["dbg_hT"], dma=True)
        if "modT" in dbg:
            S.add("sp", lambda e: e.dma_start(out=dbg_out["modT"], in_=modT[:]), r=["modT"], dma=True)

        S.emit(st)
    return nc


def _prep_core(c, I):
    b, par = c // 2, c % 2
    f = np.ascontiguousarray
    xs = I["x_sample"][b]
    if par:
        xs = xs[::-1]
    p0 = I["x_prompt"][2 * c]
    p1 = I["x_prompt"][2 * c + 1]
    if par:
        p0, p1 = p0[::-1], p1[::-1]
    x_all = np.concatenate([xs[:1024], p0, p1, xs[1024:]], 0)
    cond = np.stack([I["c"][b], I["c_ctx"]], 0)
    condT = cond.reshape(2, 8, 128).transpose(2, 1, 0).reshape(128, 16)
    w_in = I["w_in"][0]
    o = np.cumsum([0, 1024, 1024, 1024, 1024, 1024, 512, 512, 1024, 1024, 16, 16, 1024, 1024])
    hq, hf0, hf1, hi, hgate, gq, gk, gv, gr, ga0, ga1, ma, mb = [w_in[:, o[i]:o[i + 1]] for i in range(13)]
    d0, d1 = (1, 0) if par else (0, 1)
    if par:
        hf0, hf1 = hf1, hf0
        ga0, ga1 = ga1, ga0
    hd = lambda w, h, n: w[:, h * n:(h + 1) * n]
    w_hg = np.stack([np.concatenate([hd(hq, h, 128), hd(hf0, h, 128), hd(hf1, h, 128), hd(hi, h, 128),
                                     hd(hgate, h, 128)], 1) for h in range(8)], 0)
    w_gla = np.stack([np.concatenate([hd(gq, h, 128), hd(gk, h, 128), hd(gv, h, 256), hd(gr, h, 256)], 1)
                      for h in range(4)], 0)
    w_ga = np.concatenate([ga0, ga1], 1)
    w_m = np.concatenate([ma.reshape(D, 8, 128).transpose(1, 0, 2), mb.reshape(D, 8, 128).transpose(1, 0, 2)], 0)
    lbp = np.concatenate([I["hg_lb_param"][0].reshape(8, 128).T, I["hg_lb_param"][1].reshape(8, 128).T], 1)
    wa = I["gla_wa_up"][0][[d0, d1]]
    ba = I["gla_ba"][0][[d0, d1]]
    baT = ba.reshape(2, 4, 128).transpose(2, 0, 1).reshape(128, 8)
    m = {
        "x_all": x_all, "condT": condT, "ada_w": I["ada_w"][0],
        "ada_bT": I["ada_b"][0].reshape(48, 128).T,
        "g1T": I["norm1_g"][0].reshape(8, 128).T, "g2T": I["norm2_g"][0].reshape(8, 128).T,
        "w_hg": w_hg, "w_gla": w_gla, "w_ga": w_ga, "w_m": w_m, "lbp": lbp,
        "hgn": I["hg_norm_g"][0].reshape(128, 1), "glan": I["gla_norm_g"][0].reshape(2, 128).T,
        "wa_up": wa, "baT": baT,
        "w_bra": I["w_br_a"][0].reshape(D, 8, 128).transpose(1, 0, 2),
        "w_brb": I["w_br_b"][0].reshape(D, 8, 128).transpose(1, 0, 2),
        "w_out": I["w_out"][0],
        "w_rt": np.concatenate([I["w_router_group"][0]] + [I["w_router_expert"][0][g] for g in range(4)], 1),
        "w_eg": I["w_exp_gate"][0], "w_eu": I["w_exp_up"][0], "w_ed": I["w_exp_down"][0],
        "fng": I["final_norm_g"].reshape(1, D),
        "s0_hg": I["state_hgrn"][b, 0][[d0, d1]], "s0_gla": I["state_gla"][b, 0][[d0, d1]],
    }
    return {k: f(np.asarray(v, dtype=np.float32)) for k, v in m.items()}


_NC_CACHE = {}


def kernel(**inputs):
    I = {k: np.asarray(v) for k, v in inputs.items()}
    if "nc" not in _NC_CACHE:
        _NC_CACHE["nc"] = build()
    nc = _NC_CACHE["nc"]
    in_maps = [_prep_core(c, I) for c in range(8)]
    res = run_bass_kernel_spmd(nc, in_maps, core_ids=list(range(8)))
    y_prompt = np.zeros((16, 256, D), np.float32)
    y_sample = np.zeros((4, 2048, D), np.float32)
    st_h = np.zeros((16, 1, 2, 8, 128, 128), np.float32)
    st_g = np.zeros((16, 1, 2, 4, 128, 256), np.float32)
    for c in range(8):
        r = res.results[c]
        b, par = c // 2, c % 2
        y = r["y_out"]
        ys, yp0, yp1 = y[:1024], y[1024:1280], y[1280:1536]
        if par:
            y_sample[b, 1024:] = ys[::-1]
            y_prompt[2 * c] = yp0[::-1]
            y_prompt[2 * c + 1] = yp1[::-1]
        else:
            y_sample[b, :1024] = ys
            y_prompt[2 * c] = yp0
            y_prompt[2 * c + 1] = yp1
        sh, sg = r["st_hg"], r["st_gla"]
        if par:
            sh, sg = sh[:, ::-1], sg[:, ::-1]
        st_h[2 * c:2 * c + 2, 0] = sh
        st_g[2 * c:2 * c + 2, 0] = sg
    return (y_prompt, y_sample, st_h, st_g)
```

```python
import os
from contextlib import ExitStack
import numpy as np
import concourse.bass as bass
import concourse.mybir as mybir
from concourse.bass_utils import run_bass_kernel_spmd

F32 = mybir.dt.float32
BF16 = mybir.dt.bfloat16
AF = mybir.ActivationFunctionType
ALU = mybir.AluOpType
AX = mybir.AxisListType

D = 1024
TO = 1536
TOTH = 1024
TA = TO + TOTH
NT_O = TO // 128
NT_A = TA // 128
EPS = 1e-6
NEXP = 32


class Sched:
    def __init__(self, nc):
        self.nc = nc
        self.ops = []
        self.lw = {}
        self.rd = {}
        self.bar = set()

    def barrier(self):
        last = {}
        dmas = {}
        for i, op in enumerate(self.ops):
            last[op["eng"]] = i
            if op["dma"]:
                dmas.setdefault(op["eng"], []).append(i)
        b = set(last.values())
        for e, l in dmas.items():
            b.update(l[-8:])
        self.bar = b

    def add(self, eng, fn, r=(), w=(), dma=False):
        deps = set(self.bar)
        for k in r:
            if k in self.lw:
                deps.add(self.lw[k])
        for k in w:
            if k in self.lw:
                deps.add(self.lw[k])
            deps.update(self.rd.get(k, ()))
        i = len(self.ops)
        self.ops.append(dict(eng=eng, fn=fn, deps=deps, dma=dma, need=dma, sig=None, pre=None))
        for k in r:
            self.rd.setdefault(k, []).append(i)
        for k in w:
            self.lw[k] = i
            self.rd[k] = []
        return i

    def emit(self, stack):
        nc = self.nc
        ops = self.ops
        for op in ops:
            for d in op["deps"]:
                if ops[d]["dma"] or not (ops[d]["eng"] == "pe" and op["eng"] == "pe"):
                    ops[d]["need"] = True
        engs = ("pe", "act", "dve", "pool", "sp")
        esem = {e: stack.enter_context(nc.semaphore("c_" + e)) for e in engs}
        KD = 8
        dsem = {e: [stack.enter_context(nc.semaphore("d_%s%d" % (e, i))) for i in range(KD)]
                for e in ("sp", "pool", "act")}
        ecnt = {e: 0 for e in engs}
        dcnt = {e: 0 for e in dsem}
        dfinal = {}
        for op in ops:
            e = op["eng"]
            if op["dma"]:
                j = dcnt[e]
                dcnt[e] += 1
                s = dsem[e][j % KD]
                u = j // KD
                if u > 0:
                    op["pre"] = (s, 16 * u)
                op["sig"] = (s, 16 * (u + 1), 16)
                dfinal[id(s)] = (s, 16 * (u + 1))
            elif op["need"]:
                ecnt[e] += 1
                op["sig"] = (esem[e], ecnt[e], 1)

        def run(name, e):
            waited = {}

            def wait(s, v):
                if waited.get(id(s), 0) < v:
                    e.wait_ge(s, v)
                    waited[id(s)] = v

            for op in ops:
                if op["eng"] != name:
                    continue
                if op["pre"] is not None:
                    wait(*op["pre"])
                for d in sorted(op["deps"]):
                    dop = ops[d]
                    if dop["dma"] or not (dop["eng"] == "pe" and name == "pe"):
                        wait(dop["sig"][0], dop["sig"][1])
                ins = op["fn"](e)
                if op["sig"] is not None:
                    ins.then_inc(op["sig"][0], op["sig"][2])
            if name == "sp":
                for s, v in dfinal.values():
                    wait(s, v)

        with nc.Block() as block:
            @block.sync
            def _(e):
                run("sp", e)

            @block.tensor
            def _(e):
                run("pe", e)

            @block.scalar
            def _(e):
                run("act", e)

            @block.vector
            def _(e):
                run("dve", e)

            @block.gpsimd
            def _(e):
                run("pool", e)


def build(dbg=None):
    nc = bass.Bass("TRN2", target_bir_lowering=False)
    S = Sched(nc)
    dbg = dbg or {}

    def din(name, shape):
        return nc.dram_tensor(name, list(shape), F32, kind="ExternalInput").ap()

    def dout(name, shape, dt=F32):
        return nc.dram_tensor(name, list(shape), dt, kind="ExternalOutput").ap()

    x_all = din("x_all", [TA, D])
    condT_d = din("condT", [128, 16])
    ada_w_d = din("ada_w", [D, 6 * D])
    ada_bT_d = din("ada_bT", [128, 48])
    g1T_d = din("g1T", [128, 8])
    g2T_d = din("g2T", [128, 8])
    w_hg_d = din("w_hg", [8, D, 640])
    w_gla_d = din("w_gla", [4, D, 768])
    w_ga_d = din("w_ga", [D, 32])
    w_m_d = din("w_m", [16, D, 128])
    lbp_d = din("lbp", [128, 16])
    hgn_d = din("hgn", [128, 1])
    glan_d = din("glan", [128, 2])
    wa_up_d = din("wa_up", [2, 16, 512])
    nbaT_d = din("baT", [128, 8])
    w_bra_d = din("w_bra", [8, D, 128])
    w_brb_d = din("w_brb", [8, D, 128])
    w_out_d = din("w_out", [D, D])
    w_rt_d = din("w_rt", [D, 36])
    w_eg_d = din("w_eg", [NEXP, D, 256])
    w_eu_d = din("w_eu", [NEXP, D, 256])
    w_ed_d = din("w_ed", [NEXP, 256, D])
    fng_d = din("fng", [1, D])
    s0_hg_d = din("s0_hg", [2, 8, 128, 128])
    s0_gla_d = din("s0_gla", [2, 4, 128, 256])

    y_out = dout("y_out", [TO, D])
    st_hg = dout("st_hg", [2, 2, 8, 128, 128])
    st_gla = dout("st_gla", [2, 2, 4, 128, 256])
    dbg_out = {k: dout("dbg_" + k, shp, BF16 if k in ("yT",) else F32) for k, shp in dbg.items() if not k.startswith("_")}

    with ExitStack() as st:
        def sb(name, shape, dt=F32):
            return st.enter_context(nc.sbuf_tensor("s_" + name, list(shape), dt))

        psb = [st.enter_context(nc.psum_tensor("ps%d" % i, [128, 512], F32)) for i in range(8)]

        def PSK(b):
            return ("ps", b)

        ident_f = sb("ident_f", [128, 128])
        ident_b = sb("ident_b", [128, 128], BF16)
        ones_f = sb("ones_f", [128, 512], BF16)
        one_c = sb("one_c", [128, 1])
        mask32 = sb("mask32", [128, 1024], BF16)
        trimask = sb("trimask", [128, 512], BF16)
        S.add("pool", lambda e: e.memset(ident_f[:], 0.0), w=["ident_f"])
        S.add("pool", lambda e: e.affine_select(out=ident_f[:], in_=ident_f[:], pattern=[[-1, 128]],
                                                compare_op=ALU.not_equal, fill=1.0, base=0,
                                                channel_multiplier=1), r=["ident_f"], w=["ident_f"])
        S.add("dve", lambda e: e.tensor_copy(out=ident_b[:], in_=ident_f[:]), r=["ident_f"], w=["ident_b"])
        S.add("pool", lambda e: e.memset(ones_f[:], 1.0), w=["ones_f"])
        S.add("pool", lambda e: e.memset(one_c[:], 1.0), w=["one_c"])
        S.add("pool", lambda e: e.memset(mask32[:], 1.0), w=["mask32"])
        S.add("pool", lambda e: e.memset(mask32[:].rearrange("p (c j) -> p c j", j=32)[:, :, 0:1], 0.0),
              r=["mask32"], w=["mask32"])
        tmv = trimask[:].rearrange("p (a d t) -> p a d t", a=4, d=2)
        onv = ones_f[:].rearrange("p (a d t) -> p a d t", a=4, d=2)
        for half in range(2):
            pr = slice(half * 64, half * 64 + 64)
            S.add("pool", lambda e, pr=pr: e.affine_select(
                out=tmv[pr, :, 0, :], in_=onv[pr, :, 0, :], pattern=[[0, 4], [1, 64]],
                compare_op=ALU.is_ge, fill=0.0, base=0, channel_multiplier=-1),
                r=["ones_f"], w=["trimask"])
            S.add("pool", lambda e, pr=pr: e.affine_select(
                out=tmv[pr, :, 1, :], in_=onv[pr, :, 1, :], pattern=[[0, 4], [-1, 64]],
                compare_op=ALU.is_ge, fill=0.0, base=0, channel_multiplier=1),
                r=["ones_f"], w=["trimask"])

        condT = sb("condT", [128, 16])
        scT = sb("scT", [128, 16])
        ada_bT = sb("ada_bT", [128, 48])
        g1T = sb("g1T", [128, 8])
        g2T = sb("g2T", [128, 8])
        lbp = sb("lbp", [128, 16])
        hgn = sb("hgn", [128, 1])
        glan = sb("glan", [128, 2])
        baT = sb("baT_s", [128, 8])
        nbaT = sb("nbaT", [128, 8])
        modT = sb("modT", [128, 96])
        A1 = sb("A1", [128, 16])
        A2 = sb("A2", [128, 16])
        lb = sb("lb", [128, 8])
        oml = sb("oml", [128, 8])
        noml = sb("noml", [128, 8])
        epsb = sb("epsb", [128, 1])
        for t_, d_, nm in ((condT, condT_d, "condT"), (ada_bT, ada_bT_d, "ada_bT"), (g1T, g1T_d, "g1T"),
                           (g2T, g2T_d, "g2T"), (lbp, lbp_d, "lbp"), (hgn, hgn_d, "hgn"),
                           (glan, glan_d, "glan"), (baT, nbaT_d, "baT")):
            S.add("sp", lambda e, t_=t_, d_=d_: e.dma_start(out=t_[:], in_=d_), w=[nm], dma=True)
        S.add("pool", lambda e: e.memset(epsb[:], EPS), w=["epsb"])
        S.add("act", lambda e: e.activation(out=scT[:], in_=condT[:], func=AF.Silu), r=["condT"], w=["scT"])
        S.add("dve", lambda e: e.tensor_sub(out=lb[:], in0=lbp[:, 0:8], in1=lbp[:, 8:16]), r=["lbp"], w=["lb"])
        S.add("act", lambda e: e.activation(out=lb[:], in_=lb[:], func=AF.Sigmoid), r=["lb"], w=["lb"])
        S.add("dve", lambda e: e.tensor_scalar(out=oml[:], in0=lb[:], scalar1=-1.0, scalar2=1.0,
                                               op0=ALU.mult, op1=ALU.add), r=["lb"], w=["oml"])
        S.add("dve", lambda e: e.tensor_single_scalar(out=noml[:], in_=oml[:], scalar=-1.0, op=ALU.mult),
              r=["oml"], w=["noml"])
        S.add("dve", lambda e: e.tensor_single_scalar(out=nbaT[:], in_=baT[:], scalar=-1.0, op=ALU.mult),
              r=["baT"], w=["nbaT"])

        def pipeline(stages, n, skews):
            for i in range(n + max(skews)):
                for stg, sk in zip(stages, skews):
                    if 0 <= i - sk < n:
                        stg(i - sk)

        hT = sb("hT", [128, 8, TA], BF16)
        ydram = nc.dram_tensor("yscr", [TO, 2 * D], BF16).ap()
        ssq = sb("ssq", [128, NT_A])
        rstd = sb("rstd", [128, NT_A])
        mv = modT[:].rearrange("p (j c) -> p j c", c=2)

        def modkey(j):
            return "modT0" if j < 16 else "modT1"

        def modcol(j0, dc, c):
            return modT[:, (j0 + dc) * 2 + c:(j0 + dc) * 2 + c + 1]

        def cond_of_tile(ti):
            return 1 if 8 <= ti < 12 else 0

        with ExitStack() as st01:
            adaw = [st01.enter_context(nc.sbuf_tensor("adaw%d" % i, [128, 8, 512], F32)) for i in range(3)]
            scTb = st01.enter_context(nc.sbuf_tensor("scTb", [128, 16], BF16))
            modrow = st01.enter_context(nc.sbuf_tensor("modrow", [2, 6 * D], F32))
            xts = [st01.enter_context(nc.sbuf_tensor("xt%d" % i, [128, D], F32)) for i in range(3)]
            xns = [st01.enter_context(nc.sbuf_tensor("xn%d" % i, [128, D], BF16)) for i in range(3)]
            adv = ada_w_d.rearrange("(dc p) n -> p dc n", p=128)
            S.add("dve", lambda e: e.tensor_copy(out=scTb[:], in_=scT[:]), r=["scT"], w=["scTb"])

            def mod_block(blk):
                a = adaw[blk % 3]
                S.add("sp", lambda e: e.dma_start(out=a[:], in_=adv[:, :, blk * 512:(blk + 1) * 512]),
                      w=[("adaw", blk % 3)], dma=True)

                def f(e):
                    ins = None
                    for dc in range(8):
                        ins = e.matmul(psb[0][0:2, 0:512], lhsT=scT[:, dc * 2:dc * 2 + 2], rhs=a[:, dc, :],
                                       start=(dc == 0), stop=(dc == 7))
                    return ins
                S.add("pe", f, r=[("adaw", blk % 3), "scT"], w=[PSK(0)])
                S.add("dve", lambda e: e.tensor_copy(out=modrow[0:2, blk * 512:(blk + 1) * 512], in_=psb[0][0:2, 0:512]),
                      r=[PSK(0)], w=[("modrow", blk)])

                def ft(e):
                    ins = None
                    for j4 in range(4):
                        jc = blk * 4 + j4
                        ins = e.transpose(psb[7][:, jc * 2:jc * 2 + 2], modrow[0:2, jc * 128:(jc + 1) * 128], ident_f[0:2, 0:2])
                    return ins
                S.add("pe", ft, r=[("modrow", blk), "ident_f"], w=[PSK(7)])

            def mod_evac(j_lo, j_hi, key):
                for c in range(2):
                    S.add("dve", lambda e, c=c: e.tensor_tensor(
                        out=mv[:, j_lo:j_hi, c], in0=psb[7][:, 0:96].rearrange("p (j c) -> p j c", c=2)[:, j_lo:j_hi, c],
                        in1=ada_bT[:, j_lo:j_hi], op=ALU.add), r=[PSK(7), "ada_bT"], w=[key])

            for blk in range(4):
                mod_block(blk)
            mod_evac(0, 16, "modT0")
            Av = A1[:].rearrange("p (j c) -> p j c", c=2)
            for c in range(2):
                S.add("dve", lambda e, c=c: e.scalar_tensor_tensor(
                    out=Av[:, :, c], in0=mv[:, 8:16, c], scalar=1.0, in1=g1T[:],
                    op0=ALU.add, op1=ALU.mult), r=["modT0", "g1T"], w=["A1"])

            def p1_stage1(ti):
                xt = xts[ti % 3]
                xn = xns[ti % 3]
                S.add("sp", lambda e: e.dma_start(out=xt[:], in_=x_all[ti * 128:(ti + 1) * 128, :]),
                      w=[("xt", ti % 3)], dma=True)
                S.add("act", lambda e: e.activation(out=xn[:], in_=xt[:], func=AF.Square, accum_out=ssq[:, ti:ti + 1]),
                      r=[("xt", ti % 3)], w=[("xn", ti % 3), ("ssq", ti)])
                S.add("act", lambda e: e.activation(out=rstd[:, ti:ti + 1], in_=ssq[:, ti:ti + 1], func=AF.Sqrt,
                                                    bias=epsb[:], scale=1.0 / D),
                      r=[("ssq", ti), "epsb"], w=[("rstd", ti)])
                S.add("dve", lambda e: e.reciprocal(out=rstd[:, ti:ti + 1], in_=rstd[:, ti:ti + 1]),
                      r=[("rstd", ti)], w=[("rstd", ti)])
                S.add("dve", lambda e: e.tensor_scalar(out=xn[:], in0=xt[:], scalar1=rstd[:, ti:ti + 1], scalar2=None, op0=ALU.mult),
                      r=[("xt", ti % 3), ("rstd", ti)], w=[("xn", ti % 3)])

            def p1_stage2(ti):
                xn = xns[ti % 3]
                bank = 1 + (ti % 2)
                c = cond_of_tile(ti)
                pbv = psb[bank][:].bitcast(BF16)

                def ftr(e):
                    ins = None
                    for dc in range(8):
                        ins = e.transpose(pbv[:, dc * 128:(dc + 1) * 128], xn[:, dc * 128:(dc + 1) * 128], ident_b[:])
                    return ins
                S.add("pe", ftr, r=[("xn", ti % 3), "ident_b"], w=[PSK(bank)])
                for dc in range(8):
                    a_ap = A1[:, dc * 2 + c:dc * 2 + c + 1]
                    s_ap = modcol(0, dc, c)
                    o_ap = hT[:, dc, ti * 128:(ti + 1) * 128]
                    i_ap = pbv[:, dc * 128:(dc + 1) * 128]
                    if ti % 2 == 0:
                        S.add("dve", lambda e, o_ap=o_ap, i_ap=i_ap, a_ap=a_ap, s_ap=s_ap: e.tensor_scalar(
                            out=o_ap, in0=i_ap, scalar1=a_ap, scalar2=s_ap, op0=ALU.mult, op1=ALU.add),
                            r=[PSK(bank), "A1", "modT0"], w=[("hT", ti)])
                    else:
                        S.add("act", lambda e, o_ap=o_ap, i_ap=i_ap, a_ap=a_ap, s_ap=s_ap: e.activation(
                            out=o_ap, in_=i_ap, func=AF.Identity, bias=s_ap, scale=a_ap),
                            r=[PSK(bank), "A1", "modT0"], w=[("hT", ti)])

            def p1_mod(ti):
                if ti < 8:
                    mod_block(4 + ti)
                if ti == 8:
                    mod_evac(16, 48, "modT1")
                    Av2 = A2[:].rearrange("p (j c) -> p j c", c=2)
                    for c in range(2):
                        S.add("dve", lambda e, c=c: e.scalar_tensor_tensor(
                            out=Av2[:, :, c], in0=mv[:, 32:40, c], scalar=1.0, in1=g2T[:],
                            op0=ALU.add, op1=ALU.mult), r=["modT1", "g2T"], w=["A2"])
            pipeline([p1_stage1, p1_mod, p1_stage2], NT_A, [0, 0, 2])

        st2 = st.enter_context(ExitStack())
        S.barrier()

        def sb2(name, shape, dt=F32):
            return st2.enter_context(nc.sbuf_tensor("s_" + name, list(shape), dt))

        W = sb2("W", [128, 8, 768], BF16)
        NSET = 4
        B1 = sb2("B1", [128, NSET, 512])
        B2 = sb2("B2", [128, NSET, 512])
        B3 = sb2("B3", [128, NSET, 512])
        q_bf = sb2("q_bf", [128, TO], BF16)
        k_bf = sb2("k_bf", [128, 2048], BF16)
        k_o = sb2("k_o", [128, TOTH], BF16)
        qI = [sb2("qI%d" % d, [128, TO], BF16) for d in range(2)]
        qX2 = [sb2("qX2%d" % d, [128, TO], BF16) for d in range(2)]
        kX1 = [sb2("kX1%d" % d, [128, TO], BF16) for d in range(2)]
        kX2 = [sb2("kX2%d" % d, [128, TO], BF16) for d in range(2)]
        kSb = sb2("kSb", [128, NSET, 512], BF16)
        kS_tm = [sb2("kStm0", [128, NT_O, 128], BF16), sb2("kStm1", [128, NT_A, 128], BF16)]
        Dd = [sb2("Dd%d" % d, [128, 40]) for d in range(2)]
        Eo = sb2("Eo", [128, 8 * NSET, 2])
        v_tm = sb2("v_tm", [128, NT_A, 256], BF16)
        G_tm = sb2("G_tm", [128, NT_O, 256], BF16)
        S32 = sb2("S32", [128, 6, 256])
        S_bf = [sb2("S_bf0", [128, 4, 256], BF16), sb2("S_bf1", [128, 24, 256], BF16)]
        scS = sb2("scS", [128, 512], BF16)
        y_tm = [sb2("y_tm%d" % i, [128, 256], BF16) for i in range(4)]
        ssq2 = sb2("ssq2", [128, 160])
        rs2 = sb2("rs2", [128, 160])
        gaT = sb2("gaT", [64, TA], BF16)
        wa_bf = sb2("wa_bf", [64, 512], BF16)
        junk2 = sb2("junk2", [128, 256], BF16)

        for d in range(2):
            for i_, (arr, nm) in enumerate(((qX2[d], "qX2"), (kX1[d], "kX1"), (kX2[d], "kX2"))):
                S.add("dve" if (i_ + d) % 2 == 0 else "pool", lambda e, arr=arr: e.memset(arr[:], 0.0), w=[(nm, d)])
        S.add("pool", lambda e: e.memset(ssq2[:], 0.0), w=["ssq2"])
        for d in range(2):
            S.add("pool", lambda e, d=d: e.dma_start(out=wa_bf[d * 32:d * 32 + 16, :], in_=wa_up_d[d]),
                  w=[("wa_bf", d)], dma=True)

        GEO = [dict(fh=slice(0, 32), sh=slice(32, 64), pf=31, pse=63),
               dict(fh=slice(32, 64), sh=slice(0, 32), pf=32, pse=0)]
        cm_ctr = [0]
        tm_ctr = [0]
        st_ctr = [0]

        def c64(ap):
            return ap.rearrange("p (c j) -> p c j", j=64)

        def cmaj(col0, tok0, ntok, consume, M=128, wkey="Wc", wsrc=None, pbase=0):
            bank = cm_ctr[0] % 2
            cm_ctr[0] += 1
            src = W if wsrc is None else wsrc

            def f(e):
                ins = None
                for dc in range(8):
                    ins = e.matmul(psb[bank][pbase:pbase + M, 0:ntok], lhsT=src[:, dc, col0:col0 + M],
                                   rhs=hT[:, dc, tok0:tok0 + ntok], start=(dc == 0), stop=(dc == 7))
                return ins
            tiles = range(tok0 // 128, (tok0 + ntok) // 128)
            S.add("pe", f, r=[wkey] + [("hT", t) for t in tiles], w=[PSK(bank)])
            consume(psb[bank][pbase:pbase + M, 0:ntok], PSK(bank))

        def gating(d, n, tokbase, gbase, k_ap, kkey, se, own, par):
            g = GEO[d]
            fh, sh, pf, pse = g["fh"], g["sh"], g["pf"], g["pse"]
            ncnk = n // 64
            b1, b2, b3 = B1[:, par, 0:n], B2[:, par, 0:n], B3[:, par, 0:n]
            k1, k2, k3 = ("B1", par), ("B2", par), ("B3", par)
            c32v = c64(b3)
            e32v = c64(b1)
            iev = c64(b2)
            kv = c64(k_ap)
            Dv = Dd[d][:, gbase:gbase + ncnk]
            Dv3 = Dv.rearrange("p (c o) -> p c o", o=1)
            bc = lambda ap: ap.to_broadcast([128, ncnk, 32])
            S.add("act", lambda e: e.activation(out=b2, in_=b3, func=AF.Exp, scale=-se), r=[k3], w=[k2])
            if own:
                S.add("act", lambda e: e.activation(out=b1, in_=b3, func=AF.Exp, scale=se), r=[k3], w=[k1])
                EF = e32v[:, :, pf:pf + 1]
                ES = e32v[:, :, pse:pse + 1]
                ekey = k1
            else:
                eo = Eo[:, par * 8:par * 8 + ncnk, :]
                S.add("act", lambda e: e.activation(out=eo[:, :, 0:1], in_=c32v[:, :, pf:pf + 1], func=AF.Exp, scale=se),
                      r=[k3], w=[("Eo", par)])
                S.add("act", lambda e: e.activation(out=eo[:, :, 1:2], in_=c32v[:, :, pse:pse + 1], func=AF.Exp, scale=se),
                      r=[k3], w=[("Eo", par)])
                EF = eo[:, :, 0:1]
                ES = eo[:, :, 1:2]
                ekey = ("Eo", par)
            S.add("dve", lambda e: e.tensor_tensor(out=Dv3, in0=EF, in1=ES, op=ALU.mult), r=[ekey], w=[("Dd", d)])
            if own:
                qv = c64(q_bf[:, tokbase:tokbase + n])
                for (eng, dst, nm, a_, b_, hs) in (
                        ("pool", kX1[d], "kX1", kv, iev, fh), ("pool", kX2[d], "kX2", kv, iev, sh),
                        ("dve", qX2[d], "qX2", qv, e32v, sh), ("dve", qI[d], "qI", qv, e32v, fh)):
                    dv_ = c64(dst[:, tokbase:tokbase + n])
                    S.add(eng, lambda e, dv_=dv_, a_=a_, b_=b_, hs=hs: e.tensor_tensor(
                        out=dv_[:, :, hs], in0=a_[:, :, hs], in1=b_[:, :, hs], op=ALU.mult),
                        r=[kkey, "q_bf", k1, k2], w=[(nm, d)])
            S.add("pool", lambda e: e.tensor_tensor(out=iev[:, :, fh], in0=iev[:, :, fh], in1=bc(Dv3), op=ALU.mult),
                  r=[k2, ("Dd", d)], w=[k2])
            S.add("pool", lambda e: e.tensor_tensor(out=iev[:, :, sh], in0=iev[:, :, sh], in1=bc(ES), op=ALU.mult),
                  r=[k2, ekey], w=[k2])
            if own:
                S.add("dve", lambda e: e.tensor_tensor(out=e32v[:, :, sh], in0=e32v[:, :, sh], in1=bc(EF), op=ALU.mult),
                      r=[k1], w=[k1])
                dv_ = c64(qI[d][:, tokbase:tokbase + n])
                S.add("dve", lambda e: e.tensor_tensor(out=dv_[:, :, sh], in0=qv[:, :, sh], in1=e32v[:, :, sh], op=ALU.mult),
                      r=["q_bf", k1], w=[("qI", d)])
            ksb = kSb[:, par, 0:n]
            S.add("dve", lambda e: e.tensor_tensor(out=ksb, in0=k_ap, in1=b2, op=ALU.mult), r=[kkey, k2], w=[("kSb", par)])

        def ks_transpose(d, n, tokbase, par):
            ksb = kSb[:, par, 0:n]
            nt = n // 128
            tile0 = tokbase // 128
            pbv = psb[4][:].bitcast(BF16)

            def ftr(e):
                ins = None
                for j in range(nt):
                    ins = e.transpose(pbv[:, j * 128:(j + 1) * 128], ksb[:, j * 128:(j + 1) * 128], ident_b[:])
                return ins
            S.add("pe", ftr, r=[("kSb", par), "ident_b"], w=[PSK(4)])
            S.add("act", lambda e: e.copy(out=kS_tm[d][:, tile0:tile0 + nt, :].rearrange("p a b -> p (a b)"),
                                          in_=pbv[:, 0:nt * 128]), r=[PSK(4)], w=[("kStm", d)])

        def seg_scan(d, n, par):
            b2, b3 = B2[:, par, 0:n], B3[:, par, 0:n]
            if d == 0:
                S.add("dve", lambda e: e.tensor_tensor_scan(out=b3, data0=mask32[:, 0:n], data1=b2,
                                                            initial=0.0, op0=ALU.mult, op1=ALU.add),
                      r=[("B2", par), "mask32"], w=[("B3", par)])
            else:
                S.add("dve", lambda e: e.tensor_tensor_scan(out=b3[:, ::-1], data0=mask32[:, 0:n], data1=b2[:, ::-1],
                                                            initial=0.0, op0=ALU.mult, op1=ALU.add),
                      r=[("B2", par), "mask32"], w=[("B3", par)])

        def tok_of_chunk(g):
            return g * 64

        zero_state = set()

        def state_step(dv, ci, d, g, first_zero, copy_to=None):
            ti, hf = g // 2, g % 2
            pr = slice(hf * 64, hf * 64 + 64)
            bank = 5 + st_ctr[0] % 2
            st_ctr[0] += 1
            pS = psb[bank][:, 0:dv]
            pkey = PSK(bank)
            S.add("pe", lambda e: e.matmul(pS, lhsT=kS_tm[d][pr, ti, :], rhs=v_tm[pr, ti, 0:dv], start=True, stop=True),
                  r=[("kStm", d), ("v_tm", ti)], w=[pkey])
            if copy_to is not None:
                if first_zero:
                    zero_state.add((d, g))
                else:
                    S.add("act", lambda e: e.copy(out=copy_to[0], in_=S32[:, ci, 0:dv]), r=[("S32", ci)], w=[copy_to[1]])
            if first_zero:
                S.add("dve", lambda e: e.tensor_copy(out=S32[:, ci, 0:dv], in_=pS), r=[pkey], w=[("S32", ci)])
            else:
                S.add("dve", lambda e: e.scalar_tensor_tensor(
                    out=S32[:, ci, 0:dv], in0=S32[:, ci, 0:dv], scalar=Dd[d][:, g:g + 1], in1=pS,
                    op0=ALU.mult, op1=ALU.add), r=[pkey, ("S32", ci), ("Dd", d)], w=[("S32", ci)])

        def sweep_dir1(kind, h, dv):
            s0d = s0_hg_d if kind == "hg" else s0_gla_d
            std = st_hg if kind == "hg" else st_gla
            zero_state.clear()
            ch1 = [dict(ci=3, seq=list(range(39, 23, -1)) + list(range(15, -1, -1)), init=1, prompt=None),
                   dict(ci=4, seq=list(range(19, 15, -1)), init=None, prompt=0),
                   dict(ci=5, seq=list(range(23, 19, -1)), init=None, prompt=1)]
            S.add("sp", lambda e: e.dma_start(out=S32[:, 3, 0:dv], in_=s0d[1, h]), w=[("S32", 3)], dma=True)
            S.add("sp", lambda e: e.dma_start(out=S32[:, 0, 0:dv], in_=s0d[0, h]), w=[("S32", 0)], dma=True)
            for step in range(32):
                for ch in ch1:
                    if step >= len(ch["seq"]):
                        continue
                    g = ch["seq"][step]
                    ci = ch["ci"]
                    fz = ch["init"] is None and step == 0
                    cp = (S_bf[1][:, g, 0:dv], ("S_bf", 1, g)) if g < 24 else None
                    state_step(dv, ci, 1, g, fz, cp)
                    if ch["prompt"] is not None and step == len(ch["seq"]) - 1:
                        S.add("sp", lambda e, ci=ci, ch=ch: e.dma_start(out=std[ch["prompt"], 1, h], in_=S32[:, ci, 0:dv]),
                              r=[("S32", ci)], dma=True)
                if step % 4 == 3:
                    yield

        def dir0_and_output(kind, h, dv, ychunk0):
            std = st_hg if kind == "hg" else st_gla
            pscb = psb[7]
            pbv = psb[4][:].bitcast(BF16)
            nvc = dv // 128

            def stageA(ti):
                ci = 0 if ti < 8 else (1 if ti < 10 else 2)
                for hf in range(2):
                    g = 2 * ti + hf
                    fz = ci > 0 and g in (16, 20)
                    rs = g % 4
                    state_step(dv, ci, 0, g, fz, (S_bf[0][:, rs, 0:dv], ("S_bf", 0, rs)))
                if ti in (9, 11):
                    S.add("sp", lambda e, ci=ci: e.dma_start(out=std[ci - 1, 0, h], in_=S32[:, ci, 0:dv]),
                          r=[("S32", ci)], dma=True)
                slot = ti % 4

                def fsc(e):
                    ins = None
                    for hf in range(2):
                        g = 2 * ti + hf
                        tk = slice(g * 64, g * 64 + 64)
                        pr = slice(hf * 64, hf * 64 + 64)
                        for d in range(2):
                            o_ap = pscb[pr, slot * 128 + d * 64:slot * 128 + d * 64 + 64]
                            e.matmul(o_ap, lhsT=kX1[d][:, tk], rhs=qI[d][:, tk], start=True, stop=False)
                            ins = e.matmul(o_ap, lhsT=kX2[d][:, tk], rhs=qX2[d][:, tk], start=False, stop=True)
                    return ins
                S.add("pe", fsc, r=[("kX1", 0), ("kX1", 1), ("kX2", 0), ("kX2", 1), ("qI", 0), ("qI", 1),
                                    ("qX2", 0), ("qX2", 1)], w=[PSK(7)])
                S.add("dve", lambda e: e.tensor_tensor(
                    out=scS[:, slot * 128:(slot + 1) * 128], in0=pscb[:, slot * 128:(slot + 1) * 128],
                    in1=trimask[:, slot * 128:(slot + 1) * 128], op=ALU.mult),
                    r=[PSK(7), "trimask"], w=[("scS", slot)])

            def stageB(ti):
                slot = ti % 4
                ob = 2 + (ti % 2)
                po = psb[ob][:, 0:dv]

                def fo(e):
                    ins = None
                    for hf in range(2):
                        g = 2 * ti + hf
                        tk = slice(g * 64, g * 64 + 64)
                        pr = slice(hf * 64, hf * 64 + 64)
                        mms = []
                        if (0, g) not in zero_state:
                            mms.append((qI[0][:, tk], S_bf[0][:, g % 4, 0:dv]))
                        if (1, g) not in zero_state:
                            mms.append((qI[1][:, tk], S_bf[1][:, g, 0:dv]))
                        for d in range(2):
                            mms.append((scS[pr, slot * 128 + d * 64:slot * 128 + d * 64 + 64], v_tm[pr, ti, 0:dv]))
                        for i, (l, r_) in enumerate(mms):
                            ins = e.matmul(psb[ob][pr, 0:dv], lhsT=l, rhs=r_, start=(i == 0), stop=(i == len(mms) - 1))
                    return ins
                S.add("pe", fo, r=[("qI", 0), ("qI", 1), ("scS", slot), ("v_tm", ti)] +
                      [("S_bf", 0, (2 * ti + hf) % 4) for hf in range(2)] +
                      [("S_bf", 1, 2 * ti + hf) for hf in range(2)], w=[PSK(ob)])
                si = (ychunk0 if kind == "hg" else 8 + h) * 12 + ti
                sc_ = ssq2[:, si:si + 1]
                rc_ = rs2[:, si:si + 1]
                S.add("act", lambda e: e.activation(out=junk2[:, 0:dv], in_=po, func=AF.Square, accum_out=sc_),
                      r=[PSK(ob)], w=["junk2", ("ssq2", ti % 2)])
                S.add("act", lambda e: e.activation(out=rc_, in_=sc_, func=AF.Ln, bias=epsb[:], scale=1.0 / dv),
                      r=[("ssq2", ti % 2), "epsb"], w=[("rs2", ti % 2)])
                S.add("act", lambda e: e.activation(out=rc_, in_=rc_, func=AF.Exp, scale=-0.5), r=[("rs2", ti % 2)], w=[("rs2", ti % 2)])
                yt = y_tm[ti % 4]
                S.add("dve", lambda e: e.scalar_tensor_tensor(
                    out=yt[:, 0:dv], in0=po, scalar=rc_, in1=G_tm[:, ti, 0:dv], op0=ALU.mult, op1=ALU.mult),
                    r=[PSK(ob), ("rs2", ti % 2), ("G_tm", ti)], w=[("y_tm", ti % 4)])

            def stageC(ti):
                yt = y_tm[ti % 4]
                S.add("sp", lambda e: e.dma_start(out=ydram[ti * 128:(ti + 1) * 128, ychunk0 * 128:ychunk0 * 128 + dv], in_=yt[:, 0:dv]),
                      r=[("y_tm", ti % 4)], dma=True)

            for i in range(NT_O + 2):
                if i < NT_O:
                    stageA(i)
                if 0 <= i - 1 < NT_O:
                    stageB(i - 1)
                if 0 <= i - 2 < NT_O:
                    stageC(i - 2)

        def tmaj_pass(col_v, ncol_v, col_g, ncol_g):
            for ti in range(NT_A):
                own = ti < NT_O
                bank = 2 + tm_ctr[0] % 2
                tm_ctr[0] += 1
                ncol = ncol_v + (ncol_g if own else 0)

                def f(e, ti=ti, bank=bank, ncol=ncol):
                    ins = None
                    for dc in range(8):
                        ins = e.matmul(psb[bank][:, 0:ncol], lhsT=hT[:, dc, ti * 128:(ti + 1) * 128],
                                       rhs=W[:, dc, col_v:col_v + ncol], start=(dc == 0), stop=(dc == 7))
                    return ins
                S.add("pe", f, r=["Wt", ("hT", ti)], w=[PSK(bank)])
                S.add("act", lambda e, ti=ti, bank=bank: e.copy(out=v_tm[:, ti, 0:ncol_v], in_=psb[bank][:, 0:ncol_v]),
                      r=[PSK(bank)], w=[("v_tm", ti)])
                if own:
                    S.add("act", lambda e, ti=ti, bank=bank: e.activation(
                        out=G_tm[:, ti, 0:ncol_g], in_=psb[bank][:, ncol_v:ncol_v + ncol_g], func=AF.Silu),
                        r=[PSK(bank)], w=[("G_tm", ti)])

        PASSES = ([(1, TO + i * 512, 512, 24 + i * 8, False) for i in range(2)] +
                  [(1, i * 512, 512, i * 8, True) for i in range(3)] +
                  [(0, i * 512, 512, i * 8, True) for i in range(3)])
        N_D1 = 5

        def run_passes(p0, p1, p2_, sweep):
            n = len(PASSES)
            p0(0)
            sw = None
            for i in range(n + 2):
                if i + 1 < n:
                    p0(i + 1)
                if i < n:
                    p1(i)
                if 0 <= i - 1 < n:
                    p2_(i - 1)
                if 0 <= i - 2 < n:
                    d, tok0, nn, gb, own = PASSES[i - 2]
                    ks_transpose(d, nn, tok0, (i - 2) % NSET)
                    if i - 2 == N_D1 - 1:
                        sw = sweep()
                if sw is not None:
                    for _ in range(2):
                        next(sw, None)
            if sw is not None:
                for _ in sw:
                    pass

        def hg_head(h):
            wv = w_hg_d[h].rearrange("(dc p) n -> p dc n", p=128)
            S.add("pool", lambda e: e.dma_start(out=W[:, :, 0:384], in_=wv[:, :, 0:384]), w=["Wc"], dma=True)
            S.add("pool", lambda e: e.dma_start(out=W[:, :, 384:640], in_=wv[:, :, 384:640]), w=["Wt"], dma=True)
            for gi in range(3):
                cmaj(0, gi * 512, 512, lambda ps, pk, gi=gi: S.add(
                    "act", lambda e: e.activation(out=q_bf[:, gi * 512:(gi + 1) * 512], in_=ps, func=AF.Copy, scale=128 ** -0.5),
                    r=[pk], w=["q_bf"]))
            tmaj_pass(384, 128, 512, 128)

            pbank = {}

            def p0(pi):
                d, tok0, n, gb, own = PASSES[pi]
                col = 128 if d == 0 else 256
                cmaj(col, tok0, 512, lambda ps, pk: pbank.__setitem__(pi, (ps, pk)))

            def p1(pi):
                d, tok0, n, gb, own = PASSES[pi]
                par = pi % NSET
                ps, pk = pbank[pi]
                b1, b2, b3 = B1[:, par, :], B2[:, par, :], B3[:, par, :]
                k1, k2, k3 = ("B1", par), ("B2", par), ("B3", par)
                S.add("act", lambda e: e.activation(out=b1, in_=ps, func=AF.Exp, scale=-1.0), r=[pk], w=[k1])
                S.add("act", lambda e: e.activation(out=b3, in_=b1, func=AF.Ln, bias=one_c[:], scale=1.0), r=[k1, "one_c"], w=[k3])
                S.add("act", lambda e: e.activation(out=b2, in_=b1, func=AF.Ln, bias=one_c[:], scale=lb[:, h:h + 1]),
                      r=[k1, "one_c", "lb"], w=[k2])
                S.add("dve", lambda e: e.tensor_sub(out=b2, in0=b2, in1=b3), r=[k2, k3], w=[k2])
                S.add("act", lambda e: e.activation(out=b1, in_=b3, func=AF.Exp, scale=-1.0), r=[k3], w=[k1])
                k_ap = k_bf[:, par * 512:(par + 1) * 512]
                S.add("pool", lambda e: e.tensor_scalar(out=k_ap, in0=b1, scalar1=-1.0,
                                                        scalar2=noml[:, h:h + 1], op0=ALU.add, op1=ALU.mult),
                      r=[k1, "noml"], w=[("k_src", par)])
                seg_scan(d, n, par)

            def p2_(pi):
                d, tok0, n, gb, own = PASSES[pi]
                par = pi % NSET
                gating(d, n, tok0, gb, k_bf[:, par * 512:(par + 1) * 512], ("k_src", par), 1.0, own, par)
            run_passes(p0, p1, p2_, lambda: sweep_dir1("hg", h, 128))
            dir0_and_output("hg", h, 128, h)

        def gla_prep():
            wga = sb2("wga", [128, 8, 32], BF16)
            S.add("pool", lambda e: e.dma_start(out=wga[:], in_=w_ga_d.rearrange("(dc p) n -> p dc n", p=128)),
                  w=["wga"], dma=True)
            for d in range(2):
                ntok = TO if d == 0 else TA
                for gi in range(ntok // 512):
                    cmaj(d * 16, gi * 512, 512, lambda ps, pk, gi=gi, d=d: S.add(
                        "act", lambda e: e.copy(out=gaT[d * 32:d * 32 + 16, gi * 512:(gi + 1) * 512], in_=ps),
                        r=[pk], w=[("gaT", d)]), M=16, wkey="wga", wsrc=wga, pbase=d * 32)

        def gla_head(h):
            wv = w_gla_d[h].rearrange("(dc p) n -> p dc n", p=128)
            S.add("pool", lambda e: e.dma_start(out=W[:, :, 0:256], in_=wv[:, :, 0:256]), w=["Wc"], dma=True)
            S.add("pool", lambda e: e.dma_start(out=W[:, :, 256:768], in_=wv[:, :, 256:768]), w=["Wt"], dma=True)
            for gi in range(3):
                cmaj(0, gi * 512, 512, lambda ps, pk, gi=gi: S.add(
                    "act", lambda e: e.activation(out=q_bf[:, gi * 512:(gi + 1) * 512], in_=ps, func=AF.Copy, scale=128 ** -0.5),
                    r=[pk], w=["q_bf"]))
            for gi in range(5):
                dst = k_bf[:, gi * 512:(gi + 1) * 512] if gi < 3 else k_o[:, (gi - 3) * 512:(gi - 2) * 512]
                cmaj(128, gi * 512, 512, lambda ps, pk, dst=dst: S.add(
                    "act", lambda e: e.copy(out=dst, in_=ps), r=[pk], w=["k_gla"]))
            tmaj_pass(256, 256, 512, 256)

            pbank = {}

            def p0(pi):
                d, tok0, n, gb, own = PASSES[pi]
                bank = cm_ctr[0] % 2
                cm_ctr[0] += 1
                S.add("pe", lambda e: e.matmul(
                    psb[bank][:, 0:512], lhsT=wa_bf[d * 32:d * 32 + 16, h * 128:(h + 1) * 128],
                    rhs=gaT[d * 32:d * 32 + 16, tok0:tok0 + 512], start=True, stop=True),
                    r=[("wa_bf", d), ("gaT", d)], w=[PSK(bank)])
                pbank[pi] = bank

            def p1(pi):
                d, tok0, n, gb, own = PASSES[pi]
                par = pi % NSET
                bank = pbank[pi]
                S.add("act", lambda e: e.activation(
                    out=B1[:, par, :], in_=psb[bank][:, 0:512], func=AF.Exp,
                    bias=nbaT[:, d * 4 + h:d * 4 + h + 1], scale=-1.0),
                    r=[PSK(bank), "nbaT"], w=[("B1", par)])
                S.add("act", lambda e: e.activation(out=B2[:, par, :], in_=B1[:, par, :], func=AF.Ln, bias=one_c[:], scale=1.0),
                      r=[("B1", par), "one_c"], w=[("B2", par)])
                seg_scan(d, n, par)

            def p2_(pi):
                d, tok0, n, gb, own = PASSES[pi]
                par = pi % NSET
                k_ap = k_bf[:, tok0:tok0 + n] if own else k_o[:, tok0 - TO:tok0 - TO + n]
                gating(d, n, tok0, gb, k_ap, "k_gla", -1.0 / 16.0, own, par)
            run_passes(p0, p1, p2_, lambda: sweep_dir1("gla", h, 256))
            dir0_and_output("gla", h, 256, 8 + 2 * h)

        NH_HG = dbg.get("_nh_hg", 8)
        NH_GLA = dbg.get("_nh_gla", 4)
        for h in range(NH_HG):
            hg_head(h)
        if NH_GLA:
            gla_prep()
        for h in range(NH_GLA):
            gla_head(h)

        if dbg.get("_p2only"):
            if "modT" in dbg:
                S.add("sp", lambda e: e.dma_start(out=dbg_out["modT"], in_=modT[:]), r=["modT0", "modT1"], dma=True)
            S.emit(st)
            return nc
        st2.close()
        S.barrier()
        st3 = st.enter_context(ExitStack())

        def sb3(name, shape, dt=F32):
            return st3.enter_context(nc.sbuf_tensor("s_" + name, list(shape), dt))

        mergedT = sb3("mergedT", [128, 8, TO], BF16)
        yT = sb3("yT", [128, 16, TO], BF16)
        with ExitStack() as st3a:
            def sb3a(name, shape, dt=F32):
                return st3a.enter_context(nc.sbuf_tensor("s_" + name, list(shape), dt))
            stgA = [sb3a("stgA%d" % i, [128, 8, 128]) for i in range(2)]
            stgB = [sb3a("stgB%d" % i, [128, 8, 128]) for i in range(2)]
            wbA = [sb3a("wbA%d" % i, [128, 8, 128], BF16) for i in range(2)]
            wbB = [sb3a("wbB%d" % i, [128, 8, 128], BF16) for i in range(2)]
            wmA = [sb3a("wmA%d" % i, [128, 8, 128], BF16) for i in range(2)]
            wmB = [sb3a("wmB%d" % i, [128, 8, 128], BF16) for i in range(2)]
            sgA = [sb3a("sgA%d" % i, [128, 512]) for i in range(2)]
            sgB = [sb3a("sgB%d" % i, [128, 512]) for i in range(2)]
            tpA = [sb3a("tpA%d" % i, [128, 512]) for i in range(2)]
            tpB = [sb3a("tpB%d" % i, [128, 512]) for i in range(2)]
            S.barrier()
            ytok = [sb3a("ytok%d" % i, [128, 2 * D], BF16) for i in range(2)]
            for ti in range(NT_O):
                sl = ti % 2
                S.add("sp", lambda e, ti=ti, sl=sl: e.dma_start(out=ytok[sl][:], in_=ydram[ti * 128:(ti + 1) * 128, :]),
                      w=[("ytok", sl)], dma=True)
                for hb in range(2):
                    bank = (2 * ti + hb) % 4
                    pbv = psb[bank][:].bitcast(BF16)

                    def fyt(e, sl=sl, hb=hb, pbv=pbv):
                        ins = None
                        for c in range(8):
                            cc = hb * 8 + c
                            ins = e.transpose(pbv[:, c * 128:(c + 1) * 128], ytok[sl][:, cc * 128:(cc + 1) * 128], ident_b[:])
                        return ins
                    S.add("pe", fyt, r=[("ytok", sl), "ident_b"], w=[PSK(bank)])
                    S.add("act" if hb == 0 else "dve",
                          (lambda e, ti=ti, hb=hb, pbv=pbv: e.copy(out=yT[:, hb * 8:hb * 8 + 8, ti * 128:(ti + 1) * 128],
                                                                 in_=pbv[:, 0:1024].rearrange("p (c t) -> p c t", t=128)))
                          if hb == 0 else
                          (lambda e, ti=ti, hb=hb, pbv=pbv: e.tensor_copy(out=yT[:, hb * 8:hb * 8 + 8, ti * 128:(ti + 1) * 128],
                                                                        in_=pbv[:, 0:1024].rearrange("p (c t) -> p c t", t=128))),
                          r=[PSK(bank)], w=[("yT", c, ti) for c in range(hb * 8, hb * 8 + 8)])
            it = 0
            for ncn in range(8):
                p = ncn % 2
                S.add("sp", lambda e, p=p, ncn=ncn: e.dma_start(out=stgA[p][:], in_=w_bra_d[ncn].rearrange("(vc q) n -> q vc n", q=128)),
                      w=[("stgA", p)], dma=True)
                S.add("sp", lambda e, p=p, ncn=ncn: e.dma_start(out=stgB[p][:], in_=w_brb_d[ncn].rearrange("(vc q) n -> q vc n", q=128)),
                      w=[("stgB", p)], dma=True)
                S.add("pool", lambda e, p=p, ncn=ncn: e.dma_start(out=wmA[p][:], in_=w_m_d[ncn].rearrange("(dc q) n -> q dc n", q=128)),
                      w=[("wmA", p)], dma=True)
                S.add("pool", lambda e, p=p, ncn=ncn: e.dma_start(out=wmB[p][:], in_=w_m_d[8 + ncn].rearrange("(dc q) n -> q dc n", q=128)),
                      w=[("wmB", p)], dma=True)
                S.add("act", lambda e, p=p: e.activation(out=wbA[p][:], in_=stgA[p][:], func=AF.Copy, scale=hgn[:, 0:1]),
                      r=[("stgA", p), "hgn"], w=[("wbA", p)])
                for v2 in range(2):
                    S.add("act", lambda e, p=p, v2=v2: e.activation(
                        out=wbB[p][:].rearrange("q (h v) n -> q h v n", v=2)[:, :, v2, :],
                        in_=stgB[p][:].rearrange("q (h v) n -> q h v n", v=2)[:, :, v2, :],
                        func=AF.Copy, scale=glan[:, v2:v2 + 1]),
                        r=[("stgB", p), "glan"], w=[("wbB", p)])
                for tg in range(3):
                    b0 = 4 * (it % 2)
                    q = it % 2
                    it += 1
                    tk = slice(tg * 512, (tg + 1) * 512)
                    tiles = [("hT", t) for t in range(tg * 4, tg * 4 + 4)]
                    ytl = lambda c0: [("yT", c, t) for c in range(c0, c0 + 8) for t in range(tg * 4, tg * 4 + 4)]

                    def mm8(e, bank, wt, src, c0, tk=tk):
                        ins = None
                        for c in range(8):
                            ins = e.matmul(psb[bank][:, 0:512], lhsT=wt[:, c, :], rhs=src[:, c0 + c, tk],
                                           start=(c == 0), stop=(c == 7))
                        return ins
                    S.add("pe", lambda e, b0=b0, p=p, mm8=mm8: mm8(e, b0 + 2, wmA[p], hT, 0), r=[("wmA", p)] + tiles, w=[PSK(b0 + 2)])
                    S.add("pe", lambda e, b0=b0, p=p, mm8=mm8: mm8(e, b0 + 3, wmB[p], hT, 0), r=[("wmB", p)] + tiles, w=[PSK(b0 + 3)])
                    S.add("pe", lambda e, b0=b0, p=p, mm8=mm8: mm8(e, b0 + 0, wbA[p], yT, 0), r=[("wbA", p)] + ytl(0), w=[PSK(b0 + 0)])
                    S.add("pe", lambda e, b0=b0, p=p, mm8=mm8: mm8(e, b0 + 1, wbB[p], yT, 8), r=[("wbB", p)] + ytl(8), w=[PSK(b0 + 1)])
                    S.add("act", lambda e, b0=b0, q=q: e.activation(out=sgA[q][:], in_=psb[b0 + 2][:, 0:512], func=AF.Sigmoid),
                          r=[PSK(b0 + 2)], w=[("sgA", q)])
                    S.add("act", lambda e, b0=b0, q=q: e.activation(out=sgB[q][:], in_=psb[b0 + 3][:, 0:512], func=AF.Sigmoid),
                          r=[PSK(b0 + 3)], w=[("sgB", q)])
                    S.add("dve", lambda e, b0=b0, q=q: e.tensor_tensor(out=tpA[q][:], in0=psb[b0 + 0][:, 0:512], in1=sgA[q][:], op=ALU.mult),
                          r=[PSK(b0 + 0), ("sgA", q)], w=[("tpA", q)])
                    S.add("dve", lambda e, b0=b0, q=q: e.tensor_tensor(out=tpB[q][:], in0=psb[b0 + 1][:, 0:512], in1=sgB[q][:], op=ALU.mult),
                          r=[PSK(b0 + 1), ("sgB", q)], w=[("tpB", q)])
                    S.add("pool", lambda e, q=q, ncn=ncn, tk=tk: e.tensor_tensor(out=mergedT[:, ncn, tk], in0=tpA[q][:], in1=tpB[q][:], op=ALU.add),
                          r=[("tpA", q), ("tpB", q)], w=[("mergedT", ncn, tg)])
        if "mergedT" in dbg:
            S.add("sp", lambda e: e.dma_start(out=dbg_out["mergedT"], in_=mergedT[:]),
                  r=[("mergedT", n_, t_) for n_ in range(8) for t_ in range(3)], dma=True)
        S.barrier()
        x1s = yT[:].bitcast(F32).rearrange("p a b -> p (a b)").rearrange("p (t d) -> p t d", d=D)
        hflat = hT[:].rearrange("p a b -> p (a b)")
        h2T = hflat[:, 0:8 * TO].rearrange("p (a b) -> p a b", b=TO)
        cwT = sb3("cwT", [32, TO])
        ssq3 = sb3("ssq3", [128, NT_O])
        rs3 = sb3("rs3", [128, NT_O])
        with ExitStack() as st3b:
            def sb3b(name, shape, dt=F32):
                return st3b.enter_context(nc.sbuf_tensor("s_" + name, list(shape), dt))
            wout = sb3b("wout", [128, 8, D], BF16)
            wrt = sb3b("wrt", [128, 8, 36])
            g1bc = [sb3b("g1bc%d" % c, [128, D]) for c in range(2)]
            ones32 = sb3b("ones32", [128, 128])
            dg = [sb3b("dg%d" % i, [128, 128]) for i in range(2)]
            xr = [sb3b("xr%d" % i, [128, D]) for i in range(2)]
            tmpx = [sb3b("tmpx%d" % i, [128, D]) for i in range(2)]
            xn2 = [sb3b("xn2_%d" % i, [128, D]) for i in range(2)]
            h2f = [sb3b("h2f%d" % i, [128, 8, 128]) for i in range(2)]
            junk3 = sb3b("junk3", [128, D], BF16)
            NT = NT_O
            lga = sb3b("lga", [128, NT, 36])
            cwa = sb3b("cwa", [128, NT, 32])
            rA = sb3b("rA", [128, NT, 4])
            rB = sb3b("rB", [128, NT, 4])
            rC = sb3b("rC", [128, NT, 32])
            rsel = sb3b("rsel", [128, NT, 8])
            rsel2 = sb3b("rsel2", [128, NT, 8])
            rmk1 = sb3b("rmk1", [128, NT, 8])
            rmk2 = sb3b("rmk2", [128, NT, 8])
            rcw8 = sb3b("rcw8", [128, NT, 8])
            rs = sb3b("rs", [128, 10, NT])
            S.add("pool", lambda e: e.dma_start(out=wout[:], in_=w_out_d.rearrange("(c q) j -> q c j", q=128)), w=["wout"], dma=True)
            S.add("sp", lambda e: e.dma_start(out=wrt[:], in_=w_rt_d.rearrange("(c q) j -> q c j", q=128)), w=["wrt"], dma=True)
            S.add("pool", lambda e: e.memset(ones32[:], 1.0), w=["ones32"])

            def bcast_tile(dst, j0, c, nm):
                for jc in range(8):
                    d_ = dg[jc % 2]
                    S.add("dve", lambda e, d_=d_, jc=jc: e.tensor_scalar(out=d_[:], in0=ident_f[:], scalar1=modcol(j0, jc, c),
                                                                         scalar2=None, op0=ALU.mult),
                          r=["ident_f", "modT1"], w=[("dg", jc % 2)])
                    bank = jc % 2
                    S.add("pe", lambda e, d_=d_, bank=bank: e.matmul(psb[bank][:, 0:128], lhsT=ones32[:], rhs=d_[:], start=True, stop=True),
                          r=["ones32", ("dg", jc % 2)], w=[PSK(bank)])
                    S.add("act", lambda e, bank=bank, jc=jc: e.copy(out=dst[:, jc * 128:(jc + 1) * 128], in_=psb[bank][:, 0:128]),
                          r=[PSK(bank)], w=[nm])
            for c in range(2):
                bcast_tile(g1bc[c], 16, c, ("g1bc", c))

            def s3b_1(ti):
                c = cond_of_tile(ti)
                sl = ti % 2
                S.add("sp", lambda e, ti=ti, sl=sl: e.dma_start(out=xr[sl][:], in_=x_all[ti * 128:(ti + 1) * 128, :]),
                      w=[("xr", sl)], dma=True)
                for jh in range(2):
                    bank = 2 + jh

                    def fm(e, ti=ti, jh=jh, bank=bank):
                        ins = None
                        for n_ in range(8):
                            ins = e.matmul(psb[bank][:, 0:512], lhsT=mergedT[:, n_, ti * 128:(ti + 1) * 128],
                                           rhs=wout[:, n_, jh * 512:(jh + 1) * 512], start=(n_ == 0), stop=(n_ == 7))
                        return ins
                    S.add("pe", fm, r=["wout"] + [("mergedT", n_, ti // 4) for n_ in range(8)], w=[PSK(bank)])
                    js = slice(jh * 512, (jh + 1) * 512)
                    S.add("dve", lambda e, sl=sl, bank=bank, js=js, c=c: e.tensor_tensor(
                        out=tmpx[sl][:, js], in0=psb[bank][:, 0:512], in1=g1bc[c][:, js], op=ALU.mult),
                        r=[PSK(bank), ("g1bc", c)], w=[("tmpx", sl)])
                    S.add("pool", lambda e, sl=sl, js=js, ti=ti: e.tensor_tensor(
                        out=x1s[:, ti, js], in0=tmpx[sl][:, js], in1=xr[sl][:, js], op=ALU.add),
                        r=[("tmpx", sl), ("xr", sl)], w=[("x1s", ti)])

            def s3b_2(ti):
                c = cond_of_tile(ti)
                sl = ti % 2
                S.add("act", lambda e, ti=ti: e.activation(out=junk3[:], in_=x1s[:, ti, :], func=AF.Square, accum_out=ssq3[:, ti:ti + 1]),
                      r=[("x1s", ti)], w=["junk3", ("ssq3", ti)])
                S.add("act", lambda e, ti=ti: e.activation(out=rs3[:, ti:ti + 1], in_=ssq3[:, ti:ti + 1], func=AF.Ln, bias=epsb[:], scale=1.0 / D),
                      r=[("ssq3", ti), "epsb"], w=[("rs3", ti)])
                S.add("act", lambda e, ti=ti: e.activation(out=rs3[:, ti:ti + 1], in_=rs3[:, ti:ti + 1], func=AF.Exp, scale=-0.5),
                      r=[("rs3", ti)], w=[("rs3", ti)])
                S.add("dve", lambda e, ti=ti, sl=sl: e.tensor_scalar(out=xn2[sl][:], in0=x1s[:, ti, :], scalar1=rs3[:, ti:ti + 1],
                                                                     scalar2=None, op0=ALU.mult),
                      r=[("x1s", ti), ("rs3", ti)], w=[("xn2", sl)])
                for half in range(2):
                    bank = 4 + half

                    def ft(e, sl=sl, half=half, bank=bank):
                        ins = None
                        for j in range(4):
                            dc = half * 4 + j
                            ins = e.transpose(psb[bank][:, j * 128:(j + 1) * 128], xn2[sl][:, dc * 128:(dc + 1) * 128], ident_f[:])
                        return ins
                    S.add("pe", ft, r=[("xn2", sl), "ident_f"], w=[PSK(bank)])
                    for j in range(4):
                        dc = half * 4 + j
                        S.add("dve" if half == 0 else "act",
                              (lambda e, sl=sl, bank=bank, j=j, dc=dc, c=c: e.tensor_scalar(
                                  out=h2f[sl][:, dc, :], in0=psb[bank][:, j * 128:(j + 1) * 128],
                                  scalar1=A2[:, dc * 2 + c:dc * 2 + c + 1], scalar2=modcol(24, dc, c), op0=ALU.mult, op1=ALU.add))
                              if half == 0 else
                              (lambda e, sl=sl, bank=bank, j=j, dc=dc, c=c: e.activation(
                                  out=h2f[sl][:, dc, :], in_=psb[bank][:, j * 128:(j + 1) * 128], func=AF.Identity,
                                  bias=modcol(24, dc, c), scale=A2[:, dc * 2 + c:dc * 2 + c + 1])),
                              r=[PSK(bank), "A2", "modT1"], w=[("h2f", sl)])
                S.add("act", lambda e, sl=sl, ti=ti: e.copy(out=h2T[:, :, ti * 128:(ti + 1) * 128], in_=h2f[sl][:]),
                      r=[("h2f", sl)], w=[("h2T", ti)])
                def frt(e, sl=sl):
                    ins = None
                    for dc in range(8):
                        ins = e.matmul(psb[6][:, 0:36], lhsT=h2f[sl][:, dc, :], rhs=wrt[:, dc, :], start=(dc == 0), stop=(dc == 7))
                    return ins
                S.add("pe", frt, r=[("h2f", sl), "wrt"], w=[PSK(6)])
                S.add("act", lambda e, ti=ti: e.copy(out=lga[:, ti, :], in_=psb[6][:, 0:36]), r=[PSK(6)], w=[("lga", ti)])

            pipeline([s3b_1, s3b_2], NT_O, [0, 1])
            lgk = [("lga", t) for t in range(NT)]
            gm, gs, ptop, m1, m2, e2, den, w1, w2 = (rs[:, i, :] for i in range(9))
            lg = lga[:, :, 0:4]
            le = lga[:, :, 4:36].rearrange("p t (g e) -> p t g e", e=8)
            b3 = lambda ap, k: ap.rearrange("p (t o) -> p t o", o=1).to_broadcast([128, NT, k])
            V = lambda fn, r, w: S.add("dve", fn, r=r, w=w)
            V(lambda e: e.tensor_reduce(out=gm, in_=lg, axis=AX.X, op=ALU.max), lgk, ["r_gm"])
            V(lambda e: e.tensor_tensor(out=rA[:], in0=lg, in1=b3(gm, 4), op=ALU.is_ge), lgk + ["r_gm"], ["rA"])
            V(lambda e: e.tensor_tensor(out=rB[:], in0=lg, in1=b3(gm, 4), op=ALU.subtract), lgk + ["r_gm"], ["rB"])
            S.add("act", lambda e: e.activation(out=rB[:], in_=rB[:], func=AF.Exp), r=["rB"], w=["rB"])
            V(lambda e: e.tensor_reduce(out=gs, in_=rB[:], axis=AX.X, op=ALU.add), ["rB"], ["r_gs"])
            V(lambda e: e.reciprocal(out=ptop, in_=gs), ["r_gs"], ["r_ptop"])
            rC4 = rC[:].rearrange("p t (g e) -> p t g e", e=8)
            V(lambda e: e.tensor_tensor(out=rC4, in0=le, in1=rA[:].rearrange("p t (g o) -> p t g o", o=1).to_broadcast([128, NT, 4, 8]),
                                        op=ALU.mult), lgk + ["rA"], ["rC"])
            V(lambda e: e.tensor_reduce(out=rsel[:], in_=rC[:].rearrange("p t (g e) -> p t e g", e=8), axis=AX.X, op=ALU.add),
              ["rC"], ["rsel"])
            V(lambda e: e.tensor_reduce(out=m1, in_=rsel[:], axis=AX.X, op=ALU.max), ["rsel"], ["r_m1"])
            V(lambda e: e.tensor_tensor(out=rmk1[:], in0=rsel[:], in1=b3(m1, 8), op=ALU.is_ge), ["rsel", "r_m1"], ["rmk1"])
            V(lambda e: e.scalar_tensor_tensor(out=rsel2[:], in0=rmk1[:], scalar=-1e30, in1=rsel[:], op0=ALU.mult, op1=ALU.add),
              ["rmk1", "rsel"], ["rsel2"])
            V(lambda e: e.tensor_reduce(out=m2, in_=rsel2[:], axis=AX.X, op=ALU.max), ["rsel2"], ["r_m2"])
            V(lambda e: e.tensor_tensor(out=rmk2[:], in0=rsel2[:], in1=b3(m2, 8), op=ALU.is_ge), ["rsel2", "r_m2"], ["rmk2"])
            V(lambda e: e.tensor_sub(out=e2, in0=m2, in1=m1), ["r_m1", "r_m2"], ["r_e2"])
            S.add("act", lambda e: e.activation(out=e2, in_=e2, func=AF.Exp), r=["r_e2"], w=["r_e2"])
            V(lambda e: e.tensor_scalar(out=den, in0=e2, scalar1=1.0, scalar2=None, op0=ALU.add), ["r_e2"], ["r_den"])
            V(lambda e: e.reciprocal(out=den, in_=den), ["r_den"], ["r_den"])
            V(lambda e: e.tensor_tensor(out=w1, in0=den, in1=ptop, op=ALU.mult), ["r_den", "r_ptop"], ["r_w1"])
            V(lambda e: e.tensor_tensor(out=w2, in0=w1, in1=e2, op=ALU.mult), ["r_w1", "r_e2"], ["r_w2"])
            V(lambda e: e.tensor_tensor(out=rcw8[:], in0=rmk1[:], in1=b3(w1, 8), op=ALU.mult), ["rmk1", "r_w1"], ["rcw8"])
            V(lambda e: e.tensor_tensor(out=rmk2[:], in0=rmk2[:], in1=b3(w2, 8), op=ALU.mult), ["rmk2", "r_w2"], ["rmk2"])
            V(lambda e: e.tensor_tensor(out=rcw8[:], in0=rcw8[:], in1=rmk2[:], op=ALU.add), ["rcw8", "rmk2"], ["rcw8"])
            cwa4 = cwa[:].rearrange("p t (g e) -> p t g e", e=8)
            for g_ in range(4):
                V(lambda e, g_=g_: e.tensor_tensor(out=cwa4[:, :, g_, :], in0=rcw8[:], in1=rA[:, :, g_:g_ + 1].to_broadcast([128, NT, 8]),
                                                   op=ALU.mult), ["rcw8", "rA"], ["cwa"])
            for t3 in range(3):
                def ftc(e, t3=t3):
                    ins = None
                    for j in range(4):
                        ti = t3 * 4 + j
                        ins = e.transpose(psb[5 + t3][0:32, j * 128:(j + 1) * 128], cwa[:, ti, :], ident_f[:])
                    return ins
                S.add("pe", ftc, r=["cwa", "ident_f"], w=[PSK(5 + t3)])
                S.add("act", lambda e, t3=t3: e.copy(out=cwT[:, t3 * 512:(t3 + 1) * 512], in_=psb[5 + t3][0:32, 0:512]),
                      r=[PSK(5 + t3)], w=[("cwT", t3 * 4 + j) for j in range(4)])
        if "x1" in dbg:
            S.add("sp", lambda e: e.dma_start(out=dbg_out["x1"], in_=x1s[:]), r=[("x1s", t) for t in range(NT_O)], dma=True)
        if "cwT" in dbg:
            S.add("sp", lambda e: e.dma_start(out=dbg_out["cwT"], in_=cwT[:]), r=[("cwT", t) for t in range(NT_O)], dma=True)
        S.barrier()

        with ExitStack() as st4:
            def sb4(name, shape, dt=F32):
                return st4.enter_context(nc.sbuf_tensor("s_" + name, list(shape), dt))
            accA = mergedT[:].bitcast(F32).rearrange("p a b -> p (a b)").rearrange("p (t d) -> p t d", d=D)
            accB = sb4("accB", [128, 6, D])
            acc = lambda ti: accA[:, ti, :] if ti < 6 else accB[:, ti - 6, :]
            spare = hflat[:, 8 * TO:8 * TA]
            wsl = lambda i: spare[:, i * 2048:(i + 1) * 2048].rearrange("p (c f) -> p c f", f=256)
            wg = [wsl(0), wsl(1)]
            wu = [wsl(2), wsl(3)]
            wd = [sb4("wd%d" % i, [128, 2, D], BF16) for i in range(2)]
            selE = [sb4("selE%d" % i, [32, 128]) for i in range(2)]
            selEb = [sb4("selEb%d" % i, [32, 128], BF16) for i in range(2)]
            cw_hi = sb4("cw_hi", [32, TO], BF16)
            cw_lo = sb4("cw_lo", [32, TO], BF16)
            cw_d = sb4("cw_d", [32, TO])
            cwk = [("cwT", t) for t in range(NT_O)]
            S.add("dve", lambda e: e.tensor_copy(out=cw_hi[:], in_=cwT[:]), r=cwk, w=["cw_hi"])
            S.add("dve", lambda e: e.tensor_tensor(out=cw_d[:], in0=cwT[:], in1=cw_hi[:], op=ALU.subtract), r=cwk + ["cw_hi"], w=["cw_d"])
            S.add("dve", lambda e: e.tensor_copy(out=cw_lo[:], in_=cw_d[:]), r=["cw_d"], w=["cw_lo"])
            sA = [sb4("sA%d" % i, [128, 512]) for i in range(2)]
            tA = [sb4("tA%d" % i, [128, 512]) for i in range(2)]
            cbs = [sb4("cbs%d" % i, [128, 512]) for i in range(2)]
            hid = [[sb4("hid%d_%d" % (i, j), [128, 512], BF16) for j in range(2)] for i in range(2)]
            g2bc = [sb4("g2bc%d" % c, [128, D]) for c in range(2)]
            fngbc = sb4("fngbc", [128, D])
            ones32b = sb4("ones32b", [128, 128])
            dgb = [sb4("dgb%d" % i, [128, 128]) for i in range(2)]
            junk4 = sb4("junk4", [128, D], BF16)
            ssq4 = sb4("ssq4", [128, NT_O])
            rs4 = sb4("rs4", [128, NT_O])
            S.add("pool", lambda e: e.memset(ones32b[:], 1.0), w=["ones32b"])
            S.add("sp", lambda e: e.dma_start(out=fngbc[:], in_=fng_d.to_broadcast([128, D])), w=["fngbc"], dma=True)
            for c in range(2):
                for jc in range(8):
                    d_ = dgb[jc % 2]
                    bank = jc % 2
                    S.add("dve", lambda e, d_=d_, jc=jc, c=c: e.tensor_scalar(out=d_[:], in0=ident_f[:], scalar1=modcol(40, jc, c),
                                                                              scalar2=None, op0=ALU.mult),
                          r=["ident_f", "modT1"], w=[("dgb", jc % 2)])
                    S.add("pe", lambda e, d_=d_, bank=bank: e.matmul(psb[bank][:, 0:128], lhsT=ones32b[:], rhs=d_[:], start=True, stop=True),
                          r=["ones32b", ("dgb", jc % 2)], w=[PSK(bank)])
                    S.add("act", lambda e, bank=bank, jc=jc, c=c: e.copy(out=g2bc[c][:, jc * 128:(jc + 1) * 128], in_=psb[bank][:, 0:128]),
                          r=[PSK(bank)], w=[("g2bc", c)])

            NE = dbg.get("_nexp", NEXP)
            itc = 0
            pending = [None]
            for e_ in range(NE):
                pp = e_ % 2
                S.add("pool", lambda e, pp=pp, e_=e_: e.dma_start(out=wg[pp], in_=w_eg_d[e_].rearrange("(c q) f -> q c f", q=128)),
                      w=[("wg", pp)], dma=True)
                S.add("pool", lambda e, pp=pp, e_=e_: e.dma_start(out=wu[pp], in_=w_eu_d[e_].rearrange("(c q) f -> q c f", q=128)),
                      w=[("wu", pp)], dma=True)
                S.add("pool", lambda e, pp=pp, e_=e_: e.dma_start(out=wd[pp][:], in_=w_ed_d[e_].rearrange("(c q) d -> q c d", q=128)),
                      w=[("wd", pp)], dma=True)
                S.add("pool", lambda e, pp=pp: e.memset(selE[pp][:], 0.0), w=[("selE", pp)])
                S.add("pool", lambda e, pp=pp, e_=e_: e.affine_select(out=selE[pp][:], in_=selE[pp][:], pattern=[[0, 128]],
                                                                     compare_op=ALU.not_equal, fill=1.0, base=-e_, channel_multiplier=1),
                      r=[("selE", pp)], w=[("selE", pp)])
                S.add("pool", lambda e, pp=pp: e.tensor_copy(out=selEb[pp][:], in_=selE[pp][:]), r=[("selE", pp)], w=[("selEb", pp)])
                for tg in range(3):
                    tk = slice(tg * 512, (tg + 1) * 512)
                    hs = itc % 2
                    itc += 1
                    def fsel(e, pp=pp, tk=tk):
                        e.matmul(psb[4][:, 0:512], lhsT=selEb[pp][:], rhs=cw_hi[:, tk], start=True, stop=False)
                        return e.matmul(psb[4][:, 0:512], lhsT=selEb[pp][:], rhs=cw_lo[:, tk], start=False, stop=True)
                    S.add("pe", fsel, r=[("selEb", pp), "cw_hi", "cw_lo"], w=[PSK(4)])
                    S.add("act", lambda e, hs=hs: e.copy(out=cbs[hs][:], in_=psb[4][:, 0:512]), r=[PSK(4)], w=[("cbs", hs)])
                    for f in range(2):
                        def fg(e, wt, bank, f=f, tk=tk):
                            ins = None
                            for dc in range(8):
                                ins = e.matmul(psb[bank][:, 0:512], lhsT=wt[:, dc, f * 128:(f + 1) * 128], rhs=h2T[:, dc, tk],
                                               start=(dc == 0), stop=(dc == 7))
                            return ins
                        ba, bu = 2 * f, 2 * f + 1
                        h2k = [("h2T", t) for t in range(tg * 4, tg * 4 + 4)]
                        S.add("pe", lambda e, fg=fg, pp=pp, ba=ba: fg(e, wg[pp], ba), r=[("wg", pp)] + h2k, w=[PSK(ba)])
                        S.add("pe", lambda e, fg=fg, pp=pp, bu=bu: fg(e, wu[pp], bu), r=[("wu", pp)] + h2k, w=[PSK(bu)])
                        S.add("act", lambda e, ba=ba, f=f: e.activation(out=sA[f][:], in_=psb[ba][:, 0:512], func=AF.Silu),
                              r=[PSK(ba)], w=[("sA", f)])
                        S.add("dve", lambda e, bu=bu, f=f: e.tensor_tensor(out=tA[f][:], in0=psb[bu][:, 0:512], in1=sA[f][:], op=ALU.mult),
                              r=[PSK(bu), ("sA", f)], w=[("tA", f)])
                        S.add("pool", lambda e, f=f, hs=hs: e.tensor_tensor(out=hid[hs][f][:], in0=tA[f][:], in1=cbs[hs][:], op=ALU.mult),
                              r=[("tA", f), ("cbs", hs)], w=[("hid", hs, f)])
                    def emit_down(tg=tg, hs=hs, pp=pp, e_=e_):
                        for t4 in range(4):
                            ti = tg * 4 + t4
                            for dh in range(2):
                                bank = 5 + (t4 * 2 + dh) % 3

                                def fd(e, t4=t4, dh=dh, bank=bank):
                                    ins = None
                                    for f in range(2):
                                        ins = e.matmul(psb[bank][:, 0:512], lhsT=hid[hs][f][:, t4 * 128:(t4 + 1) * 128],
                                                       rhs=wd[pp][:, f, dh * 512:(dh + 1) * 512], start=(f == 0), stop=(f == 1))
                                    return ins
                                S.add("pe", fd, r=[("hid", hs, 0), ("hid", hs, 1), ("wd", pp)], w=[PSK(bank)])
                                ds = slice(dh * 512, (dh + 1) * 512)
                                a_ap = acc(ti)[:, ds]
                                if e_ == 0:
                                    S.add("dve", lambda e, a_ap=a_ap, bank=bank: e.tensor_copy(out=a_ap, in_=psb[bank][:, 0:512]),
                                          r=[PSK(bank)], w=[("acc", ti, dh)])
                                else:
                                    S.add("dve", lambda e, a_ap=a_ap, bank=bank: e.tensor_tensor(
                                        out=a_ap, in0=psb[bank][:, 0:512], in1=a_ap, op=ALU.add),
                                        r=[PSK(bank), ("acc", ti, dh)], w=[("acc", ti, dh)])
                    if pending[0] is not None:
                        pending[0]()
                    pending[0] = emit_down
            pending[0]()
            def fin_1(ti):
                c = cond_of_tile(ti)
                a_ap = acc(ti)
                ak = [("acc", ti, 0), ("acc", ti, 1)]
                S.add("pool", lambda e: e.tensor_tensor(out=a_ap, in0=a_ap, in1=g2bc[c][:], op=ALU.mult),
                      r=ak + [("g2bc", c)], w=ak)
                S.add("dve", lambda e: e.tensor_tensor(out=a_ap, in0=a_ap, in1=x1s[:, ti, :], op=ALU.add),
                      r=ak + [("x1s", ti)], w=ak)
                S.add("act", lambda e: e.activation(out=junk4[:], in_=a_ap, func=AF.Square, accum_out=ssq4[:, ti:ti + 1]),
                      r=ak, w=["junk4", ("ssq4", ti)])
                S.add("act", lambda e: e.activation(out=rs4[:, ti:ti + 1], in_=ssq4[:, ti:ti + 1], func=AF.Ln, bias=epsb[:], scale=1.0 / D),
                      r=[("ssq4", ti), "epsb"], w=[("rs4", ti)])
                S.add("act", lambda e: e.activation(out=rs4[:, ti:ti + 1], in_=rs4[:, ti:ti + 1], func=AF.Exp, scale=-0.5),
                      r=[("rs4", ti)], w=[("rs4", ti)])

            def fin_2(ti):
                a_ap = acc(ti)
                ak = [("acc", ti, 0), ("acc", ti, 1)]
                S.add("dve", lambda e: e.tensor_tensor(out=a_ap, in0=a_ap, in1=fngbc[:], op=ALU.mult), r=ak + ["fngbc"], w=ak)
                S.add("act", lambda e: e.activation(out=a_ap, in_=a_ap, func=AF.Copy, scale=rs4[:, ti:ti + 1]),
                      r=ak + [("rs4", ti)], w=ak)
                S.add("sp", lambda e: e.dma_start(out=y_out[ti * 128:(ti + 1) * 128, :], in_=a_ap), r=ak, dma=True)

            pipeline([fin_1, fin_2], NT_O, [0, 1])
        if "hT" in dbg:
            tmp = sb("dbg_hT", [128, 8, TA], F32)
            S.add("dve", lambda e: e.tensor_copy(out=tmp[:], in_=hT[:]), r=[("hT", i) for i in range(NT_A)], w=["dbg_hT"])
            S.add("sp", lambda e: e.dma_start(out=dbg_out["hT"], in_=tmp[:]), r=["dbg_hT"], dma=True)
        if "modT" in dbg:
            S.add("sp", lambda e: e.dma_start(out=dbg_out["modT"], in_=modT[:]), r=["modT"], dma=True)

        S.emit(st)
    return nc


def _prep_core(c, I):
    b, par = c // 2, c % 2
    f = np.ascontiguousarray
    xs = I["x_sample"][b]
    if par:
        xs = xs[::-1]
    p0 = I["x_prompt"][2 * c]
    p1 = I["x_prompt"][2 * c + 1]
    if par:
        p0, p1 = p0[::-1], p1[::-1]
    x_all = np.concatenate([xs[:1024], p0, p1, xs[1024:]], 0)
    cond = np.stack([I["c"][b], I["c_ctx"]], 0)
    condT = cond.reshape(2, 8, 128).transpose(2, 1, 0).reshape(128, 16)
    w_in = I["w_in"][0]
    o = np.cumsum([0, 1024, 1024, 1024, 1024, 1024, 512, 512, 1024, 1024, 16, 16, 1024, 1024])
    hq, hf0, hf1, hi, hgate, gq, gk, gv, gr, ga0, ga1, ma, mb = [w_in[:, o[i]:o[i + 1]] for i in range(13)]
    d0, d1 = (1, 0) if par else (0, 1)
    if par:
        hf0, hf1 = hf1, hf0
        ga0, ga1 = ga1, ga0
    hd = lambda w, h, n: w[:, h * n:(h + 1) * n]
    w_hg = np.stack([np.concatenate([hd(hq, h, 128), hd(hf0, h, 128), hd(hf1, h, 128), hd(hi, h, 128),
                                     hd(hgate, h, 128)], 1) for h in range(8)], 0)
    w_gla = np.stack([np.concatenate([hd(gq, h, 128), hd(gk, h, 128), hd(gv, h, 256), hd(gr, h, 256)], 1)
                      for h in range(4)], 0)
    w_ga = np.concatenate([ga0, ga1], 1)
    w_m = np.concatenate([ma.reshape(D, 8, 128).transpose(1, 0, 2), mb.reshape(D, 8, 128).transpose(1, 0, 2)], 0)
    lbp = np.concatenate([I["hg_lb_param"][0].reshape(8, 128).T, I["hg_lb_param"][1].reshape(8, 128).T], 1)
    wa = I["gla_wa_up"][0][[d0, d1]]
    ba = I["gla_ba"][0][[d0, d1]]
    baT = ba.reshape(2, 4, 128).transpose(2, 0, 1).reshape(128, 8)
    m = {
        "x_all": x_all, "condT": condT, "ada_w": I["ada_w"][0],
        "ada_bT": I["ada_b"][0].reshape(48, 128).T,
        "g1T": I["norm1_g"][0].reshape(8, 128).T, "g2T": I["norm2_g"][0].reshape(8, 128).T,
        "w_hg": w_hg, "w_gla": w_gla, "w_ga": w_ga, "w_m": w_m, "lbp": lbp,
        "hgn": I["hg_norm_g"][0].reshape(128, 1), "glan": I["gla_norm_g"][0].reshape(2, 128).T,
        "wa_up": wa, "baT": baT,
        "w_bra": I["w_br_a"][0].reshape(D, 8, 128).transpose(1, 0, 2),
        "w_brb": I["w_br_b"][0].reshape(D, 8, 128).transpose(1, 0, 2),
        "w_out": I["w_out"][0],
        "w_rt": np.concatenate([I["w_router_group"][0]] + [I["w_router_expert"][0][g] for g in range(4)], 1),
        "w_eg": I["w_exp_gate"][0], "w_eu": I["w_exp_up"][0], "w_ed": I["w_exp_down"][0],
        "fng": I["final_norm_g"].reshape(1, D),
        "s0_hg": I["state_hgrn"][b, 0][[d0, d1]], "s0_gla": I["state_gla"][b, 0][[d0, d1]],
    }
    return {k: f(np.asarray(v, dtype=np.float32)) for k, v in m.items()}


_NC_CACHE = {}


def kernel(**inputs):
    I = {k: np.asarray(v) for k, v in inputs.items()}
    if "nc" not in _NC_CACHE:
        _NC_CACHE["nc"] = build()
    nc = _NC_CACHE["nc"]
    in_maps = [_prep_core(c, I) for c in range(8)]
    res = run_bass_kernel_spmd(nc, in_maps, core_ids=list(range(8)))
    y_prompt = np.zeros((16, 256, D), np.float32)
    y_sample = np.zeros((4, 2048, D), np.float32)
    st_h = np.zeros((16, 1, 2, 8, 128, 128), np.float32)
    st_g = np.zeros((16, 1, 2, 4, 128, 256), np.float32)
    for c in range(8):
        r = res.results[c]
        b, par = c // 2, c % 2
        y = r["y_out"]
        ys, yp0, yp1 = y[:1024], y[1024:1280], y[1280:1536]
        if par:
            y_sample[b, 1024:] = ys[::-1]
            y_prompt[2 * c] = yp0[::-1]
            y_prompt[2 * c + 1] = yp1[::-1]
        else:
            y_sample[b, :1024] = ys
            y_prompt[2 * c] = yp0
            y_prompt[2 * c + 1] = yp1
        sh, sg = r["st_hg"], r["st_gla"]
        if par:
            sh, sg = sh[:, ::-1], sg[:, ::-1]
        st_h[2 * c:2 * c + 2, 0] = sh
        st_g[2 * c:2 * c + 2, 0] = sg
    return (y_prompt, y_sample, st_h, st_g)
```
